# Optimizing a Trainium2 kernel written in Bass

```python
import jax, jax.numpy as jnp
from jax import lax
import numpy as np

D_MODEL = 1024
BATCH = 8
SEQ = 4096
DEPTH = 2
DEC_BATCH = 4
DEC_SEQ = 8192
PAST_LEN = 128

GRID_W = 64
NA_HEADS = 8
NA_HD = 64
NA_WIDTH = NA_HEADS * NA_HD
WIN_R = 8
WIN_C = 16
HG_HEADS = 4
HG_DK = 128
HG_DV = 128
HG_KW = HG_HEADS * HG_DK
HG_VW = HG_HEADS * HG_DV
HG_CHUNK = 64
F_MIN = 1e-20
D_CONV = 512
CONV_K = 31
N_BRANCH = 3
MEM_TOKENS = 256
MEM_HEADS = 4
MEM_HD = 128
MEM_WIDTH = MEM_HEADS * MEM_HD
N_EXPERTS = 16
EC_CAPACITY = 2
D_FF = 2752
EPS = 1e-6

_IN_SIZES = (NA_WIDTH, NA_WIDTH, NA_WIDTH, HG_KW, HG_KW, HG_KW, HG_VW, HG_VW,
             D_CONV, D_CONV, D_MODEL, D_MODEL, D_MODEL)
D_IN = 3 * NA_WIDTH + 3 * HG_KW + 2 * HG_VW + 2 * D_CONV + N_BRANCH * D_MODEL

kernel_name = "hybrid_natten_hgrn2_conformer_ec_encoder"

F32 = jnp.float32


def _split_points():
    pts, acc = [], 0
    for s in _IN_SIZES[:-1]:
        acc += s
        pts.append(acc)
    return pts


def _rms(x, g):
    xf = x.astype(F32)
    y = xf * lax.rsqrt(jnp.mean(xf * xf, axis=-1, keepdims=True) + EPS)
    return (y * g.astype(F32)).astype(x.dtype)


def _neighbourhood_attention(q, k, v, rel_bias):
    B, L, H, hd = q.shape
    rows = L // GRID_W
    wr = min(WIN_R, rows)
    qg = q.reshape(B, rows, GRID_W, H, hd)
    kg = k.reshape(B, rows, GRID_W, H, hd)
    vg = v.reshape(B, rows, GRID_W, H, hd)
    col = jnp.arange(GRID_W)
    c_idx = jnp.clip(col - WIN_C // 2, 0, GRID_W - WIN_C)[:, None] + jnp.arange(WIN_C)[None, :]
    dc_idx = (c_idx - col[:, None] + WIN_C - 1)[:, None, :]
    scale = hd ** -0.5

    def row_block(r):
        r0 = jnp.clip(r - wr // 2, 0, rows - wr)
        k_nb = lax.dynamic_slice_in_dim(kg, r0, wr, axis=1)[:, :, c_idx]
        v_nb = lax.dynamic_slice_in_dim(vg, r0, wr, axis=1)[:, :, c_idx]
        q_r = lax.dynamic_index_in_dim(qg, r, axis=1, keepdims=False)
        dr_idx = (r0 + jnp.arange(wr) - r + WIN_R - 1)[None, :, None]
        bias = rel_bias[:, dr_idx, dc_idx].astype(F32)
        s = jnp.einsum('bqhd,brqchd->bhqrc', q_r, k_nb).astype(F32) * scale + bias[None]
        p = jax.nn.softmax(s.reshape(B, H, GRID_W, wr * WIN_C), axis=-1).reshape(s.shape)
        return jnp.einsum('bhqrc,brqchd->bqhd', p.astype(v.dtype), v_nb)

    o = lax.map(row_block, jnp.arange(rows))
    return o.transpose(1, 0, 2, 3, 4).reshape(B, L, H * hd)


def _hgrn2_direction(q, k, logf, v):
    B, L, H, dk = q.shape
    dv = v.shape[-1]
    n = L // HG_CHUNK

    def to_chunks(a):
        return a.reshape(B, n, HG_CHUNK, H, a.shape[-1]).transpose(1, 0, 3, 2, 4)

    lower = jnp.tril(jnp.ones((HG_CHUNK, HG_CHUNK), dtype=bool))[:, :, None]

    def step(S, blk):
        qi, ki, gi, vi = blk
        A = jnp.cumsum(gi, axis=2)
        o_inter = jnp.einsum('bhtk,bhkv->bhtv', qi * jnp.exp(A), S)
        diff = A[:, :, :, None, :] - A[:, :, None, :, :]
        decay = jnp.where(lower, jnp.exp(jnp.where(lower, diff, 0.0)), 0.0)
        att = jnp.einsum('bhtk,bhtsk,bhsk->bhts', qi, decay, ki)
        o_intra = jnp.einsum('bhts,bhsv->bhtv', att, vi)
        A_end = A[:, :, -1, :]
        S = jnp.exp(A_end)[..., None] * S + jnp.einsum(
            'bhsk,bhsv->bhkv', ki * jnp.exp(A_end[:, :, None, :] - A), vi)
        return S, o_inter + o_intra

    S0 = jnp.zeros((B, H, dk, dv), F32)
    _, o = lax.scan(step, S0, tuple(map(to_chunks, (q, k, logf, v))))
    return o.transpose(1, 0, 3, 2, 4).reshape(B, L, H, dv)


def _hgrn2_branch(hq, hff, hfb, hv, hg, lb_f, lb_b, out_g):
    B, L, _ = hq.shape

    def heads(a, d):
        return a.astype(F32).reshape(B, L, HG_HEADS, d)

    q = heads(jax.nn.silu(hq.astype(F32)) * HG_DK ** -0.5, HG_DK)
    v = heads(hv, HG_DV)

    def gate(hf, lb):
        hf = hf.astype(F32)
        f = lb + (1.0 - lb) * jax.nn.sigmoid(hf)
        logf = jnp.log(jnp.maximum(f, F_MIN))
        k = (1.0 - lb) * jax.nn.sigmoid(-hf)
        return heads(logf, HG_DK), heads(k, HG_DK)

    logf_f, k_f = gate(hff, lb_f)
    logf_b, k_b = gate(hfb, lb_b)
    o_f = _hgrn2_direction(q, k_f, logf_f, v)
    flip = lambda a: jnp.flip(a, axis=1)
    o_b = flip(_hgrn2_direction(flip(q), flip(k_b), flip(logf_b), flip(v)))
    o = _rms(o_f + o_b, out_g) * jax.nn.silu(heads(hg, HG_DV))
    return o.reshape(B, L, HG_VW).astype(hq.dtype)


def _conv_module(a, b, conv_w, conv_b, ln_g, ln_b):
    u = a * jax.nn.sigmoid(b)
    y = lax.conv_general_dilated(
        u, conv_w[:, None, :].astype(u.dtype), window_strides=(1,),
        padding=[(CONV_K // 2, CONV_K // 2)], dimension_numbers=('NWC', 'WIO', 'NWC'),
        feature_group_count=D_CONV)
    yf = y.astype(F32) + conv_b.astype(F32)
    mu = jnp.mean(yf, axis=-1, keepdims=True)
    var = jnp.mean(jnp.square(yf - mu), axis=-1, keepdims=True)
    yn = (yf - mu) * lax.rsqrt(var + EPS) * ln_g.astype(F32) + ln_b.astype(F32)
    return jax.nn.silu(yn).astype(a.dtype)


def _memory_attention(h, mem_n, wq, wkv, qn, kn, wo):
    B, L, _ = h.shape
    M = mem_n.shape[1]
    q = _rms((h @ wq).reshape(B, L, MEM_HEADS, MEM_HD), qn)
    k, v = jnp.split(mem_n @ wkv, 2, axis=-1)
    k = _rms(k.reshape(B, M, MEM_HEADS, MEM_HD), kn)
    v = v.reshape(B, M, MEM_HEADS, MEM_HD)
    s = jnp.einsum('blhd,bmhd->bhlm', q, k).astype(F32) * MEM_HD ** -0.5
    p = jax.nn.softmax(s, axis=-1)
    o = jnp.einsum('bhlm,bmhd->blhd', p.astype(v.dtype), v).reshape(B, L, MEM_WIDTH)
    return o @ wo


def _expert_choice_ffn(h, w_router, w_gu, w_down):
    B, L, D = h.shape
    N = B * L
    cap = EC_CAPACITY * N // N_EXPERTS
    hf = h.reshape(N, D)
    aff = jax.nn.softmax((hf @ w_router).astype(F32), axis=-1)
    g, idx = lax.top_k(aff.T, cap)
    xe = hf[idx]

    def expert(args):
        xi, wgu, wd = args
        gate, up = jnp.split(xi @ wgu, 2, axis=-1)
        return (jax.nn.silu(gate) * up) @ wd

    ye = lax.map(expert, (xe, w_gu, w_down)) * g[..., None].astype(h.dtype)
    out = jnp.zeros((N, D), ye.dtype).at[idx.reshape(-1)].add(ye.reshape(-1, D))
    return out.reshape(B, L, D)


def _trunk(x, mem, norm_mix, w_in, na_q_norm, na_k_norm, na_rel_bias, w_na_br, hg_lb, hg_out_norm,
           w_hg_br, conv_w, conv_b, conv_ln_g, conv_ln_b, w_cv_br, w_out, norm_mem, mem_kv_norm,
           wq_mem, wkv_mem, mem_q_norm, mem_k_norm, wo_mem, norm_ffn, w_router, w_gate_up, w_down):
    B, L, _ = x.shape
    P = jax.nn.softmax(hg_lb.astype(F32), axis=1)
    lb = jnp.cumsum(P, axis=1) - P[:, :1]
    pts = _split_points()
    for l in range(DEPTH):
        h = _rms(x, norm_mix[l])
        (na_q, na_k, na_v, hg_q, hg_ff, hg_fb, hg_v, hg_g, cv_a, cv_b,
         g_na, g_hg, g_cv) = jnp.split(h @ w_in[l], pts, axis=-1)
        q = _rms(na_q.reshape(B, L, NA_HEADS, NA_HD), na_q_norm[l])
        k = _rms(na_k.reshape(B, L, NA_HEADS, NA_HD), na_k_norm[l])
        v = na_v.reshape(B, L, NA_HEADS, NA_HD)
        br_na = _neighbourhood_attention(q, k, v, na_rel_bias[l]) @ w_na_br[l]
        br_hg = _hgrn2_branch(hg_q, hg_ff, hg_fb, hg_v, hg_g, lb[0, l], lb[1, l], hg_out_norm[l]) @ w_hg_br[l]
        br_cv = _conv_module(cv_a, cv_b, conv_w[l], conv_b[l], conv_ln_g[l], conv_ln_b[l]) @ w_cv_br[l]
        merged = (jax.nn.sigmoid(g_na) * br_na + jax.nn.sigmoid(g_hg) * br_hg
                  + jax.nn.sigmoid(g_cv) * br_cv)
        x = x + merged @ w_out[l]
        x = x + _memory_attention(_rms(x, norm_mem[l]), _rms(mem, mem_kv_norm[l]), wq_mem[l], wkv_mem[l],
                                  mem_q_norm[l], mem_k_norm[l], wo_mem[l])
        x = x + _expert_choice_ffn(_rms(x, norm_ffn[l]), w_router[l], w_gate_up[l], w_down[l])
    return x


def setup_inputs(seed: int = 0) -> dict:
    key = jax.random.key(seed)
    ks = jax.random.split(key, 32)
    nrm = lambda k, shape, s: jax.random.normal(k, shape, F32) * s
    gain = lambda k, shape: 1.0 + 0.1 * jax.random.normal(k, shape, F32)
    return {
        "x_prompt": nrm(ks[0], (BATCH, SEQ, D_MODEL), 1.0),
        "x_sample": nrm(ks[1], (DEC_BATCH, DEC_SEQ, D_MODEL), 1.0),
        "mem_prompt": nrm(ks[2], (BATCH, MEM_TOKENS, D_MODEL), 1.0),
        "mem_sample": nrm(ks[3], (DEC_BATCH, MEM_TOKENS, D_MODEL), 1.0),
        "norm_mix": gain(ks[4], (DEPTH, D_MODEL)),
        "w_in": nrm(ks[5], (DEPTH, D_MODEL, D_IN), D_MODEL ** -0.5),
        "na_q_norm": gain(ks[6], (DEPTH, NA_HD)),
        "na_k_norm": gain(ks[7], (DEPTH, NA_HD)),
        "na_rel_bias": nrm(ks[8], (DEPTH, NA_HEADS, 2 * WIN_R - 1, 2 * WIN_C - 1), 0.1),
        "w_na_br": nrm(ks[9], (DEPTH, NA_WIDTH, D_MODEL), NA_WIDTH ** -0.5),
        "hg_lb": nrm(ks[10], (2, DEPTH, HG_KW), 0.5),
        "hg_out_norm": gain(ks[11], (DEPTH, HG_DV)),
        "w_hg_br": nrm(ks[12], (DEPTH, HG_VW, D_MODEL), HG_VW ** -0.5),
        "conv_w": nrm(ks[13], (DEPTH, CONV_K, D_CONV), CONV_K ** -0.5),
        "conv_b": nrm(ks[14], (DEPTH, D_CONV), 0.01),
        "conv_ln_g": gain(ks[15], (DEPTH, D_CONV)),
        "conv_ln_b": nrm(ks[16], (DEPTH, D_CONV), 0.01),
        "w_cv_br": nrm(ks[17], (DEPTH, D_CONV, D_MODEL), D_CONV ** -0.5),
        "w_out": nrm(ks[18], (DEPTH, D_MODEL, D_MODEL), D_MODEL ** -0.5),
        "norm_mem": gain(ks[19], (DEPTH, D_MODEL)),
        "mem_kv_norm": gain(ks[20], (DEPTH, D_MODEL)),
        "wq_mem": nrm(ks[21], (DEPTH, D_MODEL, MEM_WIDTH), D_MODEL ** -0.5),
        "wkv_mem": nrm(ks[22], (DEPTH, D_MODEL, 2 * MEM_WIDTH), D_MODEL ** -0.5),
        "mem_q_norm": gain(ks[23], (DEPTH, MEM_HD)),
        "mem_k_norm": gain(ks[24], (DEPTH, MEM_HD)),
        "wo_mem": nrm(ks[25], (DEPTH, MEM_WIDTH, D_MODEL), MEM_WIDTH ** -0.5),
        "norm_ffn": gain(ks[26], (DEPTH, D_MODEL)),
        "w_router": nrm(ks[27], (DEPTH, D_MODEL, N_EXPERTS), D_MODEL ** -0.5),
        "w_gate_up": nrm(ks[28], (DEPTH, N_EXPERTS, D_MODEL, 2 * D_FF), D_MODEL ** -0.5),
        "w_down": nrm(ks[29], (DEPTH, N_EXPERTS, D_FF, D_MODEL), D_FF ** -0.5),
    }


def reference(x_prompt, x_sample, mem_prompt, mem_sample, norm_mix, w_in, na_q_norm, na_k_norm,
              na_rel_bias, w_na_br, hg_lb, hg_out_norm, w_hg_br, conv_w, conv_b, conv_ln_g, conv_ln_b,
              w_cv_br, w_out, norm_mem, mem_kv_norm, wq_mem, wkv_mem, mem_q_norm, mem_k_norm, wo_mem,
              norm_ffn, w_router, w_gate_up, w_down):
    params = (norm_mix, w_in, na_q_norm, na_k_norm, na_rel_bias, w_na_br, hg_lb, hg_out_norm, w_hg_br,
              conv_w, conv_b, conv_ln_g, conv_ln_b, w_cv_br, w_out, norm_mem, mem_kv_norm, wq_mem,
              wkv_mem, mem_q_norm, mem_k_norm, wo_mem, norm_ffn, w_router, w_gate_up, w_down)
    y_prompt = _trunk(x_prompt, mem_prompt, *params)
    y_sample = _trunk(x_sample, mem_sample, *params)
    return (y_prompt, y_sample)
```

```python
import os
import numpy as np
import concourse.bass as bass
import concourse.mybir as mybir
from concourse.bass_utils import run_bass_kernel_spmd
from contextlib import ExitStack

F32 = mybir.dt.float32
BF16 = mybir.dt.bfloat16
U32 = mybir.dt.uint32
AF = mybir.ActivationFunctionType
ALU = mybir.AluOpType
AX = mybir.AxisListType

T = 8192
ST = 512
NST = T // ST
NT = T // 128
D = 1024
DIN = 8192
DEPTH = 2
NE = 16
DFF = 2752
CAPK = 4096
EPS = 1e-6
NEG = -30000.0
NBIS = 26
NCH = 4
GT = NCH * T
ROW = 1060
BIG = 1.0e6


class Buf:
    __slots__ = ("w", "r")

    def __init__(self):
        self.w = None
        self.r = {}


class TT:
    __slots__ = ("t", "b")

    def __init__(self, t):
        self.t = t
        self.b = Buf()


class KB:
    def __init__(self, nc, es):
        self.nc = nc
        self.E = {"pe": nc.tensor, "dve": nc.vector, "act": nc.scalar, "pool": nc.gpsimd, "sp": nc.sync}
        self.semh = {}
        self.cnt = {}
        for e in ("pe", "dve", "act", "pool"):
            self.semh[e] = es.enter_context(nc.semaphore("s_" + e))
            self.cnt[e] = 0
        self.nd = 26
        self.qsems = {"sp": list(range(0, 16)), "pool": list(range(16, 26))}
        self.qnext = {"sp": 0, "pool": 0}
        self.dcnt = [0] * self.nd
        for i in range(self.nd):
            self.semh[("d", i)] = es.enter_context(nc.semaphore("d%d" % i))
        self.seen = {e: {} for e in self.E}
        self.ninst = 0
        self.bregs = {}

    def _deps(self, reads, writes):
        deps = {}
        for b in reads:
            if b.w is not None and deps.get(b.w[0], 0) < b.w[1]:
                deps[b.w[0]] = b.w[1]
        for b in writes:
            if b.w is not None and deps.get(b.w[0], 0) < b.w[1]:
                deps[b.w[0]] = b.w[1]
            for k, v in b.r.items():
                if deps.get(k, 0) < v:
                    deps[k] = v
        return deps

    def _wait(self, eng, deps):
        E = self.E[eng]
        seen = self.seen[eng]
        for k, v in deps.items():
            if eng == "pe" and k == "pe":
                continue
            if seen.get(k, 0) >= v:
                continue
            E.wait_ge(self.semh[k], v)
            seen[k] = v

    def op(self, eng, fn, reads=(), writes=()):
        reads = [x.b if isinstance(x, TT) else x for x in reads]
        writes = [x.b if isinstance(x, TT) else x for x in writes]
        self._wait(eng, self._deps(reads, writes))
        inst = fn(self.E[eng])
        self.cnt[eng] += 1
        c = self.cnt[eng]
        inst.then_inc(self.semh[eng], 1)
        for b in reads:
            b.r[eng] = c
        for b in writes:
            b.w = (eng, c)
            b.r = {}
        self.ninst += 1
        return inst

    def dma(self, q, out, in_, reads=(), writes=(), **kw):
        reads = [x.b if isinstance(x, TT) else x for x in reads]
        writes = [x.b if isinstance(x, TT) else x for x in writes]
        i = self.next_dsem(q)
        key = ("d", i)
        deps = self._deps(reads, writes)
        if self.dcnt[i] > 0 and deps.get(key, 0) < self.dcnt[i]:
            deps[key] = self.dcnt[i]
        self._wait(q, deps)
        inst = self.E[q].dma_start(out=out, in_=in_, **kw)
        self.dcnt[i] += 16
        inst.then_inc(self.semh[key], 16)
        for b in reads:
            b.r[key] = self.dcnt[i]
        for b in writes:
            b.w = (key, self.dcnt[i])
            b.r = {}
        self.ninst += 1
        return inst

    def idma(self, reads=(), writes=(), **kw):
        reads = [x.b if isinstance(x, TT) else x for x in reads]
        writes = [x.b if isinstance(x, TT) else x for x in writes]
        i = self.next_dsem("pool")
        key = ("d", i)
        deps = self._deps(reads, writes)
        if self.dcnt[i] > 0 and deps.get(key, 0) < self.dcnt[i]:
            deps[key] = self.dcnt[i]
        self._wait("pool", deps)
        bc = kw.pop("bounds_check")
        if bc not in self.bregs:
            self.bregs[bc] = self.nc.gpsimd.to_reg(bc)
        inst = self.nc.gpsimd.indirect_dma_start(bounds_check=self.bregs[bc], **kw)
        self.dcnt[i] += 16
        inst.then_inc(self.semh[key], 16)
        for b in reads:
            b.r[key] = self.dcnt[i]
        for b in writes:
            b.w = (key, self.dcnt[i])
            b.r = {}
        self.ninst += 1
        return inst

    def next_dsem(self, q):
        lst = self.qsems[q]
        i = lst[self.qnext[q] % len(lst)]
        self.qnext[q] += 1
        return i

    def barrier(self):
        allk = {e: self.cnt[e] for e in self.cnt if self.cnt[e] > 0}
        for i in range(self.nd):
            if self.dcnt[i] > 0:
                allk[("d", i)] = self.dcnt[i]
        for eng in self.E:
            self._wait(eng, {k: v for k, v in allk.items() if k != eng})


class Ctx:
    pass


_UID = [0]


def _alloc(es, nc, name, shape, dt, psum=False):
    f = nc.psum_tensor if psum else nc.sbuf_tensor
    _UID[0] += 1
    return TT(es.enter_context(f("t%d_%s" % (_UID[0], name), shape, dt)))


def build_program():
    nc = bass.Bass("TRN2", target_bir_lowering=False)
    g = Ctx()
    g.nc = nc

    debug = os.environ.get("KDEBUG", "").split(",")

    def dscr(name, shape, dt):
        dbg = ("1" in debug) or (name in debug)
        return nc.dram_tensor(name, shape, dt, kind=("ExternalOutput" if dbg else "Internal")).ap()

    ishapes = {
        "x": [GT, D], "mem": [NCH, 2, 256, D], "tokid": [128, 256], "zrows": [CAPK, ROW], "sut": [128, 128], "cflag": [128, 1], "smask": [128, 128], "bt": [DEPTH, 128, 4 * 31 * 64],
        "ident": [128, 128], "ones": [128, 128], "bones": [128, 128], "trif": [128, 128], "trib": [128, 128],
        "norm_mix": [DEPTH, D], "w_in": [DEPTH, D, DIN], "na_q_norm": [DEPTH, 64], "na_k_norm": [DEPTH, 64],
        "w_na_br": [DEPTH, 512, D], "hg_lb": [2, DEPTH, 512], "hg_out_norm": [DEPTH, 128],
        "w_hg_br": [DEPTH, 512, D], "conv_w": [DEPTH, 31, 512], "conv_b": [DEPTH, 512],
        "conv_ln_g": [DEPTH, 512], "conv_ln_b": [DEPTH, 512], "w_cv_br": [DEPTH, 512, D],
        "w_out": [DEPTH, D, D], "norm_mem": [DEPTH, D], "mem_kv_norm": [DEPTH, D], "wq_mem": [DEPTH, D, 512],
        "wkv_mem": [DEPTH, D, D], "mem_q_norm": [DEPTH, 128], "mem_k_norm": [DEPTH, 128],
        "wo_mem": [DEPTH, 512, D], "norm_ffn": [DEPTH, D], "w_router": [DEPTH, D, NE],
        "w_gate_up": [DEPTH, NE, D, 2 * DFF], "w_down": [DEPTH, NE, DFF, D],
    }

    class LazyIn(dict):
        def __missing__(self, k):
            ap = nc.dram_tensor(k, ishapes[k], F32, kind="ExternalInput").ap()
            self[k] = ap
            return ap

    I = LazyIn()
    y = nc.dram_tensor("y", [GT, D], F32, kind="ExternalOutput").ap()
    S = {}
    S["qT"] = dscr("qT_d", [512, T], BF16)
    S["kT"] = dscr("kT_d", [512, T], BF16)
    S["v"] = dscr("v_d", [T, 512], BF16)
    S["qh"] = dscr("qh_d", [512, T], BF16)
    S["lf"] = dscr("lf_d", [2, 512, T], F32)
    S["kk"] = dscr("kk_d", [2, 512, T], BF16)
    S["vh"] = dscr("vh_d", [T, 512], BF16)
    S["gs"] = dscr("gs_d", [512, T], BF16)
    S["u"] = dscr("u_d", [512, T], F32)
    S["sg"] = dscr("sg_d", [3072, T], BF16)
    S["naT"] = dscr("naT_d", [512, T], BF16)
    S["of"] = dscr("of_d", [512, T], F32)
    S["hgT"] = dscr("hgT_d", [512, T], BF16)
    S["cvT"] = dscr("cvT_d", [512, T], BF16)
    S["h2T"] = dscr("h2T_d", [D, T], BF16)
    S["affall"] = dscr("affall_d", [GT, NE], F32)
    S["h2r"] = dscr("h2r_d", [GT, D], BF16)
    g.G = [nc.dram_tensor("G%d" % e, [CAPK, ROW], BF16, kind="Internal").ap() for e in range(NE)]
    g.Gbuf = [Buf() for _ in range(NE)]
    g.ybuf = Buf()
    g.I, g.S, g.y = I, S, y
    nc._k_inputs = I
    nc._k_scratch = S
    g.DBall = {k: Buf() for k in ["affall", "h2r"]}

    g.lim = int(os.environ.get("KLIM", "0"))
    with ExitStack() as es:
        kb = KB(nc, es)
        g.kb = kb
        g.ident_bf = _alloc(es, nc, "ident_bf", [128, 128], BF16)
        g.ident_f = _alloc(es, nc, "ident_f", [128, 128], F32)
        g.ones_f = _alloc(es, nc, "ones_f", [128, 128], F32)
        g.ones_bf = _alloc(es, nc, "ones_bf", [128, 128], BF16)
        g.bones_f = _alloc(es, nc, "bones_f", [128, 128], F32)
        g.trif = _alloc(es, nc, "trif", [128, 128], F32)
        g.trib = _alloc(es, nc, "trib", [128, 128], F32)
        g.cflag = _alloc(es, nc, "cflag_sb", [128, 1], F32)
        g.wg = _alloc(es, nc, "wg_sb", [128, NT, NE], F32)
        kb.dma("pool", g.ident_bf.t[:], I["ident"][:, :], writes=[g.ident_bf])
        kb.dma("sp", g.ident_f.t[:], I["ident"][:, :], writes=[g.ident_f])
        kb.dma("sp", g.ones_f.t[:], I["ones"][:, :], writes=[g.ones_f])
        kb.dma("pool", g.ones_bf.t[:], I["ones"][:, :], writes=[g.ones_bf])
        kb.dma("sp", g.bones_f.t[:], I["bones"][:, :], writes=[g.bones_f])
        kb.dma("sp", g.trif.t[:], I["trif"][:, :], writes=[g.trif])
        kb.dma("sp", g.trib.t[:], I["trib"][:, :], writes=[g.trib])
        kb.dma("sp", g.cflag.t[:], I["cflag"][:, :], writes=[g.cflag])

        only = os.environ.get("KONLY", "")
        only = set(only.split(",")) if only else None
        nlayers = int(os.environ.get("KLAYERS", DEPTH))
        nch = int(os.environ.get("KCHUNKS", NCH))
        for l in range(nlayers):
            for k in range(nch):
                g.DB = {kk: [Buf() for _ in range(NST)] for kk in list(S.keys()) + ["y", "aff"]}
                xsrc = (I["x"] if l == 0 else y)[k * T:(k + 1) * T, :]
                g.yc = y[k * T:(k + 1) * T, :]
                g.memc = I["mem"][k]
                g.affc = S["affall"][k * T:(k + 1) * T, :]
                g.h2rc = S["h2r"][k * T:(k + 1) * T, :]
                plan = [("A0", lambda: stage_A(g, l, xsrc, 0)), ("A1", lambda: stage_A(g, l, xsrc, 1)), ("B", lambda: stage_B(g, l)),
                        ("C", lambda: stage_C(g, l)), ("D", lambda: stage_D(g, l)), ("E", lambda: stage_E(g, l, xsrc)),
                        ("F", lambda: stage_F(g, l))]
                for nm, fn in plan:
                    if only is not None and nm not in only:
                        continue
                    fn()
                    kb.barrier()
                print("stage", l, k, "A-F ninst", kb.ninst, flush=True)
            for nm, fn in (("H", lambda: stage_H2(g, l)), ("I", lambda: stage_I2(g, l))):
                if only is not None and nm not in only:
                    continue
                fn()
                kb.barrier()
                print("stage", l, nm, "ninst", kb.ninst, flush=True)
        kb.barrier()
    return nc


def load_bcast_row(g, es, name, src_row, n):
    t = _alloc(es, g.nc, name, [128, n], F32)
    g.kb.dma("sp", t.t[:], src_row.partition_broadcast(128), writes=[t])
    return t


def load_col(g, es, name, src_vec, nchunks):
    t = _alloc(es, g.nc, name, [128, nchunks], F32)
    with g.nc.allow_non_contiguous_dma(reason="tiny param load"):
        g.kb.dma("sp", t.t[:], src_vec.rearrange("(c p) -> p c", p=128), writes=[t])
    return t


def rms_rows(g, xa, ss, rstd, junk, ntb):
    kb = g.kb
    for tb in range(ntb):
        kb.op("act", lambda e, tb=tb: e.activation(out=junk.t[:], in_=xa[tb].t[:], func=AF.Square,
                                                  accum_out=ss.t[:, tb:tb + 1]),
              reads=[xa[tb]], writes=[junk, ss])
    kb.op("act", lambda e: e.activation(out=rstd.t[:, 0:ntb], in_=ss.t[:, 0:ntb], func=AF.Ln, scale=1.0 / D, bias=EPS),
          reads=[ss], writes=[rstd])
    kb.op("act", lambda e: e.activation(out=rstd.t[:, 0:ntb], in_=rstd.t[:, 0:ntb], func=AF.Exp, scale=-0.5),
          reads=[rstd], writes=[rstd])


def norm_transpose(g, src, st, gbc, W, out_hT, also_store=None):
    kb = g.kb
    t0 = st * ST
    for tb in range(4):
        kb.dma("sp", W.xa[tb].t[:], src[t0 + tb * 128:t0 + (tb + 1) * 128, :],
               reads=[g.DB["y"][st]], writes=[W.xa[tb]])
    rms_rows(g, W.xa, W.ss, W.rstd, W.junk, 4)
    for tb in range(4):
        kb.op("dve", lambda e, tb=tb: e.scalar_tensor_tensor(out=W.hb[tb].t[:], in0=W.xa[tb].t[:],
                                                            scalar=W.rstd.t[:, tb:tb + 1], in1=gbc.t[:],
                                                            op0=ALU.mult, op1=ALU.mult),
              reads=[W.xa[tb], W.rstd, gbc], writes=[W.hb[tb]])
        for kc in range(8):
            kb.op("pe", lambda e, tb=tb, kc=kc: e.transpose(out=W.ptr.t[:, kc, :], in_=W.hb[tb].t[:, kc * 128:(kc + 1) * 128],
                                                           identity=g.ident_bf.t[:]),
                  reads=[W.hb[tb], g.ident_bf], writes=[W.ptr])
        kb.op("act", lambda e, tb=tb: e.copy(out=out_hT.t[:, :, tb * 128:(tb + 1) * 128], in_=W.ptr.t[:]),
              reads=[W.ptr], writes=[out_hT])


class NormWork:
    def __init__(self, g, es, pfx):
        nc = g.nc
        self.xa = [_alloc(es, nc, pfx + "xa%d" % i, [128, D], F32) for i in range(4)]
        self.hb = [_alloc(es, nc, pfx + "hb%d" % i, [128, D], BF16) for i in range(2)] * 2
        self.ss = _alloc(es, nc, pfx + "ss", [128, 4], F32)
        self.rstd = _alloc(es, nc, pfx + "rstd", [128, 4], F32)
        self.junk = _alloc(es, nc, pfx + "junk", [128, D], BF16)
        self.ptr = _alloc(es, nc, pfx + "ptr", [128, 8, 128], BF16, psum=True)


def stage_A(g, l, xsrc, half):
    nc, kb, I, S = g.nc, g.kb, g.I, g.S
    with ExitStack() as es:
        W = NormWork(g, es, "A")
        gbc = load_bcast_row(g, es, "A_gbc", I["norm_mix"][l, :], D)
        hT = _alloc(es, nc, "A_hT", [128, 8, ST], BF16)
        wsb = _alloc(es, nc, "A_w", [128, 8, 4096], BF16)
        for kc in range(8):
            kb.dma("pool", wsb.t[:, kc, :], I["w_in"][l, kc * 128:(kc + 1) * 128, half * 4096:(half + 1) * 4096],
                   writes=[wsb])
        ps = [_alloc(es, nc, "A_ps%d" % i, [128, ST], F32, psum=True) for i in range(4)]
        ps2 = [_alloc(es, nc, "A_ps2%d" % i, [128, ST], F32, psum=True) for i in range(2)]
        f32t = [_alloc(es, nc, "A_f%d" % i, [128, ST], F32) for i in range(6)]
        bft = [_alloc(es, nc, "A_b%d" % i, [128, ST], BF16) for i in range(6)]
        cnt = {"ps": 0, "ps2": 0, "f": 0, "b": 0}

        def nxt(lst, key):
            r = lst[cnt[key] % len(lst)]
            cnt[key] += 1
            return r

        if half == 0:
            qg = _alloc(es, nc, "A_qg", [128, 2], F32)
            with nc.allow_non_contiguous_dma(reason="tiny"):
                for hh in range(2):
                    kb.dma("sp", qg.t[hh * 64:(hh + 1) * 64, 0:1], I["na_q_norm"][l, :].rearrange("(p o) -> p o", o=1), writes=[qg])
                    kb.dma("sp", qg.t[hh * 64:(hh + 1) * 64, 1:2], I["na_k_norm"][l, :].rearrange("(p o) -> p o", o=1), writes=[qg])
            kb.op("dve", lambda e: e.tensor_scalar(out=qg.t[:, 0:1], in0=qg.t[:, 0:1], scalar1=0.125, scalar2=None, op0=ALU.mult),
                  reads=[qg], writes=[qg])
            lbv = _alloc(es, nc, "A_lb", [128, 8], F32)
            oml = _alloc(es, nc, "A_oml", [128, 8], F32)
            noml = _alloc(es, nc, "A_noml", [128, 8], F32)
            if l == 0:
                kb.op("dve", lambda e: e.memset(lbv.t[:], 0.0), writes=[lbv])
            else:
                b0 = _alloc(es, nc, "A_b0", [128, 8], F32)
                b1 = _alloc(es, nc, "A_b1", [128, 8], F32)
                with nc.allow_non_contiguous_dma(reason="tiny"):
                    for d_ in range(2):
                        kb.dma("sp", b0.t[:, d_ * 4:(d_ + 1) * 4], I["hg_lb"][d_, 0, :].rearrange("(c p) -> p c", p=128), writes=[b0])
                        kb.dma("sp", b1.t[:, d_ * 4:(d_ + 1) * 4], I["hg_lb"][d_, 1, :].rearrange("(c p) -> p c", p=128), writes=[b1])
                kb.op("dve", lambda e: e.tensor_tensor(out=b1.t[:], in0=b1.t[:], in1=b0.t[:], op=ALU.subtract), reads=[b0, b1], writes=[b1])
                kb.op("act", lambda e: e.activation(out=lbv.t[:], in_=b1.t[:], func=AF.Sigmoid), reads=[b1], writes=[lbv])
            kb.op("dve", lambda e: e.tensor_scalar(out=oml.t[:], in0=lbv.t[:], scalar1=-1.0, scalar2=1.0, op0=ALU.mult, op1=ALU.add),
                  reads=[lbv], writes=[oml])
            kb.op("dve", lambda e: e.tensor_scalar(out=noml.t[:], in0=oml.t[:], scalar1=-1.0, scalar2=None, op0=ALU.mult),
                  reads=[oml], writes=[noml])

        def proj_fm(cb):
            p = nxt(ps, "ps")
            for kc in range(8):
                kb.op("pe", lambda e, kc=kc: e.matmul(p.t[:], wsb.t[:, kc, cb * 128:(cb + 1) * 128], hT.t[:, kc, :],
                                                     start=(kc == 0), stop=(kc == 7)),
                      reads=[wsb, hT], writes=[p])
            return p

        def store(q, dst, src_tt, dbufs):
            kb.dma(q, dst, src_tt.t[:], reads=[src_tt], writes=dbufs)

        for st in range(NST):
            t0 = st * ST
            norm_transpose(g, xsrc, st, gbc, W, hT)
            if half == 0:
                for cb in range(8):
                    p = proj_fm(cb)
                    sq = nxt(f32t, "f")
                    kb.op("act", lambda e: e.activation(out=sq.t[:], in_=p.t[:], func=AF.Square), reads=[p], writes=[sq])
                    p2 = nxt(ps2, "ps2")
                    kb.op("pe", lambda e: e.matmul(p2.t[:], g.bones_f.t[:], sq.t[:], start=True, stop=True),
                          reads=[g.bones_f, sq], writes=[p2])
                    rs = nxt(f32t, "f")
                    kb.op("act", lambda e: e.activation(out=rs.t[:], in_=p2.t[:], func=AF.Ln, scale=1.0 / 64, bias=EPS),
                          reads=[p2], writes=[rs])
                    kb.op("act", lambda e: e.activation(out=rs.t[:], in_=rs.t[:], func=AF.Exp, scale=-0.5), reads=[rs], writes=[rs])
                    ob = nxt(bft, "b")
                    gi = 0 if cb < 4 else 1
                    kb.op("dve", lambda e: e.scalar_tensor_tensor(out=ob.t[:], in0=p.t[:], scalar=qg.t[:, gi:gi + 1], in1=rs.t[:],
                                                                 op0=ALU.mult, op1=ALU.mult),
                          reads=[p, qg, rs], writes=[ob])
                    dn = "qT" if cb < 4 else "kT"
                    c = cb % 4
                    store("sp", S[dn][c * 128:(c + 1) * 128, t0:t0 + ST], ob, [g.DB[dn][st]])
                for (c0, dn) in ((1024, "v"), (3072, "vh")):
                    for tb in range(4):
                        p = nxt(ps, "ps")
                        for kc in range(8):
                            kb.op("pe", lambda e, kc=kc: e.matmul(p.t[:], hT.t[:, kc, tb * 128:(tb + 1) * 128],
                                                                 wsb.t[:, kc, c0:c0 + 512], start=(kc == 0), stop=(kc == 7)),
                                  reads=[wsb, hT], writes=[p])
                        ob = nxt(bft, "b")
                        kb.op("act", lambda e: e.copy(out=ob.t[:], in_=p.t[:]), reads=[p], writes=[ob])
                        store("sp", S[dn][t0 + tb * 128:t0 + (tb + 1) * 128, :], ob, [g.DB[dn][st]])
                for cb in list(range(12, 16)) + list(range(28, 32)):
                    p = proj_fm(cb)
                    sgm = nxt(f32t, "f")
                    kb.op("act", lambda e: e.activation(out=sgm.t[:], in_=p.t[:], func=AF.Sigmoid), reads=[p], writes=[sgm])
                    ob = nxt(bft, "b")
                    sc = (128.0 ** -0.5) if cb < 16 else 1.0
                    kb.op("dve", lambda e: e.scalar_tensor_tensor(out=ob.t[:], in0=p.t[:], scalar=sc, in1=sgm.t[:],
                                                                 op0=ALU.mult, op1=ALU.mult),
                          reads=[p, sgm], writes=[ob])
                    dn = "qh" if cb < 16 else "gs"
                    c = cb % 4
                    store("sp", S[dn][c * 128:(c + 1) * 128, t0:t0 + ST], ob, [g.DB[dn][st]])
                for cb in range(16, 24):
                    d_ = (cb - 16) // 4
                    h = cb % 4
                    j = d_ * 4 + h
                    p = proj_fm(cb)
                    sgm = nxt(f32t, "f")
                    kb.op("act", lambda e: e.activation(out=sgm.t[:], in_=p.t[:], func=AF.Sigmoid), reads=[p], writes=[sgm])
                    ff = nxt(f32t, "f")
                    kb.op("dve", lambda e: e.tensor_scalar(out=ff.t[:], in0=sgm.t[:], scalar1=oml.t[:, j:j + 1], scalar2=lbv.t[:, j:j + 1],
                                                          op0=ALU.mult, op1=ALU.add), reads=[sgm, oml, lbv], writes=[ff])
                    kb.op("pool", lambda e: e.tensor_scalar(out=ff.t[:], in0=ff.t[:], scalar1=1e-20, scalar2=None, op0=ALU.max), reads=[ff], writes=[ff])
                    kb.op("act", lambda e: e.activation(out=ff.t[:], in_=ff.t[:], func=AF.Ln), reads=[ff], writes=[ff])
                    store("sp", S["lf"][d_, h * 128:(h + 1) * 128, t0:t0 + ST], ff, [g.DB["lf"][st]])
                    ob = nxt(bft, "b")
                    kb.op("dve", lambda e: e.tensor_scalar(out=ob.t[:], in0=sgm.t[:], scalar1=noml.t[:, j:j + 1], scalar2=oml.t[:, j:j + 1],
                                                          op0=ALU.mult, op1=ALU.add), reads=[sgm, noml, oml], writes=[ob])
                    store("sp", S["kk"][d_, h * 128:(h + 1) * 128, t0:t0 + ST], ob, [g.DB["kk"][st]])
            else:
                for c in range(4):
                    pb = proj_fm(4 + c)
                    sgm = nxt(f32t, "f")
                    kb.op("act", lambda e: e.activation(out=sgm.t[:], in_=pb.t[:], func=AF.Sigmoid), reads=[pb], writes=[sgm])
                    pa = proj_fm(c)
                    uu = nxt(f32t, "f")
                    kb.op("dve", lambda e: e.tensor_tensor(out=uu.t[:], in0=pa.t[:], in1=sgm.t[:], op=ALU.mult), reads=[pa, sgm], writes=[uu])
                    store("sp", S["u"][c * 128:(c + 1) * 128, t0:t0 + ST], uu, [g.DB["u"][st]])
                for cb in range(8, 32):
                    p = proj_fm(cb)
                    ob = nxt(bft, "b")
                    kb.op("act", lambda e: e.activation(out=ob.t[:], in_=p.t[:], func=AF.Sigmoid), reads=[p], writes=[ob])
                    store("sp", S["sg"][(cb - 8) * 128:(cb - 7) * 128, t0:t0 + ST], ob, [g.DB["sg"][st]])


def na_window(r, typ):
    if typ == 0:
        base = (r // 64) * 64
        r0 = min(max((r % 64) - 4, 0), 56) + base
    else:
        r0 = min(max(r - 4, 0), 120)
    return r0


def stage_B(g, l):
    nc, kb, I, S = g.nc, g.kb, g.I, g.S
    NR = 24
    with ExitStack() as es:
        bt = _alloc(es, nc, "B_bt", [128, 4, 31, 64], F32)
        kb.dma("sp", bt.t[:], I["bt"][l].rearrange("p (c e q) -> p c e q", c=4, e=31), writes=[bt])
        smask = _alloc(es, nc, "B_sm", [128, 128], F32)
        kb.dma("sp", smask.t[:], I["smask"][:, :], writes=[smask])
        kbd = [_alloc(es, nc, "B_k%d" % i, [128, 4, 128], BF16) for i in range(NR)]
        vbd = [_alloc(es, nc, "B_v%d" % i, [128, 4, 130], BF16) for i in range(NR)]
        qr = [_alloc(es, nc, "B_q%d" % i, [128, 4, 64], BF16) for i in range(6)]
        for i in range(NR):
            kb.op("pool", lambda e, i=i: e.memset(kbd[i].t[:], 0.0), writes=[kbd[i]])
            kb.op("pool", lambda e, i=i: e.memset(vbd[i].t[:], 0.0), writes=[vbd[i]])
            kb.op("pool", lambda e, i=i: e.memset(vbd[i].t[0:64, :, 64:65], 1.0), writes=[vbd[i]])
            kb.op("pool", lambda e, i=i: e.memset(vbd[i].t[64:128, :, 129:130], 1.0), writes=[vbd[i]])
        pss = [_alloc(es, nc, "B_ps%d" % i, [128, 8, 64], F32, psum=True) for i in range(3)]
        pos = [_alloc(es, nc, "B_po%d" % i, [128, 512], F32, psum=True) for i in range(4)]
        ptt = _alloc(es, nc, "B_pt", [128, 4, 64], BF16, psum=True)
        sct = [_alloc(es, nc, "B_sc%d" % i, [128, 8, 64], F32) for i in range(3)]
        ptl = [_alloc(es, nc, "B_pT%d" % i, [128, 8, 64], BF16) for i in range(8)]
        rcp = [_alloc(es, nc, "B_rc%d" % i, [64, 4], F32) for i in range(4)]
        obf = [_alloc(es, nc, "B_ob%d" % i, [64, 512], BF16) for i in range(2)]
        outb = [_alloc(es, nc, "B_out%d" % i, [128, 4, ST], BF16) for i in range(2)]
        kTv = S["kT"].rearrange("(c p) t -> p c t", p=128)
        qTv = S["qT"].rearrange("(c p) t -> p c t", p=128)
        vv = S["v"].rearrange("t (c x) -> t c x", x=128)
        loaded = [-1]
        cn = {"ps": 0, "sc": 0, "pT": 0, "po": 0}

        def load_row(rr):
            sl = rr % NR
            st = (rr * 64) // ST
            cs = slice(rr * 64, rr * 64 + 64)
            with nc.allow_non_contiguous_dma(reason="row blocks"):
                kb.dma("sp", kbd[sl].t[0:64, :, 0:64], kTv[0:64, :, cs], reads=[g.DB["kT"][st]], writes=[kbd[sl]])
                kb.dma("sp", kbd[sl].t[64:128, :, 64:128], kTv[64:128, :, cs], reads=[g.DB["kT"][st]], writes=[kbd[sl]])
                kb.dma("pool", vbd[sl].t[0:64, :, 0:64], vv[cs, :, 0:64], reads=[g.DB["v"][st]], writes=[vbd[sl]])
                kb.dma("pool", vbd[sl].t[64:128, :, 65:129], vv[cs, :, 64:128], reads=[g.DB["v"][st]], writes=[vbd[sl]])

        for r in range(g.lim if g.lim else 128):
            special = 60 <= r <= 67
            if special:
                krows = list(range(56, 72))
            else:
                r0 = na_window(r, 1)
                krows = list(range(r0, r0 + 8))
            while loaded[0] < krows[-1]:
                loaded[0] += 1
                load_row(loaded[0])
            q = qr[r % len(qr)]
            with nc.allow_non_contiguous_dma(reason="row blocks"):
                kb.dma("sp", q.t[:], qTv[:, :, r * 64:(r + 1) * 64], reads=[g.DB["qT"][(r * 64) // ST]], writes=[q])
            groups = [krows[i:i + 8] for i in range(0, len(krows), 8)]
            pot = [pos[cn["po"] % 4], pos[(cn["po"] + 1) % 4]]
            cn["po"] += 2
            for c in range(4):
                pts = []
                for gi, grp in enumerate(groups):
                    p = pss[cn["ps"] % 3]
                    cn["ps"] += 1
                    for j, rr in enumerate(grp):
                        kb.op("pe", lambda e, j=j, rr=rr: e.matmul(p.t[:, j, :], kbd[rr % NR].t[:, c, :], q.t[:, c, :], start=True, stop=True),
                              reads=[kbd[rr % NR], q], writes=[p])
                    sc = sct[cn["sc"] % 3]
                    cn["sc"] += 1
                    e0 = (grp[0] - r + 7) + 8
                    kb.op("dve", lambda e: e.tensor_tensor(out=sc.t[:], in0=p.t[:], in1=bt.t[:, c, e0:e0 + 8, :], op=ALU.add),
                          reads=[p, bt], writes=[sc])
                    if special:
                        a = (r - 60) * 16 + gi * 8
                        kb.op("dve", lambda e: e.tensor_tensor(out=sc.t[:], in0=sc.t[:],
                                                              in1=smask.t[:, a:a + 8].unsqueeze(2).broadcast_to([128, 8, 64]), op=ALU.add),
                              reads=[sc, smask], writes=[sc])
                    pT = ptl[cn["pT"] % 8]
                    cn["pT"] += 1
                    kb.op("act", lambda e: e.activation(out=pT.t[:], in_=sc.t[:], func=AF.Exp), reads=[sc], writes=[pT])
                    pts.append((pT, grp))
                po = pot[c // 2]
                n_mm = sum(len(grp) for _, grp in pts)
                i_mm = 0
                for pT, grp in pts:
                    for j, rr in enumerate(grp):
                        kb.op("pe", lambda e, j=j, rr=rr, i_mm=i_mm: e.matmul(po.t[0:64, (c % 2) * 130:(c % 2) * 130 + 130], pT.t[:, j, :], vbd[rr % NR].t[:, c, :],
                                                                             start=(i_mm == 0), stop=(i_mm == n_mm - 1)),
                              reads=[pT, vbd[rr % NR]], writes=[po])
                        i_mm += 1
            rc = rcp[r % 4]
            ob = obf[r % 2]
            for hf in range(2):
                pv = pot[hf].t[0:64, 0:260].rearrange("p (a x) -> p a x", x=65)
                kb.op("dve", lambda e: e.reciprocal(out=rc.t[:, :].unsqueeze(2), in_=pv[:, :, 64:65]), reads=[pot[hf]], writes=[rc])
                kb.op("dve", lambda e: e.tensor_tensor(out=ob.t[:, hf * 256:(hf + 1) * 256].rearrange("p (a x) -> p a x", x=64),
                                                      in0=pv[:, :, 0:64], in1=rc.t[:, :].unsqueeze(2).broadcast_to([64, 4, 64]), op=ALU.mult),
                      reads=[pot[hf], rc], writes=[ob])
            for fc in range(4):
                kb.op("pe", lambda e, fc=fc: e.transpose(out=ptt.t[:, fc, :], in_=ob.t[:, fc * 128:(fc + 1) * 128], identity=g.ident_bf.t[0:64, 0:64]),
                      reads=[ob, g.ident_bf], writes=[ptt])
            st = (r * 64) // ST
            o = outb[st % 2]
            off = (r * 64) % ST
            kb.op("act", lambda e: e.copy(out=o.t[:, :, off:off + 64], in_=ptt.t[:]), reads=[ptt], writes=[o])
            if off + 64 == ST:
                kb.dma("pool", S["naT"].rearrange("(c p) t -> p c t", p=128)[:, :, st * ST:(st + 1) * ST], o.t[:],
                       reads=[o], writes=[g.DB["naT"][st]])


def stage_C(g, l):
    nc, kb, I, S = g.nc, g.kb, g.I, g.S
    with ExitStack() as es:
        og = load_col(g, es, "C_og", I["hg_out_norm"][l, :], 1)
        NB_ = 3
        lf = [_alloc(es, nc, "C_lf%d" % i, [128, 4, 128], F32) for i in range(NB_)]
        kk = [_alloc(es, nc, "C_kk%d" % i, [128, 4, 128], BF16) for i in range(NB_)]
        qh = [_alloc(es, nc, "C_qh%d" % i, [128, 4, 128], BF16) for i in range(NB_)]
        vh = [_alloc(es, nc, "C_vh%d" % i, [128, 512], BF16) for i in range(NB_)]
        gsb = [_alloc(es, nc, "C_gs%d" % i, [128, 4, 128], BF16) for i in range(NB_)]
        ofb = [_alloc(es, nc, "C_of%d" % i, [128, 4, 128], F32) for i in range(NB_)]
        cb_ = [_alloc(es, nc, "C_c%d" % i, [128, 132], F32) for i in range(4)]
        cm = [_alloc(es, nc, "C_cm%d" % i, [128, 128], F32) for i in range(4)]
        e1 = [_alloc(es, nc, "C_e1%d" % i, [128, 128], F32) for i in range(4)]
        e2 = [_alloc(es, nc, "C_e2%d" % i, [128, 128], F32) for i in range(4)]
        qt = [_alloc(es, nc, "C_qt%d" % i, [128, 128], BF16) for i in range(4)]
        kt = [_alloc(es, nc, "C_kt%d" % i, [128, 128], BF16) for i in range(4)]
        ktok = [_alloc(es, nc, "C_ktok%d" % i, [128, 128], BF16) for i in range(4)]
        attm = [_alloc(es, nc, "C_att%d" % i, [128, 128], BF16) for i in range(4)]
        t6 = [_alloc(es, nc, "C_t6%d" % i, [128, 2, 3], F32) for i in range(4)]
        x6 = [_alloc(es, nc, "C_x6%d" % i, [128, 2, 3], F32) for i in range(4)]
        bs = [_alloc(es, nc, "C_bs%d" % i, [128, 2], F32) for i in range(4)]
        Sst = [_alloc(es, nc, "C_S%d" % i, [128, 128], F32) for i in range(4)]
        Smid = [_alloc(es, nc, "C_Sm%d" % i, [128, 128], BF16) for i in range(8)]
        tmp = [_alloc(es, nc, "C_tmp%d" % i, [128, 128], F32) for i in range(4)]
        osb = [_alloc(es, nc, "C_o%d" % i, [128, 4, 128], F32) for i in range(2)]
        sqb = [_alloc(es, nc, "C_sq%d" % i, [128, 128], F32) for i in range(2)]
        rsb = [_alloc(es, nc, "C_rs%d" % i, [128, 128], F32) for i in range(2)]
        hgo = [_alloc(es, nc, "C_hg%d" % i, [128, 4, 128], BF16) for i in range(2)]
        zeros = _alloc(es, nc, "C_z", [128, 128], F32)
        kb.op("dve", lambda e: e.memset(zeros.t[:], 0.0), writes=[zeros])
        p_att = [_alloc(es, nc, "C_pa%d" % i, [128, 128], F32, psum=True) for i in range(2)]
        p_tr = [_alloc(es, nc, "C_pt%d" % i, [128, 128], BF16, psum=True) for i in range(1)]
        p_o = [_alloc(es, nc, "C_po%d" % i, [128, 128], F32, psum=True) for i in range(2)]
        p_u = [_alloc(es, nc, "C_pu%d" % i, [128, 128], F32, psum=True) for i in range(2)]
        p_n = [_alloc(es, nc, "C_pn%d" % i, [128, 128], F32, psum=True) for i in range(1)]
        cnt = {"i": 0}
        fm = lambda nm: S[nm].rearrange("(h p) t -> p h t", p=128)

        for d_ in range(2):
            tri = g.trif if d_ == 0 else g.trib
            for h in range(4):
                kb.op("dve", lambda e, h=h: e.memset(Sst[h].t[:], 0.0), writes=[Sst[h]])
                kb.op("dve", lambda e, h=h: e.memset(Smid[h].t[:], 0.0), writes=[Smid[h]])
            smi = [0, 0, 0, 0]
            tiles = range(NT) if d_ == 0 else range(NT - 1, -1, -1)
            if g.lim:
                tiles = list(tiles)[:g.lim]
            for ti in tiles:
                i = cnt["i"] % NB_
                cnt["i"] += 1
                st = (ti * 128) // ST
                ts_ = slice(ti * 128, (ti + 1) * 128)
                with nc.allow_non_contiguous_dma(reason="head blocks"):
                    kb.dma("sp", lf[i].t[:], S["lf"][d_].rearrange("(h p) t -> p h t", p=128)[:, :, ts_], reads=[g.DB["lf"][st]], writes=[lf[i]])
                    kb.dma("sp", kk[i].t[:], S["kk"][d_].rearrange("(h p) t -> p h t", p=128)[:, :, ts_], reads=[g.DB["kk"][st]], writes=[kk[i]])
                    kb.dma("sp", qh[i].t[:], fm("qh")[:, :, ts_], reads=[g.DB["qh"][st]], writes=[qh[i]])
                    kb.dma("sp", vh[i].t[:], S["vh"][ts_, :], reads=[g.DB["vh"][st]], writes=[vh[i]])
                    if d_ == 1:
                        kb.dma("sp", gsb[i].t[:], fm("gs")[:, :, ts_], reads=[g.DB["gs"][st]], writes=[gsb[i]])
                        kb.dma("sp", ofb[i].t[:], fm("of")[:, :, ts_], reads=[g.DB["of"][st]], writes=[ofb[i]])
                ob = osb[ti % 2]
                for h in range(4):
                    C = cb_[h]
                    kb.op("dve", lambda e: e.memset(C.t[:, 0:1], 0.0), writes=[C])
                    kb.op("dve", lambda e: e.tensor_tensor_scan(out=C.t[:, 1:129], data0=lf[i].t[:, h, :], data1=zeros.t[:],
                                                               initial=0.0, op0=ALU.add, op1=ALU.add),
                          reads=[lf[i], zeros], writes=[C])
                    Cv = C.t[:, 1:129].rearrange("p (a x) -> p a x", x=64)
                    Z = Cv if d_ == 0 else C.t[:, 0:128].rearrange("p (a x) -> p a x", x=64)
                    endv = Cv[:, :, 63:64]
                    midv = Cv[:, :, 31:32]
                    B_ = bs[h]
                    kb.op("dve", lambda e: e.memset(B_.t[:, 0:1], 0.0), writes=[B_])
                    kb.op("dve", lambda e: e.tensor_copy(out=B_.t[:, 1:2], in_=C.t[:, 64:65]), reads=[C], writes=[B_])
                    T6 = t6[h]
                    bv = B_.t[:, :].unsqueeze(2)
                    kb.op("dve", lambda e: e.tensor_tensor(out=T6.t[:, :, 0:1], in0=endv, in1=bv, op=ALU.subtract), reads=[C, B_], writes=[T6])
                    kb.op("dve", lambda e: e.tensor_tensor(out=T6.t[:, :, 1:2], in0=endv, in1=midv, op=ALU.subtract), reads=[C], writes=[T6])
                    kb.op("dve", lambda e: e.tensor_tensor(out=T6.t[:, :, 2:3], in0=midv, in1=bv, op=ALU.subtract), reads=[C, B_], writes=[T6])
                    X6 = x6[h]
                    kb.op("act", lambda e: e.activation(out=X6.t[:], in_=T6.t[:], func=AF.Exp), reads=[T6], writes=[X6])
                    i_d2 = 1 if d_ == 0 else 2
                    i_m = 2 if d_ == 0 else 1
                    CM = cm[h]
                    kb.op("dve", lambda e: e.tensor_tensor(out=CM.t[:].rearrange("p (a x) -> p a x", x=64), in0=Z,
                                                          in1=midv.broadcast_to([128, 2, 64]), op=ALU.subtract), reads=[C], writes=[CM])
                    sgn = 1.0 if d_ == 0 else -1.0
                    kb.op("act", lambda e: e.activation(out=e1[h].t[:], in_=CM.t[:], func=AF.Exp, scale=sgn), reads=[CM], writes=[e1[h]])
                    kb.op("act", lambda e: e.activation(out=e2[h].t[:], in_=CM.t[:], func=AF.Exp, scale=-sgn), reads=[CM], writes=[e2[h]])
                    kb.op("pool", lambda e: e.tensor_tensor(out=qt[h].t[:], in0=qh[i].t[:, h, :], in1=e1[h].t[:], op=ALU.mult),
                          reads=[qh[i], e1[h]], writes=[qt[h]])
                    kb.op("dve", lambda e: e.tensor_tensor(out=kt[h].t[:], in0=kk[i].t[:, h, :], in1=e2[h].t[:], op=ALU.mult),
                          reads=[kk[i], e2[h]], writes=[kt[h]])
                    pa = p_att[h % 2]
                    kb.op("pe", lambda e: e.matmul(pa.t[:], kt[h].t[:], qt[h].t[:], start=True, stop=True), reads=[kt[h], qt[h]], writes=[pa])
                    kb.op("dve", lambda e: e.tensor_tensor(out=attm[h].t[:], in0=pa.t[:], in1=tri.t[:], op=ALU.mult), reads=[pa, tri], writes=[attm[h]])
                    ptr_ = p_tr[0]
                    kb.op("pe", lambda e: e.transpose(out=ptr_.t[:], in_=kt[h].t[:], identity=g.ident_bf.t[:]), reads=[kt[h], g.ident_bf], writes=[ptr_])
                    kb.op("act", lambda e: e.copy(out=ktok[h].t[:], in_=ptr_.t[:]), reads=[ptr_], writes=[ktok[h]])
                    po = p_o[h % 2]
                    vslice = vh[i].t[:, h * 128:(h + 1) * 128]
                    kb.op("pe", lambda e: e.matmul(po.t[:], vslice, attm[h].t[:], start=True, stop=False), reads=[vh[i], attm[h]], writes=[po])
                    chunks = (0, 1) if d_ == 0 else (1, 0)
                    smi[h] ^= 1
                    sm0 = Smid[h + 4 * smi[h]]
                    kb.op("dve", lambda e, sm0=sm0: e.tensor_scalar(out=sm0.t[:], in0=Sst[h].t[:], scalar1=X6.t[:, chunks[0], i_m:i_m + 1],
                                                                  scalar2=None, op0=ALU.mult), reads=[Sst[h], X6], writes=[sm0])
                    for ci, a in enumerate(chunks):
                        sm = Smid[h + 4 * smi[h]]
                        kb.op("pe", lambda e, a=a, sm=sm, ci=ci: e.matmul(po.t[:, a * 64:(a + 1) * 64], sm.t[:], qt[h].t[:, a * 64:(a + 1) * 64],
                                                                         start=False, stop=(ci == 1)),
                              reads=[sm, qt[h]], writes=[po])
                        pu = p_u[(h + a) % 2]
                        kb.op("pe", lambda e, a=a: e.matmul(pu.t[:], ktok[h].t[a * 64:(a + 1) * 64, :], vh[i].t[a * 64:(a + 1) * 64, h * 128:(h + 1) * 128],
                                                           start=True, stop=True), reads=[ktok[h], vh[i]], writes=[pu])
                        tm = tmp[h]
                        kb.op("dve", lambda e, a=a: e.tensor_scalar(out=tm.t[:], in0=pu.t[:], scalar1=X6.t[:, a, i_d2:i_d2 + 1], scalar2=None, op0=ALU.mult),
                              reads=[pu, X6], writes=[tm])
                        kb.op("dve", lambda e, a=a: e.scalar_tensor_tensor(out=Sst[h].t[:], in0=Sst[h].t[:], scalar=X6.t[:, a, 0:1], in1=tm.t[:],
                                                                          op0=ALU.mult, op1=ALU.add), reads=[Sst[h], X6, tm], writes=[Sst[h]])
                        chunk_id = ti * 2 + a
                        if (d_ == 0 and chunk_id == 63) or (d_ == 1 and chunk_id == 64):
                            kb.op("dve", lambda e: e.tensor_scalar(out=Sst[h].t[:], in0=Sst[h].t[:], scalar1=g.cflag.t[:, 0:1], scalar2=None, op0=ALU.mult),
                                  reads=[Sst[h], g.cflag], writes=[Sst[h]])
                        if ci == 0:
                            na_ = chunks[1]
                            smi[h] ^= 1
                            sm2 = Smid[h + 4 * smi[h]]
                            kb.op("dve", lambda e, na_=na_, sm2=sm2: e.tensor_scalar(out=sm2.t[:], in0=Sst[h].t[:], scalar1=X6.t[:, na_, i_m:i_m + 1],
                                                                                  scalar2=None, op0=ALU.mult), reads=[Sst[h], X6], writes=[sm2])
                    if d_ == 0:
                        kb.op("act", lambda e: e.copy(out=ob.t[:, h, :], in_=po.t[:]), reads=[po], writes=[ob])
                    else:
                        kb.op("dve", lambda e: e.tensor_tensor(out=ob.t[:, h, :], in0=po.t[:], in1=ofb[i].t[:, h, :], op=ALU.add),
                              reads=[po, ofb[i]], writes=[ob])
                        sq = sqb[h % 2]
                        kb.op("act", lambda e: e.activation(out=sq.t[:], in_=ob.t[:, h, :], func=AF.Square), reads=[ob], writes=[sq])
                        pn = p_n[0]
                        kb.op("pe", lambda e: e.matmul(pn.t[:], g.ones_f.t[:], sq.t[:], start=True, stop=True), reads=[g.ones_f, sq], writes=[pn])
                        rs = rsb[h % 2]
                        kb.op("act", lambda e: e.activation(out=rs.t[:], in_=pn.t[:], func=AF.Ln, scale=1.0 / 128, bias=EPS), reads=[pn], writes=[rs])
                        kb.op("act", lambda e: e.activation(out=rs.t[:], in_=rs.t[:], func=AF.Exp, scale=-0.5), reads=[rs], writes=[rs])
                        kb.op("dve", lambda e: e.scalar_tensor_tensor(out=rs.t[:], in0=ob.t[:, h, :], scalar=og.t[:, 0:1], in1=rs.t[:],
                                                                     op0=ALU.mult, op1=ALU.mult), reads=[ob, og, rs], writes=[rs])
                        kb.op("pool", lambda e: e.tensor_tensor(out=hgo[ti % 2].t[:, h, :], in0=rs.t[:], in1=gsb[i].t[:, h, :], op=ALU.mult),
                              reads=[rs, gsb[i]], writes=[hgo[ti % 2]])
                with nc.allow_non_contiguous_dma(reason="head blocks"):
                    if d_ == 0:
                        kb.dma("pool", fm("of")[:, :, ts_], ob.t[:], reads=[ob], writes=[g.DB["of"][st]])
                    else:
                        kb.dma("pool", fm("hgT")[:, :, ts_], hgo[ti % 2].t[:], reads=[hgo[ti % 2]], writes=[g.DB["hgT"][st]])


def stage_D(g, l):
    nc, kb, I, S = g.nc, g.kb, g.I, g.S
    TB = 1024
    with ExitStack() as es:
        cw = _alloc(es, nc, "D_cw", [128, 4, 31], F32)
        with nc.allow_non_contiguous_dma(reason="tiny conv weights"):
            for c in range(4):
                kb.dma("sp", cw.t[:, c, :], I["conv_w"][l, :, c * 128:(c + 1) * 128].rearrange("j p -> p j"), writes=[cw])
        cbias = load_col(g, es, "D_cb", I["conv_b"][l, :], 4)
        lng = load_col(g, es, "D_lg", I["conv_ln_g"][l, :], 4)
        lnb = load_col(g, es, "D_lb", I["conv_ln_b"][l, :], 4)
        ub = [_alloc(es, nc, "D_u%d" % i, [128, TB + 30], F32) for i in range(3)]
        yb = [_alloc(es, nc, "D_y%d" % i, [128, 4, TB], F32) for i in range(2)]
        sqt = [_alloc(es, nc, "D_sq%d" % i, [128, ST], F32) for i in range(2)]
        mean = [_alloc(es, nc, "D_mean%d" % i, [128, ST], F32) for i in range(2)]
        rstd = [_alloc(es, nc, "D_rstd%d" % i, [128, ST], F32) for i in range(2)]
        tt = [_alloc(es, nc, "D_t%d" % i, [128, ST], F32) for i in range(3)]
        ob = [_alloc(es, nc, "D_o%d" % i, [128, 4, ST], BF16) for i in range(2)]
        ps1 = [_alloc(es, nc, "D_p1%d" % i, [128, ST], F32, psum=True) for i in range(2)]
        ps2 = [_alloc(es, nc, "D_p2%d" % i, [128, ST], F32, psum=True) for i in range(2)]
        k = 0
        nsb = 0
        for blk in range(T // TB):
            t0 = blk * TB
            yv = yb[blk % 2]
            for c in range(4):
                u = ub[k % 3]
                k += 1
                lo, hi = t0 - 15, t0 + TB + 15
                a, b = max(lo, 0), min(hi, T)
                if lo < 0:
                    kb.op("pool", lambda e: e.memset(u.t[:, 0:15], 0.0), writes=[u])
                if hi > T:
                    kb.op("pool", lambda e: e.memset(u.t[:, TB + 15:TB + 30], 0.0), writes=[u])
                rd = [g.DB["u"][s_] for s_ in range(a // ST, (b - 1) // ST + 1)]
                kb.dma("sp", u.t[:, a - lo:b - lo], S["u"][c * 128:(c + 1) * 128, a:b], reads=rd, writes=[u])
                if t0 == 4096:
                    kb.op("pool", lambda e: e.tensor_scalar(out=u.t[:, 0:15], in0=u.t[:, 0:15], scalar1=g.cflag.t[:, 0:1], scalar2=None, op0=ALU.mult),
                          reads=[u, g.cflag], writes=[u])
                if t0 + TB == 4096:
                    kb.op("pool", lambda e: e.tensor_scalar(out=u.t[:, TB + 15:TB + 30], in0=u.t[:, TB + 15:TB + 30], scalar1=g.cflag.t[:, 0:1],
                                                           scalar2=None, op0=ALU.mult), reads=[u, g.cflag], writes=[u])
                kb.op("dve", lambda e: e.tensor_scalar(out=yv.t[:, c, :], in0=u.t[:, 0:TB], scalar1=cw.t[:, c, 0:1], scalar2=cbias.t[:, c:c + 1],
                                                      op0=ALU.mult, op1=ALU.add), reads=[u, cw, cbias], writes=[yv])
                for j in range(1, 31):
                    kb.op("dve", lambda e, j=j: e.scalar_tensor_tensor(out=yv.t[:, c, :], in0=u.t[:, j:j + TB], scalar=cw.t[:, c, j:j + 1], in1=yv.t[:, c, :],
                                                                      op0=ALU.mult, op1=ALU.add), reads=[u, cw, yv], writes=[yv])
            for sb_ in range(TB // ST):
                cs = slice(sb_ * ST, (sb_ + 1) * ST)
                p1 = ps1[nsb % 2]
                p2 = ps2[nsb % 2]
                for c in range(4):
                    sq = sqt[c % 2]
                    kb.op("act", lambda e, c=c: e.activation(out=sq.t[:], in_=yv.t[:, c, cs], func=AF.Square), reads=[yv], writes=[sq])
                    kb.op("pe", lambda e, c=c: e.matmul(p1.t[:], g.ones_f.t[:], yv.t[:, c, cs], start=(c == 0), stop=(c == 3)),
                          reads=[g.ones_f, yv], writes=[p1])
                    kb.op("pe", lambda e, c=c: e.matmul(p2.t[:], g.ones_f.t[:], sq.t[:], start=(c == 0), stop=(c == 3)),
                          reads=[g.ones_f, sq], writes=[p2])
                mn = mean[nsb % 2]
                rs = rstd[nsb % 2]
                kb.op("act", lambda e: e.activation(out=mn.t[:], in_=p1.t[:], func=AF.Copy, scale=1.0 / 512), reads=[p1], writes=[mn])
                msq = tt[2]
                kb.op("dve", lambda e: e.tensor_tensor(out=msq.t[:], in0=mn.t[:], in1=mn.t[:], op=ALU.mult), reads=[mn], writes=[msq])
                kb.op("dve", lambda e: e.scalar_tensor_tensor(out=rs.t[:], in0=p2.t[:], scalar=1.0 / 512, in1=msq.t[:], op0=ALU.mult, op1=ALU.subtract),
                      reads=[p2, msq], writes=[rs])
                kb.op("act", lambda e: e.activation(out=rs.t[:], in_=rs.t[:], func=AF.Ln, bias=EPS), reads=[rs], writes=[rs])
                kb.op("act", lambda e: e.activation(out=rs.t[:], in_=rs.t[:], func=AF.Exp, scale=-0.5), reads=[rs], writes=[rs])
                o = ob[nsb % 2]
                for c in range(4):
                    t_ = tt[c % 2]
                    kb.op("dve", lambda e, c=c: e.tensor_tensor(out=t_.t[:], in0=yv.t[:, c, cs], in1=mn.t[:], op=ALU.subtract), reads=[yv, mn], writes=[t_])
                    kb.op("pool", lambda e: e.tensor_tensor(out=t_.t[:], in0=t_.t[:], in1=rs.t[:], op=ALU.mult), reads=[t_, rs], writes=[t_])
                    kb.op("act", lambda e, c=c: e.activation(out=o.t[:, c, :], in_=t_.t[:], func=AF.Silu, scale=lng.t[:, c:c + 1], bias=lnb.t[:, c:c + 1]),
                          reads=[t_, lng, lnb], writes=[o])
                st = (t0 + sb_ * ST) // ST
                kb.dma("pool", S["cvT"].rearrange("(c p) t -> p c t", p=128)[:, :, st * ST:(st + 1) * ST], o.t[:], reads=[o], writes=[g.DB["cvT"][st]])
                nsb += 1


def stage_E(g, l, xsrc):
    nc, kb, I, S = g.nc, g.kb, g.I, g.S
    with ExitStack() as es:
        wbr = _alloc(es, nc, "E_wbr", [128, 12, D], BF16)
        for bi, nm in enumerate(("w_na_br", "w_hg_br", "w_cv_br")):
            kb.dma("pool", wbr.t[:, bi * 4:(bi + 1) * 4, :], I[nm][l].rearrange("(c p) n -> p c n", p=128), writes=[wbr])
        wout = _alloc(es, nc, "E_wout", [128, 8, D], BF16)
        kb.dma("pool", wout.t[:], I["w_out"][l].rearrange("(c p) n -> p c n", p=128), writes=[wout])
        brT = [[_alloc(es, nc, "E_br%d_%d" % (b, i), [128, 4, ST], BF16) for b in range(3)] for i in range(2)]
        sgt = [_alloc(es, nc, "E_sg%d" % i, [128, 24, ST], BF16) for i in range(2)]
        mT = _alloc(es, nc, "E_mT", [128, 8, ST], BF16)
        tmp = [_alloc(es, nc, "E_t%d" % i, [128, ST], F32) for i in range(6)]
        xa = [_alloc(es, nc, "E_x%d" % i, [128, D], F32) for i in range(3)]
        pb = [_alloc(es, nc, "E_pb%d" % i, [128, ST], F32, psum=True) for i in range(6)]
        pq = [_alloc(es, nc, "E_pq%d" % i, [128, ST], F32, psum=True) for i in range(2)]
        fmv = lambda nm: S[nm].rearrange("(c p) t -> p c t", p=128)
        n_pb = 0
        n_x = 0
        n_pq = 0
        for st in range(NST):
            t0 = st * ST
            br = brT[st % 2]
            sg = sgt[st % 2]
            for b, nm in enumerate(("naT", "hgT", "cvT")):
                kb.dma("sp", br[b].t[:], fmv(nm)[:, :, t0:t0 + ST], reads=[g.DB[nm][st]], writes=[br[b]])
            kb.dma("sp", sg.t[:], fmv("sg")[:, :, t0:t0 + ST], reads=[g.DB["sg"][st]], writes=[sg])
            for m in range(8):
                ps = []
                for b in range(3):
                    p = pb[n_pb % 6]
                    n_pb += 1
                    for kc in range(4):
                        kb.op("pe", lambda e, b=b, kc=kc, p=p: e.matmul(p.t[:], wbr.t[:, b * 4 + kc, m * 128:(m + 1) * 128], br[b].t[:, kc, :],
                                                                       start=(kc == 0), stop=(kc == 3)), reads=[wbr, br[b]], writes=[p])
                    ps.append(p)
                t1, t2, t3 = tmp[(m % 2) * 3], tmp[(m % 2) * 3 + 1], tmp[(m % 2) * 3 + 2]
                kb.op("dve", lambda e: e.tensor_tensor(out=t1.t[:], in0=ps[0].t[:], in1=sg.t[:, m, :], op=ALU.mult), reads=[ps[0], sg], writes=[t1])
                kb.op("dve", lambda e: e.tensor_tensor(out=t2.t[:], in0=ps[1].t[:], in1=sg.t[:, 8 + m, :], op=ALU.mult), reads=[ps[1], sg], writes=[t2])
                kb.op("dve", lambda e: e.tensor_tensor(out=t3.t[:], in0=ps[2].t[:], in1=sg.t[:, 16 + m, :], op=ALU.mult), reads=[ps[2], sg], writes=[t3])
                kb.op("pool", lambda e: e.tensor_tensor(out=t1.t[:], in0=t1.t[:], in1=t2.t[:], op=ALU.add), reads=[t1, t2], writes=[t1])
                kb.op("pool", lambda e: e.tensor_tensor(out=mT.t[:, m, :], in0=t1.t[:], in1=t3.t[:], op=ALU.add), reads=[t1, t3], writes=[mT])
            for tb in range(4):
                x = xa[n_x % 3]
                n_x += 1
                rows = slice(t0 + tb * 128, t0 + (tb + 1) * 128)
                kb.dma("sp", x.t[:], xsrc[rows, :], reads=[g.DB["y"][st]], writes=[x])
                for hf in range(2):
                    p = pq[n_pq % 2]
                    n_pq += 1
                    for m in range(8):
                        kb.op("pe", lambda e, m=m, p=p: e.matmul(p.t[:], mT.t[:, m, tb * 128:(tb + 1) * 128], wout.t[:, m, hf * 512:(hf + 1) * 512],
                                                                start=(m == 0), stop=(m == 7)), reads=[mT, wout], writes=[p])
                    kb.op("dve", lambda e, p=p: e.tensor_tensor(out=x.t[:, hf * 512:(hf + 1) * 512], in0=p.t[:], in1=x.t[:, hf * 512:(hf + 1) * 512], op=ALU.add),
                          reads=[p, x], writes=[x])
                kb.dma("pool", g.yc[rows, :], x.t[:], reads=[x], writes=[g.DB["y"][st]])


def stage_F(g, l):
    nc, kb, I, S = g.nc, g.kb, g.I, g.S
    y = g.yc
    with ExitStack() as es:
        W = NormWork(g, es, "F")
        gmem = load_bcast_row(g, es, "F_gmem", I["norm_mem"][l, :], D)
        gkv = load_bcast_row(g, es, "F_gkv", I["mem_kv_norm"][l, :], D)
        gffn = load_bcast_row(g, es, "F_gffn", I["norm_ffn"][l, :], D)
        qn = load_col(g, es, "F_qn", I["mem_q_norm"][l, :], 1)
        kn = load_col(g, es, "F_kn", I["mem_k_norm"][l, :], 1)
        kb.op("dve", lambda e: e.tensor_scalar(out=qn.t[:], in0=qn.t[:], scalar1=128.0 ** -0.5, scalar2=None, op0=ALU.mult), reads=[qn], writes=[qn])
        wq = _alloc(es, nc, "F_wq", [128, 8, 512], BF16)
        kb.dma("pool", wq.t[:], I["wq_mem"][l].rearrange("(c p) n -> p c n", p=128), writes=[wq])
        wkv = _alloc(es, nc, "F_wkv", [128, 8, D], BF16)
        kb.dma("pool", wkv.t[:], I["wkv_mem"][l].rearrange("(c p) n -> p c n", p=128), writes=[wkv])
        wo = _alloc(es, nc, "F_wo", [128, 4, D], BF16)
        kb.dma("pool", wo.t[:], I["wo_mem"][l].rearrange("(c p) n -> p c n", p=128), writes=[wo])
        wr = _alloc(es, nc, "F_wr", [128, 8, NE], BF16)
        with nc.allow_non_contiguous_dma(reason="router weights"):
            kb.dma("pool", wr.t[:], I["w_router"][l].rearrange("(c p) n -> p c n", p=128), writes=[wr])
        hT = _alloc(es, nc, "F_hT", [128, 8, ST], BF16)
        h2T = [_alloc(es, nc, "F_h2T%d" % i, [128, 8, ST], BF16) for i in range(2)]
        KT = [_alloc(es, nc, "F_KT%d" % i, [128, 4, 256], BF16) for i in range(2)]
        V = [_alloc(es, nc, "F_V%d" % i, [128, 2, 512], BF16) for i in range(2)]
        f32t = [_alloc(es, nc, "F_f%d" % i, [128, ST], F32) for i in range(4)]
        qT = [_alloc(es, nc, "F_qT%d" % i, [128, ST], BF16) for i in range(2)]
        PT = [_alloc(es, nc, "F_PT%d" % i, [128, ST], BF16) for i in range(4)]
        oT = _alloc(es, nc, "F_oT", [128, 4, ST], BF16)
        sm = [_alloc(es, nc, "F_sm%d" % i, [128, 4], F32) for i in range(2)]
        ex = [_alloc(es, nc, "F_ex%d" % i, [128, NE], F32) for i in range(2)]
        af = [_alloc(es, nc, "F_af%d" % i, [128, NE], F32) for i in range(2)]
        p_q = _alloc(es, nc, "F_pq", [128, ST], F32, psum=True)
        p_n = _alloc(es, nc, "F_pn", [128, ST], F32, psum=True)
        p_s = [_alloc(es, nc, "F_ps%d" % i, [128, ST], F32, psum=True) for i in range(2)]
        p_o = _alloc(es, nc, "F_po", [128, ST], F32, psum=True)
        p_d = _alloc(es, nc, "F_pd", [128, ST], F32, psum=True)
        p_x = _alloc(es, nc, "F_px", [128, ST], F32, psum=True)

        def chan_norm(p, n, gain_col, out_bf, out_tt):
            sq = f32t[0]
            kb.op("act", lambda e: e.activation(out=sq.t[:, 0:n], in_=p.t[:, 0:n], func=AF.Square), reads=[p], writes=[sq])
            kb.op("pe", lambda e: e.matmul(p_n.t[:, 0:n], g.ones_f.t[:], sq.t[:, 0:n], start=True, stop=True), reads=[g.ones_f, sq], writes=[p_n])
            rs = f32t[1]
            kb.op("act", lambda e: e.activation(out=rs.t[:, 0:n], in_=p_n.t[:, 0:n], func=AF.Ln, scale=1.0 / 128, bias=EPS), reads=[p_n], writes=[rs])
            kb.op("act", lambda e: e.activation(out=rs.t[:, 0:n], in_=rs.t[:, 0:n], func=AF.Exp, scale=-0.5), reads=[rs], writes=[rs])
            kb.op("dve", lambda e: e.scalar_tensor_tensor(out=out_bf, in0=p.t[:, 0:n], scalar=gain_col, in1=rs.t[:, 0:n], op0=ALU.mult, op1=ALU.mult),
                  reads=[p, rs, qn, kn], writes=[out_tt])

        for s in range(2):
            for tb in range(2):
                kb.dma("sp", W.xa[tb].t[:], g.memc[s, tb * 128:(tb + 1) * 128, :], writes=[W.xa[tb]])
            rms_rows(g, W.xa, W.ss, W.rstd, W.junk, 2)
            for tb in range(2):
                kb.op("dve", lambda e, tb=tb: e.scalar_tensor_tensor(out=W.hb[tb].t[:], in0=W.xa[tb].t[:], scalar=W.rstd.t[:, tb:tb + 1], in1=gkv.t[:],
                                                                    op0=ALU.mult, op1=ALU.mult), reads=[W.xa[tb], W.rstd, gkv], writes=[W.hb[tb]])
                for kc in range(8):
                    kb.op("pe", lambda e, tb=tb, kc=kc: e.transpose(out=W.ptr.t[:, kc, :], in_=W.hb[tb].t[:, kc * 128:(kc + 1) * 128], identity=g.ident_bf.t[:]),
                          reads=[W.hb[tb], g.ident_bf], writes=[W.ptr])
                kb.op("act", lambda e, tb=tb: e.copy(out=hT.t[:, :, tb * 128:(tb + 1) * 128], in_=W.ptr.t[:]), reads=[W.ptr], writes=[hT])
            for h in range(4):
                for kc in range(8):
                    kb.op("pe", lambda e, kc=kc: e.matmul(p_q.t[:, 0:256], wkv.t[:, kc, h * 128:(h + 1) * 128], hT.t[:, kc, 0:256], start=(kc == 0), stop=(kc == 7)),
                          reads=[wkv, hT], writes=[p_q])
                chan_norm(p_q, 256, kn.t[:, 0:1], KT[s].t[:, h, :], KT[s])
            for mc in range(2):
                for kc in range(8):
                    kb.op("pe", lambda e, kc=kc: e.matmul(p_o.t[:], hT.t[:, kc, mc * 128:(mc + 1) * 128], wkv.t[:, kc, 512:1024], start=(kc == 0), stop=(kc == 7)),
                          reads=[wkv, hT], writes=[p_o])
                kb.op("act", lambda e: e.copy(out=V[s].t[:, mc, :], in_=p_o.t[:]), reads=[p_o], writes=[V[s]])

        for st in range(NST):
            t0 = st * ST
            s = st // (NST // 2)
            norm_transpose(g, y, st, gmem, W, hT)
            for h in range(4):
                for kc in range(8):
                    kb.op("pe", lambda e, kc=kc: e.matmul(p_q.t[:], wq.t[:, kc, h * 128:(h + 1) * 128], hT.t[:, kc, :], start=(kc == 0), stop=(kc == 7)),
                          reads=[wq, hT], writes=[p_q])
                q_ = qT[h % 2]
                chan_norm(p_q, ST, qn.t[:, 0:1], q_.t[:], q_)
                pts = []
                for mc in range(2):
                    ps_ = p_s[mc]
                    kb.op("pe", lambda e: e.matmul(ps_.t[:], KT[s].t[:, h, mc * 128:(mc + 1) * 128], q_.t[:], start=True, stop=True), reads=[KT[s], q_], writes=[ps_])
                    pt = PT[(h % 2) * 2 + mc]
                    kb.op("act", lambda e: e.activation(out=pt.t[:], in_=ps_.t[:], func=AF.Exp), reads=[ps_], writes=[pt])
                    pts.append(pt)
                for mc in range(2):
                    kb.op("pe", lambda e: e.matmul(p_o.t[:], V[s].t[:, mc, h * 128:(h + 1) * 128], pts[mc].t[:], start=(mc == 0), stop=(mc == 1)),
                          reads=[V[s], pts[mc]], writes=[p_o])
                for mc in range(2):
                    kb.op("pe", lambda e: e.matmul(p_d.t[:], g.ones_bf.t[:], pts[mc].t[:], start=(mc == 0), stop=(mc == 1)), reads=[g.ones_bf, pts[mc]], writes=[p_d])
                rc = f32t[2 + h % 2]
                kb.op("dve", lambda e: e.reciprocal(out=rc.t[:], in_=p_d.t[:]), reads=[p_d], writes=[rc])
                kb.op("dve", lambda e: e.tensor_tensor(out=oT.t[:, h, :], in0=p_o.t[:], in1=rc.t[:], op=ALU.mult), reads=[p_o, rc], writes=[oT])
            for tb in range(4):
                x = W.xa[tb]
                rows = slice(t0 + tb * 128, t0 + (tb + 1) * 128)
                for hf in range(2):
                    for h in range(4):
                        kb.op("pe", lambda e, h=h: e.matmul(p_x.t[:], oT.t[:, h, tb * 128:(tb + 1) * 128], wo.t[:, h, hf * 512:(hf + 1) * 512], start=(h == 0), stop=(h == 3)),
                              reads=[oT, wo], writes=[p_x])
                    kb.op("dve", lambda e: e.tensor_tensor(out=x.t[:, hf * 512:(hf + 1) * 512], in0=p_x.t[:], in1=x.t[:, hf * 512:(hf + 1) * 512], op=ALU.add),
                          reads=[p_x, x], writes=[x])
                kb.dma("pool", y[rows, :], x.t[:], reads=[x], writes=[g.DB["y"][st]])
            rms_rows(g, W.xa, W.ss, W.rstd, W.junk, 4)
            h2 = h2T[st % 2]
            for tb in range(4):
                kb.op("dve", lambda e, tb=tb: e.scalar_tensor_tensor(out=W.hb[tb].t[:], in0=W.xa[tb].t[:], scalar=W.rstd.t[:, tb:tb + 1], in1=gffn.t[:],
                                                                    op0=ALU.mult, op1=ALU.mult), reads=[W.xa[tb], W.rstd, gffn], writes=[W.hb[tb]])
                for kc in range(8):
                    kb.op("pe", lambda e, tb=tb, kc=kc: e.transpose(out=W.ptr.t[:, kc, :], in_=W.hb[tb].t[:, kc * 128:(kc + 1) * 128], identity=g.ident_bf.t[:]),
                          reads=[W.hb[tb], g.ident_bf], writes=[W.ptr])
                kb.op("act", lambda e, tb=tb: e.copy(out=h2.t[:, :, tb * 128:(tb + 1) * 128], in_=W.ptr.t[:]), reads=[W.ptr], writes=[h2])
                kb.dma("sp", g.h2rc[t0 + tb * 128:t0 + (tb + 1) * 128, :], W.hb[tb].t[:], reads=[W.hb[tb]], writes=[g.DBall["h2r"]])
            for tb in range(4):
                for kc in range(8):
                    kb.op("pe", lambda e, kc=kc: e.matmul(p_x.t[:, 0:NE], h2.t[:, kc, tb * 128:(tb + 1) * 128], wr.t[:, kc, :], start=(kc == 0), stop=(kc == 7)),
                          reads=[h2, wr], writes=[p_x])
                s_ = sm[tb % 2]
                kb.op("dve", lambda e: e.tensor_reduce(out=s_.t[:, 0:1], in_=p_x.t[:, 0:NE], axis=AX.X, op=ALU.max), reads=[p_x], writes=[s_])
                kb.op("dve", lambda e: e.tensor_scalar(out=s_.t[:, 1:2], in0=s_.t[:, 0:1], scalar1=-1.0, scalar2=None, op0=ALU.mult), reads=[s_], writes=[s_])
                e_ = ex[tb % 2]
                kb.op("act", lambda e: e.activation(out=e_.t[:], in_=p_x.t[:, 0:NE], func=AF.Exp, bias=s_.t[:, 1:2], accum_out=s_.t[:, 2:3]),
                      reads=[p_x, s_], writes=[e_, s_])
                kb.op("dve", lambda e: e.reciprocal(out=s_.t[:, 3:4], in_=s_.t[:, 2:3]), reads=[s_], writes=[s_])
                a_ = af[tb % 2]
                kb.op("dve", lambda e: e.tensor_scalar(out=a_.t[:], in0=e_.t[:], scalar1=s_.t[:, 3:4], scalar2=None, op0=ALU.mult), reads=[e_, s_], writes=[a_])
                kb.dma("sp", g.affc[t0 + tb * 128:t0 + (tb + 1) * 128, :], a_.t[:], reads=[a_], writes=[g.DBall["affall"]])


def stage_H(g, l):
    nc, kb, I, S = g.nc, g.kb, g.I, g.S
    rd = [b for b in g.DB["aff"]]
    coll(g, "AllGather", ALU.bypass, [[0, 1, 2, 3], [4, 5, 6, 7]], S["aff"][:, :], S["affall"][:, :], rd, g.DBall["affall"])
    with ExitStack() as es:
        A = _alloc(es, nc, "H_A", [128, 256, NE], F32)
        kb.dma("sp", A.t[:], S["affall"].rearrange("(p j) e -> p j e", p=128), reads=[g.DBall["affall"]], writes=[A])
        cmp_ = _alloc(es, nc, "H_cmp", [128, 256, NE], BF16)
        lo = _alloc(es, nc, "H_lo", [128, NE], F32)
        hi = _alloc(es, nc, "H_hi", [128, NE], F32)
        mid = _alloc(es, nc, "H_mid", [128, NE], F32)
        cp = _alloc(es, nc, "H_cp", [128, NE], F32)
        ge = _alloc(es, nc, "H_ge", [128, NE], F32)
        d1 = _alloc(es, nc, "H_d1", [128, NE], F32)
        d2 = _alloc(es, nc, "H_d2", [128, NE], F32)
        pt = _alloc(es, nc, "H_pt", [128, NE], F32, psum=True)
        kb.op("dve", lambda e: e.memset(lo.t[:], 0.0), writes=[lo])
        kb.op("dve", lambda e: e.memset(hi.t[:], 1.0), writes=[hi])
        for it in range(NBIS):
            kb.op("dve", lambda e: e.tensor_tensor(out=mid.t[:], in0=lo.t[:], in1=hi.t[:], op=ALU.add), reads=[lo, hi], writes=[mid])
            kb.op("dve", lambda e: e.tensor_scalar(out=mid.t[:], in0=mid.t[:], scalar1=0.5, scalar2=None, op0=ALU.mult), reads=[mid], writes=[mid])
            kb.op("dve", lambda e: e.tensor_tensor(out=cmp_.t[:], in0=A.t[:], in1=mid.t[:, :].unsqueeze(1).broadcast_to([128, 256, NE]), op=ALU.is_gt),
                  reads=[A, mid], writes=[cmp_])
            kb.op("dve", lambda e: e.tensor_reduce(out=cp.t[:], in_=cmp_.t[:].rearrange("p j e -> p e j"), axis=AX.X, op=ALU.add), reads=[cmp_], writes=[cp])
            kb.op("pe", lambda e: e.matmul(pt.t[:], g.ones_f.t[:], cp.t[:], start=True, stop=True), reads=[g.ones_f, cp], writes=[pt])
            kb.op("dve", lambda e: e.tensor_scalar(out=ge.t[:], in0=pt.t[:], scalar1=float(CAPK) - 0.5, scalar2=None, op0=ALU.is_gt), reads=[pt], writes=[ge])
            kb.op("dve", lambda e: e.tensor_tensor(out=d1.t[:], in0=mid.t[:], in1=lo.t[:], op=ALU.subtract), reads=[mid, lo], writes=[d1])
            kb.op("dve", lambda e: e.tensor_tensor(out=d1.t[:], in0=d1.t[:], in1=ge.t[:], op=ALU.mult), reads=[d1, ge], writes=[d1])
            kb.op("dve", lambda e: e.tensor_tensor(out=d2.t[:], in0=hi.t[:], in1=mid.t[:], op=ALU.subtract), reads=[hi, mid], writes=[d2])
            kb.op("dve", lambda e: e.tensor_tensor(out=d2.t[:], in0=d2.t[:], in1=ge.t[:], op=ALU.mult), reads=[d2, ge], writes=[d2])
            kb.op("dve", lambda e: e.tensor_tensor(out=lo.t[:], in0=lo.t[:], in1=d1.t[:], op=ALU.add), reads=[lo, d1], writes=[lo])
            kb.op("dve", lambda e: e.tensor_tensor(out=hi.t[:], in0=mid.t[:], in1=d2.t[:], op=ALU.add), reads=[mid, d2], writes=[hi])
        own = _alloc(es, nc, "H_own", [128, NT, NE], F32)
        with nc.allow_non_contiguous_dma(reason="aff rows"):
            kb.dma("sp", own.t[:], S["aff"].rearrange("(j p) e -> p j e", p=128), reads=rd, writes=[own])
        msk = _alloc(es, nc, "H_msk", [128, NT, NE], F32)
        kb.op("dve", lambda e: e.tensor_tensor(out=msk.t[:], in0=own.t[:], in1=lo.t[:, :].unsqueeze(1).broadcast_to([128, NT, NE]), op=ALU.is_gt),
              reads=[own, lo], writes=[msk])
        kb.op("dve", lambda e: e.tensor_tensor(out=g.wg.t[:], in0=own.t[:], in1=msk.t[:], op=ALU.mult), reads=[own, msk], writes=[g.wg])


def coll(g, kind, op, groups, in_ap, out_ap, reads, out_buf):
    kb = g.kb
    i = kb.next_dsem("pool")
    deps_ = kb._deps(reads, [out_buf])
    if kb.dcnt[i] > 0:
        deps_[("d", i)] = max(deps_.get(("d", i), 0), kb.dcnt[i])
    kb._wait("pool", deps_)
    inst = g.nc.gpsimd.collective_compute(kind, op, replica_groups=groups, ins=[in_ap], outs=[out_ap])
    kb.dcnt[i] += 16
    inst.then_inc(kb.semh[("d", i)], 16)
    for b in reads:
        b.r[("d", i)] = kb.dcnt[i]
    out_buf.w = (("d", i), kb.dcnt[i])
    out_buf.r = {}


def stage_W(g):
    nc, kb, I = g.nc, g.kb, g.I
    g.wall = {}
    allc = [list(range(8))]
    for l in range(DEPTH):
        for nm, rows, cols in (("w_gate", D, DFF), ("w_up", D, DFF), ("w_down", DFF, D)):
            loc = nc.dram_tensor("%s_loc%d" % (nm, l), [2 * rows, cols], F32, kind="Internal").ap()
            al = nc.dram_tensor("%s_all%d" % (nm, l), [NE * rows, cols], F32, kind="Internal").ap()
            bl, ba = Buf(), Buf()
            kb.dma("sp", loc[:, :], I[nm][l], writes=[bl])
            coll(g, "AllGather", ALU.bypass, allc, loc[:, :], al[:, :], [bl], ba)
            g.wall[(nm, l)] = (al, ba)


def stage_I(g, l):
    nc, kb, I, S = g.nc, g.kb, g.I, g.S
    y = g.y
    NB = 256
    NFC = (DFF + 127) // 128
    with ExitStack() as es:
        wgu = _alloc(es, nc, "I_wgu", [128, 8, 2 * DFF], BF16)
        wd = _alloc(es, nc, "I_wd", [128, NFC, D], BF16)
        h2 = [_alloc(es, nc, "I_h2%d" % i, [128, 8, NB], BF16) for i in range(2)]
        aT = _alloc(es, nc, "I_aT", [128, NFC, NB], BF16)
        sgt = [_alloc(es, nc, "I_sg%d" % i, [128, NB], F32) for i in range(3)]
        xa = [_alloc(es, nc, "I_x%d" % i, [128, D], F32) for i in range(2)]
        pg = [_alloc(es, nc, "I_pg%d" % i, [128, NB], F32, psum=True) for i in range(2)]
        pu = [_alloc(es, nc, "I_pu%d" % i, [128, NB], F32, psum=True) for i in range(2)]
        pq = [_alloc(es, nc, "I_pq%d" % i, [128, 512], F32, psum=True) for i in range(2)]
        n = {"h": 0, "f": 0, "x": 0, "q": 0}
        for ex in range(NE):
            for kc in range(8):
                for hi_, nm_ in enumerate(("w_gate", "w_up")):
                    al_, ba_ = g.wall[(nm_, l)]
                    kb.dma("pool", wgu.t[:, kc, hi_ * DFF:(hi_ + 1) * DFF], al_[ex * D + kc * 128:ex * D + (kc + 1) * 128, :], reads=[ba_], writes=[wgu])
            for fc in range(NFC):
                fw = min(128, DFF - fc * 128)
                al_, ba_ = g.wall[("w_down", l)]
                kb.dma("pool", wd.t[0:fw, fc, :], al_[ex * DFF + fc * 128:ex * DFF + fc * 128 + fw, :], reads=[ba_], writes=[wd])
            for bk in range(T // NB):
                t0 = bk * NB
                st = t0 // ST
                hh = h2[n["h"] % 2]
                n["h"] += 1
                kb.dma("sp", hh.t[:], S["h2T"].rearrange("(c p) t -> p c t", p=128)[:, :, t0:t0 + NB], reads=[g.DB["h2T"][st]], writes=[hh])
                for fc in range(NFC):
                    fw = min(128, DFF - fc * 128)
                    p_g = pg[n["f"] % 2]
                    p_u = pu[n["f"] % 2]
                    s_ = sgt[n["f"] % 3]
                    n["f"] += 1
                    for kc in range(8):
                        kb.op("pe", lambda e, kc=kc: e.matmul(p_g.t[0:fw, :], wgu.t[:, kc, fc * 128:fc * 128 + fw], hh.t[:, kc, :], start=(kc == 0), stop=(kc == 7)),
                              reads=[wgu, hh], writes=[p_g])
                    for kc in range(8):
                        kb.op("pe", lambda e, kc=kc: e.matmul(p_u.t[0:fw, :], wgu.t[:, kc, DFF + fc * 128:DFF + fc * 128 + fw], hh.t[:, kc, :], start=(kc == 0), stop=(kc == 7)),
                              reads=[wgu, hh], writes=[p_u])
                    kb.op("act", lambda e: e.activation(out=s_.t[0:fw, :], in_=p_g.t[0:fw, :], func=AF.Sigmoid), reads=[p_g], writes=[s_])
                    kb.op("dve", lambda e: e.tensor_tensor(out=s_.t[0:fw, :], in0=p_g.t[0:fw, :], in1=s_.t[0:fw, :], op=ALU.mult), reads=[p_g, s_], writes=[s_])
                    kb.op("dve", lambda e: e.tensor_tensor(out=aT.t[0:fw, fc, :], in0=p_u.t[0:fw, :], in1=s_.t[0:fw, :], op=ALU.mult), reads=[p_u, s_], writes=[aT])
                for tb in range(NB // 128):
                    tile_i = (t0 + tb * 128) // 128
                    rows = slice(t0 + tb * 128, t0 + (tb + 1) * 128)
                    x = xa[n["x"] % 2]
                    n["x"] += 1
                    kb.dma("sp", x.t[:], y[rows, :], reads=[g.DB["y"][st]], writes=[x])
                    for hf in range(2):
                        p = pq[n["q"] % 2]
                        n["q"] += 1
                        for fc in range(NFC):
                            fw = min(128, DFF - fc * 128)
                            kb.op("pe", lambda e, fc=fc, fw=fw: e.matmul(p.t[:], aT.t[0:fw, fc, tb * 128:(tb + 1) * 128], wd.t[0:fw, fc, hf * 512:(hf + 1) * 512],
                                                                        start=(fc == 0), stop=(fc == NFC - 1)), reads=[aT, wd], writes=[p])
                        kb.op("dve", lambda e: e.scalar_tensor_tensor(out=x.t[:, hf * 512:(hf + 1) * 512], in0=p.t[:], scalar=g.wg.t[:, tile_i, ex:ex + 1],
                                                                     in1=x.t[:, hf * 512:(hf + 1) * 512], op0=ALU.mult, op1=ALU.add), reads=[p, g.wg, x], writes=[x])
                    kb.dma("pool", y[rows, :], x.t[:], reads=[x], writes=[g.DB["y"][st]])


def stage_H2(g, l):
    nc, kb, I, S = g.nc, g.kb, g.I, g.S
    with ExitStack() as es:
        A = _alloc(es, nc, "H_A", [128, 256, NE], F32)
        kb.dma("sp", A.t[:], S["affall"].rearrange("(p j) e -> p j e", p=128), reads=[g.DBall["affall"]], writes=[A])
        cmp_ = _alloc(es, nc, "H_cmp", [128, 256, NE], BF16)
        lo = _alloc(es, nc, "H_lo", [128, NE], F32)
        hi = _alloc(es, nc, "H_hi", [128, NE], F32)
        mid = _alloc(es, nc, "H_mid", [128, NE], F32)
        cp = _alloc(es, nc, "H_cp", [128, NE], F32)
        ge = _alloc(es, nc, "H_ge", [128, NE], F32)
        d1 = _alloc(es, nc, "H_d1", [128, NE], F32)
        d2 = _alloc(es, nc, "H_d2", [128, NE], F32)
        pt = _alloc(es, nc, "H_pt", [128, NE], F32, psum=True)
        kb.op("dve", lambda e: e.memset(lo.t[:], 0.0), writes=[lo])
        kb.op("dve", lambda e: e.memset(hi.t[:], 1.0), writes=[hi])
        for it in range(NBIS):
            kb.op("dve", lambda e: e.tensor_tensor(out=mid.t[:], in0=lo.t[:], in1=hi.t[:], op=ALU.add), reads=[lo, hi], writes=[mid])
            kb.op("dve", lambda e: e.tensor_scalar(out=mid.t[:], in0=mid.t[:], scalar1=0.5, scalar2=None, op0=ALU.mult), reads=[mid], writes=[mid])
            kb.op("dve", lambda e: e.tensor_tensor(out=cmp_.t[:], in0=A.t[:], in1=mid.t[:, :].unsqueeze(1).broadcast_to([128, 256, NE]), op=ALU.is_gt),
                  reads=[A, mid], writes=[cmp_])
            kb.op("dve", lambda e: e.tensor_reduce(out=cp.t[:], in_=cmp_.t[:].rearrange("p j e -> p e j"), axis=AX.X, op=ALU.add), reads=[cmp_], writes=[cp])
            kb.op("pe", lambda e: e.matmul(pt.t[:], g.ones_f.t[:], cp.t[:], start=True, stop=True), reads=[g.ones_f, cp], writes=[pt])
            kb.op("dve", lambda e: e.tensor_scalar(out=ge.t[:], in0=pt.t[:], scalar1=float(CAPK) - 0.5, scalar2=None, op0=ALU.is_gt), reads=[pt], writes=[ge])
            kb.op("dve", lambda e: e.tensor_tensor(out=d1.t[:], in0=mid.t[:], in1=lo.t[:], op=ALU.subtract), reads=[mid, lo], writes=[d1])
            kb.op("dve", lambda e: e.tensor_tensor(out=d1.t[:], in0=d1.t[:], in1=ge.t[:], op=ALU.mult), reads=[d1, ge], writes=[d1])
            kb.op("dve", lambda e: e.tensor_tensor(out=d2.t[:], in0=hi.t[:], in1=mid.t[:], op=ALU.subtract), reads=[hi, mid], writes=[d2])
            kb.op("dve", lambda e: e.tensor_tensor(out=d2.t[:], in0=d2.t[:], in1=ge.t[:], op=ALU.mult), reads=[d2, ge], writes=[d2])
            kb.op("dve", lambda e: e.tensor_tensor(out=lo.t[:], in0=lo.t[:], in1=d1.t[:], op=ALU.add), reads=[lo, d1], writes=[lo])
            kb.op("dve", lambda e: e.tensor_tensor(out=hi.t[:], in0=mid.t[:], in1=d2.t[:], op=ALU.add), reads=[mid, d2], writes=[hi])
        M = _alloc(es, nc, "H_M", [128, 256, NE], F32)
        wgt = _alloc(es, nc, "H_wgt", [128, 256, NE], F32)
        Pin = _alloc(es, nc, "H_Pin", [128, NE, 256], F32)
        idx = _alloc(es, nc, "H_idx", [128, NE, 256], U32)
        zer = _alloc(es, nc, "H_zer", [128, 256], F32)
        cnt = _alloc(es, nc, "H_cnt", [128, NE], F32)
        offs = _alloc(es, nc, "H_offs", [128, NE], F32)
        sut = _alloc(es, nc, "H_sut", [128, 128], F32)
        kb.dma("sp", sut.t[:], I["sut"][:, :], writes=[sut])
        kb.op("dve", lambda e: e.memset(zer.t[:], 0.0), writes=[zer])
        kb.op("dve", lambda e: e.tensor_tensor(out=M.t[:], in0=A.t[:], in1=lo.t[:, :].unsqueeze(1).broadcast_to([128, 256, NE]), op=ALU.is_gt),
              reads=[A, lo], writes=[M])
        kb.op("dve", lambda e: e.tensor_tensor(out=wgt.t[:], in0=A.t[:], in1=M.t[:], op=ALU.mult), reads=[A, M], writes=[wgt])
        Mt = M.t[:].rearrange("p j e -> p e j")
        for ex in range(NE):
            kb.op("dve", lambda e, ex=ex: e.tensor_tensor_scan(out=Pin.t[:, ex, :], data0=Mt[:, ex, :], data1=zer.t[:], initial=0.0, op0=ALU.add, op1=ALU.add),
                  reads=[M, zer], writes=[Pin])
        kb.op("dve", lambda e: e.tensor_copy(out=cnt.t[:, :].unsqueeze(2), in_=Pin.t[:, :, 255:256]), reads=[Pin], writes=[cnt])
        kb.op("pe", lambda e: e.matmul(pt.t[:], sut.t[:], cnt.t[:], start=True, stop=True), reads=[sut, cnt], writes=[pt])
        kb.op("dve", lambda e: e.tensor_scalar(out=offs.t[:], in0=pt.t[:], scalar1=-BIG, scalar2=None, op0=ALU.add), reads=[pt], writes=[offs])
        kb.op("dve", lambda e: e.tensor_tensor(out=Pin.t[:], in0=Pin.t[:], in1=Mt, op=ALU.subtract), reads=[Pin, M], writes=[Pin])
        kb.op("dve", lambda e: e.tensor_tensor(out=Pin.t[:], in0=Pin.t[:], in1=offs.t[:, :].unsqueeze(2).broadcast_to([128, NE, 256]), op=ALU.add),
              reads=[Pin, offs], writes=[Pin])
        kb.op("dve", lambda e: e.tensor_tensor(out=Pin.t[:], in0=Pin.t[:], in1=Mt, op=ALU.mult), reads=[Pin, M], writes=[Pin])
        kb.op("dve", lambda e: e.tensor_scalar(out=Pin.t[:], in0=Pin.t[:], scalar1=BIG, scalar2=None, op0=ALU.add), reads=[Pin], writes=[Pin])
        kb.op("dve", lambda e: e.tensor_copy(out=idx.t[:], in_=Pin.t[:]), reads=[Pin], writes=[idx])
        for ex in range(NE):
            kb.dma("pool", g.G[ex][:, :], I["zrows"][:, :], writes=[g.Gbuf[ex]])
        xj = [_alloc(es, nc, "H_xj%d" % i, [128, ROW], BF16) for i in range(4)]
        h2v = S["h2r"].rearrange("(p j) d -> p j d", j=256)
        for j in range(256):
            X = xj[j % 4]
            kb.dma("sp", X.t[:, 0:D], h2v[:, j, :], reads=[g.DBall["h2r"]], writes=[X])
            with nc.allow_non_contiguous_dma(reason="token ids"):
                kb.dma("sp", X.t[:, D + 32:D + 34].bitcast(F32), I["tokid"][:, j:j + 1], writes=[X])
            kb.op("dve", lambda e, j=j: e.tensor_copy(out=X.t[:, D:D + 32].bitcast(F32), in_=wgt.t[:, j, :]), reads=[wgt], writes=[X])
            for ex in range(NE):
                kb.idma(reads=[X, idx], writes=[g.Gbuf[ex]], out=g.G[ex][:, :],
                        out_offset=bass.IndirectOffsetOnAxis(ap=idx.t[:, ex, j:j + 1], axis=0), in_=X.t[:, :], in_offset=None,
                        bounds_check=CAPK - 1, oob_is_err=False)


def stage_I2(g, l):
    nc, kb, I, S = g.nc, g.kb, g.I, g.S
    y = g.y
    NB = 256
    NFC = (DFF + 127) // 128
    with ExitStack() as es:
        wgu = _alloc(es, nc, "I_wgu", [128, 8, 2 * DFF], BF16)
        wd = _alloc(es, nc, "I_wd", [128, NFC, D], BF16)
        Gt = [_alloc(es, nc, "I_G%d" % i, [128, ROW], BF16) for i in range(4)]
        h2 = [_alloc(es, nc, "I_h2%d" % i, [128, 8, NB], BF16) for i in range(2)]
        aT = _alloc(es, nc, "I_aT", [128, NFC, NB], BF16)
        sgt = [_alloc(es, nc, "I_sg%d" % i, [128, NB], F32) for i in range(3)]
        xo = [_alloc(es, nc, "I_x%d" % i, [128, D], F32) for i in range(2)]
        ptr = _alloc(es, nc, "I_ptr", [128, 8, 128], BF16, psum=True)
        pg = [_alloc(es, nc, "I_pg%d" % i, [128, NB], F32, psum=True) for i in range(2)]
        pu = [_alloc(es, nc, "I_pu%d" % i, [128, NB], F32, psum=True) for i in range(2)]
        pq = [_alloc(es, nc, "I_pq%d" % i, [128, 512], F32, psum=True) for i in range(2)]
        n = {"h": 0, "f": 0, "x": 0, "q": 0, "g": 0}
        nblk = int(os.environ.get("KBLK", CAPK // NB))
        for ex in range(NE):
            for kc in range(8):
                kb.dma("pool", wgu.t[:, kc, :], I["w_gate_up"][l, ex, kc * 128:(kc + 1) * 128, :], writes=[wgu])
            for fc in range(NFC):
                fw = min(128, DFF - fc * 128)
                kb.dma("pool", wd.t[0:fw, fc, :], I["w_down"][l, ex, fc * 128:fc * 128 + fw, :], writes=[wd])
            for bk in range(nblk):
                s0 = bk * NB
                hh = h2[n["h"] % 2]
                n["h"] += 1
                gts = []
                for tb in range(NB // 128):
                    G_ = Gt[n["g"] % 4]
                    n["g"] += 1
                    kb.dma("sp", G_.t[:], g.G[ex][s0 + tb * 128:s0 + (tb + 1) * 128, :], reads=[g.Gbuf[ex]], writes=[G_])
                    for kc in range(8):
                        kb.op("pe", lambda e, kc=kc, G_=G_: e.transpose(out=ptr.t[:, kc, :], in_=G_.t[:, kc * 128:(kc + 1) * 128], identity=g.ident_bf.t[:]),
                              reads=[G_, g.ident_bf], writes=[ptr])
                    kb.op("act", lambda e, tb=tb: e.copy(out=hh.t[:, :, tb * 128:(tb + 1) * 128], in_=ptr.t[:]), reads=[ptr], writes=[hh])
                    gts.append(G_)
                for fc in range(NFC):
                    fw = min(128, DFF - fc * 128)
                    p_g = pg[n["f"] % 2]
                    p_u = pu[n["f"] % 2]
                    s_ = sgt[n["f"] % 3]
                    n["f"] += 1
                    for kc in range(8):
                        kb.op("pe", lambda e, kc=kc: e.matmul(p_g.t[0:fw, :], wgu.t[:, kc, fc * 128:fc * 128 + fw], hh.t[:, kc, :], start=(kc == 0), stop=(kc == 7)),
                              reads=[wgu, hh], writes=[p_g])
                    for kc in range(8):
                        kb.op("pe", lambda e, kc=kc: e.matmul(p_u.t[0:fw, :], wgu.t[:, kc, DFF + fc * 128:DFF + fc * 128 + fw], hh.t[:, kc, :], start=(kc == 0), stop=(kc == 7)),
                              reads=[wgu, hh], writes=[p_u])
                    kb.op("act", lambda e: e.activation(out=s_.t[0:fw, :], in_=p_g.t[0:fw, :], func=AF.Sigmoid), reads=[p_g], writes=[s_])
                    kb.op("dve", lambda e: e.tensor_tensor(out=s_.t[0:fw, :], in0=p_g.t[0:fw, :], in1=s_.t[0:fw, :], op=ALU.mult), reads=[p_g, s_], writes=[s_])
                    kb.op("dve", lambda e: e.tensor_tensor(out=aT.t[0:fw, fc, :], in0=p_u.t[0:fw, :], in1=s_.t[0:fw, :], op=ALU.mult), reads=[p_u, s_], writes=[aT])
                for tb in range(NB // 128):
                    G_ = gts[tb]
                    x = xo[n["x"] % 2]
                    n["x"] += 1
                    gate = G_.t[:, D:D + 32].bitcast(F32)[:, ex:ex + 1]
                    for hf in range(2):
                        p = pq[n["q"] % 2]
                        n["q"] += 1
                        for fc in range(NFC):
                            fw = min(128, DFF - fc * 128)
                            kb.op("pe", lambda e, fc=fc, fw=fw: e.matmul(p.t[:], aT.t[0:fw, fc, tb * 128:(tb + 1) * 128], wd.t[0:fw, fc, hf * 512:(hf + 1) * 512],
                                                                        start=(fc == 0), stop=(fc == NFC - 1)), reads=[aT, wd], writes=[p])
                        kb.op("dve", lambda e: e.tensor_scalar(out=x.t[:, hf * 512:(hf + 1) * 512], in0=p.t[:], scalar1=gate, scalar2=None, op0=ALU.mult),
                              reads=[p, G_], writes=[x])
                    kb.idma(reads=[x, G_], writes=[g.ybuf], out=y[:, :],
                            out_offset=bass.IndirectOffsetOnAxis(ap=G_.t[:, D + 32:D + 34].bitcast(U32), axis=0), in_=x.t[:, :], in_offset=None,
                            bounds_check=GT - 1, oob_is_err=False, compute_op=ALU.add)


def host_consts():
    c = {}
    c["ident"] = np.eye(128, dtype=np.float32)
    c["ones"] = np.ones((128, 128), np.float32)
    bo = np.zeros((128, 128), np.float32)
    bo[:64, :64] = 1
    bo[64:, 64:] = 1
    c["bones"] = bo
    s = np.arange(128)[:, None]
    t = np.arange(128)[None, :]
    same = (s // 64) == (t // 64)
    c["trif"] = (same & (s <= t)).astype(np.float32)
    c["trib"] = (same & (s >= t)).astype(np.float32)
    return c


def host_smask(typ):
    m = np.zeros((8, 16), np.float32)
    for i, r in enumerate(range(60, 68)):
        r0 = na_window(r, typ)
        for j, rr in enumerate(range(56, 72)):
            m[i, j] = 0.0 if (r0 <= rr < r0 + 8) else NEG
    return np.ascontiguousarray(np.broadcast_to(m.reshape(1, 128), (128, 128))).astype(np.float32)


def host_bt(rel_bias):
    out = np.zeros((DEPTH, 128, 4, 31, 64), np.float32)
    qc = np.arange(64)
    c0 = np.clip(qc - 8, 0, 48)
    kc = np.arange(64)
    inwin = (kc[:, None] >= c0[None, :]) & (kc[:, None] < c0[None, :] + 16)
    dc = np.clip(kc[:, None] - qc[None, :] + 15, 0, 30)
    for c in range(4):
        for hh in range(2):
            h = 2 * c + hh
            for dr in range(15):
                gath = rel_bias[:, h, dr][:, dc]
                out[:, hh * 64:(hh + 1) * 64, c, dr + 8, :] = np.where(inwin[None], gath, np.float32(NEG))
    return np.ascontiguousarray(out.reshape(DEPTH, 128, 4 * 31 * 64))


_NC_CACHE = {}


def _run(inp, cores):
    if "nc" not in _NC_CACHE:
        _NC_CACHE["nc"] = build_program()
    nc = _NC_CACHE["nc"]
    used = list(nc._k_inputs.keys())
    consts = host_consts()
    bt = host_bt(np.asarray(inp["na_rel_bias"], dtype=np.float32))
    in_maps = []
    for c in cores:
        m = {}
        if c == 0:
            m["x"] = np.ascontiguousarray(inp["x_prompt"].reshape(GT, D))
            m["mem"] = np.ascontiguousarray(inp["mem_prompt"].reshape(NCH, 2, 256, D))
            typ = 0
        else:
            m["x"] = np.ascontiguousarray(inp["x_sample"].reshape(GT, D))
            m["mem"] = np.ascontiguousarray(np.stack([inp["mem_sample"], inp["mem_sample"]], axis=1))
            typ = 1
        m["cflag"] = np.full((128, 1), float(typ), np.float32)
        m["smask"] = host_smask(typ)
        m["bt"] = bt
        m["tokid"] = (np.arange(128, dtype=np.uint32)[:, None] * 256 + np.arange(256, dtype=np.uint32)[None, :]).view(np.float32)
        m["zrows"] = np.zeros((CAPK, ROW), np.float32)
        m["sut"] = (np.arange(128)[:, None] < np.arange(128)[None, :]).astype(np.float32)
        m.update(consts)
        mm = {}
        for k in used:
            mm[k] = np.ascontiguousarray(m[k] if k in m else inp[k], dtype=np.float32)
        in_maps.append(mm)
    res = run_bass_kernel_spmd(nc, in_maps, core_ids=list(range(len(cores))))
    return res


def kernel(**inputs):
    inp = {k: np.asarray(v) for k, v in inputs.items()}
    res = _run(inp, [0, 1])
    ys = [np.asarray(r["y"]) for r in res.results]
    y_prompt = ys[0].reshape(8, 4096, D).astype(np.float32)
    y_sample = ys[1].reshape(4, 8192, D).astype(np.float32)
    return (y_prompt, y_sample)
```

```python
import os
import numpy as np
import concourse.bass as bass
import concourse.mybir as mybir
from concourse.bass_utils import run_bass_kernel_spmd
from contextlib import ExitStack

F32 = mybir.dt.float32
BF16 = mybir.dt.bfloat16
U32 = mybir.dt.uint32
AF = mybir.ActivationFunctionType
ALU = mybir.AluOpType
AX = mybir.AxisListType

T = 8192
ST = 512
NST = T // ST
NT = T // 128
D = 1024
DIN = 8192
DEPTH = 2
NE = 16
DFF = 2752
CAPK = 4096
EPS = 1e-6
NEG = -30000.0
NBIS = 26
NCH = 4
GT = NCH * T
ROW = 1060
BIG = 1.0e6


class Buf:
    __slots__ = ("w", "r")

    def __init__(self):
        self.w = None
        self.r = {}


class TT:
    __slots__ = ("t", "b")

    def __init__(self, t):
        self.t = t
        self.b = Buf()


class KB:
    def __init__(self, nc, es):
        self.nc = nc
        self.E = {"pe": nc.tensor, "dve": nc.vector, "act": nc.scalar, "pool": nc.gpsimd, "sp": nc.sync}
        self.semh = {}
        self.cnt = {}
        for e in ("pe", "dve", "act", "pool"):
            self.semh[e] = es.enter_context(nc.semaphore("s_" + e))
            self.cnt[e] = 0
        self.nd = 26
        self.qsems = {"sp": list(range(0, 16)), "pool": list(range(16, 26))}
        self.qnext = {"sp": 0, "pool": 0}
        self.dcnt = [0] * self.nd
        for i in range(self.nd):
            self.semh[("d", i)] = es.enter_context(nc.semaphore("d%d" % i))
        self.seen = {e: {} for e in self.E}
        self.ninst = 0
        self.bregs = {}

    def _deps(self, reads, writes):
        deps = {}
        for b in reads:
            if b.w is not None and deps.get(b.w[0], 0) < b.w[1]:
                deps[b.w[0]] = b.w[1]
        for b in writes:
            if b.w is not None and deps.get(b.w[0], 0) < b.w[1]:
                deps[b.w[0]] = b.w[1]
            for k, v in b.r.items():
                if deps.get(k, 0) < v:
                    deps[k] = v
        return deps

    def _wait(self, eng, deps):
        E = self.E[eng]
        seen = self.seen[eng]
        for k, v in deps.items():
            if eng == "pe" and k == "pe":
                continue
            if seen.get(k, 0) >= v:
                continue
            E.wait_ge(self.semh[k], v)
            seen[k] = v

    def op(self, eng, fn, reads=(), writes=()):
        reads = [x.b if isinstance(x, TT) else x for x in reads]
        writes = [x.b if isinstance(x, TT) else x for x in writes]
        self._wait(eng, self._deps(reads, writes))
        inst = fn(self.E[eng])
        self.cnt[eng] += 1
        c = self.cnt[eng]
        inst.then_inc(self.semh[eng], 1)
        for b in reads:
            b.r[eng] = c
        for b in writes:
            b.w = (eng, c)
            b.r = {}
        self.ninst += 1
        return inst

    def dma(self, q, out, in_, reads=(), writes=(), **kw):
        reads = [x.b if isinstance(x, TT) else x for x in reads]
        writes = [x.b if isinstance(x, TT) else x for x in writes]
        i = self.next_dsem(q)
        key = ("d", i)
        deps = self._deps(reads, writes)
        if self.dcnt[i] > 0 and deps.get(key, 0) < self.dcnt[i]:
            deps[key] = self.dcnt[i]
        self._wait(q, deps)
        inst = self.E[q].dma_start(out=out, in_=in_, **kw)
        self.dcnt[i] += 16
        inst.then_inc(self.semh[key], 16)
        for b in reads:
            b.r[key] = self.dcnt[i]
        for b in writes:
            b.w = (key, self.dcnt[i])
            b.r = {}
        self.ninst += 1
        return inst

    def idma(self, reads=(), writes=(), **kw):
        reads = [x.b if isinstance(x, TT) else x for x in reads]
        writes = [x.b if isinstance(x, TT) else x for x in writes]
        i = self.next_dsem("pool")
        key = ("d", i)
        deps = self._deps(reads, writes)
        if self.dcnt[i] > 0 and deps.get(key, 0) < self.dcnt[i]:
            deps[key] = self.dcnt[i]
        self._wait("pool", deps)
        bc = kw.pop("bounds_check")
        if bc not in self.bregs:
            self.bregs[bc] = self.nc.gpsimd.to_reg(bc)
        inst = self.nc.gpsimd.indirect_dma_start(bounds_check=self.bregs[bc], **kw)
        self.dcnt[i] += 16
        inst.then_inc(self.semh[key], 16)
        for b in reads:
            b.r[key] = self.dcnt[i]
        for b in writes:
            b.w = (key, self.dcnt[i])
            b.r = {}
        self.ninst += 1
        return inst

    def next_dsem(self, q):
        lst = self.qsems[q]
        i = lst[self.qnext[q] % len(lst)]
        self.qnext[q] += 1
        return i

    def barrier(self):
        allk = {e: self.cnt[e] for e in self.cnt if self.cnt[e] > 0}
        for i in range(self.nd):
            if self.dcnt[i] > 0:
                allk[("d", i)] = self.dcnt[i]
        for eng in self.E:
            self._wait(eng, {k: v for k, v in allk.items() if k != eng})


class Ctx:
    pass


_UID = [0]


def _alloc(es, nc, name, shape, dt, psum=False):
    f = nc.psum_tensor if psum else nc.sbuf_tensor
    _UID[0] += 1
    return TT(es.enter_context(f("t%d_%s" % (_UID[0], name), shape, dt)))


def build_program():
    nc = bass.Bass("TRN2", target_bir_lowering=False)
    g = Ctx()
    g.nc = nc

    debug = os.environ.get("KDEBUG", "").split(",")

    def dscr(name, shape, dt):
        dbg = ("1" in debug) or (name in debug)
        return nc.dram_tensor(name, shape, dt, kind=("ExternalOutput" if dbg else "Internal")).ap()

    ishapes = {
        "x": [GT, D], "mem": [NCH, 2, 256, D], "tokid": [128, 256], "zrows": [CAPK, ROW], "sut": [128, 128], "cflag": [128, 1], "smask": [128, 128], "bt": [DEPTH, 128, 4 * 31 * 64],
        "ident": [128, 128], "ones": [128, 128], "bones": [128, 128], "trif": [128, 128], "trib": [128, 128],
        "norm_mix": [DEPTH, D], "w_in": [DEPTH, D, DIN], "na_q_norm": [DEPTH, 64], "na_k_norm": [DEPTH, 64],
        "w_na_br": [DEPTH, 512, D], "hg_lb": [2, DEPTH, 512], "hg_out_norm": [DEPTH, 128],
        "w_hg_br": [DEPTH, 512, D], "conv_w": [DEPTH, 31, 512], "conv_b": [DEPTH, 512],
        "conv_ln_g": [DEPTH, 512], "conv_ln_b": [DEPTH, 512], "w_cv_br": [DEPTH, 512, D],
        "w_out": [DEPTH, D, D], "norm_mem": [DEPTH, D], "mem_kv_norm": [DEPTH, D], "wq_mem": [DEPTH, D, 512],
        "wkv_mem": [DEPTH, D, D], "mem_q_norm": [DEPTH, 128], "mem_k_norm": [DEPTH, 128],
        "wo_mem": [DEPTH, 512, D], "norm_ffn": [DEPTH, D], "w_router": [DEPTH, D, NE],
        "w_gate_up": [DEPTH, NE, D, 2 * DFF], "w_down": [DEPTH, NE, DFF, D],
    }

    class LazyIn(dict):
        def __missing__(self, k):
            ap = nc.dram_tensor(k, ishapes[k], F32, kind="ExternalInput").ap()
            self[k] = ap
            return ap

    I = LazyIn()
    y = nc.dram_tensor("y", [GT, D], F32, kind="ExternalOutput").ap()
    S = {}
    S["qT"] = dscr("qT_d", [512, T], BF16)
    S["kT"] = dscr("kT_d", [512, T], BF16)
    S["v"] = dscr("v_d", [T, 512], BF16)
    S["qh"] = dscr("qh_d", [512, T], BF16)
    S["lf"] = dscr("lf_d", [2, 512, T], F32)
    S["kk"] = dscr("kk_d", [2, 512, T], BF16)
    S["vh"] = dscr("vh_d", [T, 512], BF16)
    S["gs"] = dscr("gs_d", [512, T], BF16)
    S["u"] = dscr("u_d", [512, T], F32)
    S["sg"] = dscr("sg_d", [3072, T], BF16)
    S["naT"] = dscr("naT_d", [512, T], BF16)
    S["of"] = dscr("of_d", [512, T], F32)
    S["hgT"] = dscr("hgT_d", [512, T], BF16)
    S["cvT"] = dscr("cvT_d", [512, T], BF16)
    S["h2T"] = dscr("h2T_d", [D, T], BF16)
    S["affall"] = dscr("affall_d", [GT, NE], F32)
    S["h2r"] = dscr("h2r_d", [GT, D], BF16)
    g.G = [nc.dram_tensor("G%d" % e, [CAPK, ROW], BF16, kind="Internal").ap() for e in range(NE)]
    g.Gbuf = [Buf() for _ in range(NE)]
    g.ybuf = Buf()
    g.I, g.S, g.y = I, S, y
    nc._k_inputs = I
    nc._k_scratch = S
    g.DBall = {k: Buf() for k in ["affall", "h2r"]}

    g.lim = int(os.environ.get("KLIM", "0"))
    with ExitStack() as es:
        kb = KB(nc, es)
        g.kb = kb
        g.ident_bf = _alloc(es, nc, "ident_bf", [128, 128], BF16)
        g.ident_f = _alloc(es, nc, "ident_f", [128, 128], F32)
        g.ones_f = _alloc(es, nc, "ones_f", [128, 128], F32)
        g.ones_bf = _alloc(es, nc, "ones_bf", [128, 128], BF16)
        g.bones_f = _alloc(es, nc, "bones_f", [128, 128], F32)
        g.trif = _alloc(es, nc, "trif", [128, 128], F32)
        g.trib = _alloc(es, nc, "trib", [128, 128], F32)
        g.cflag = _alloc(es, nc, "cflag_sb", [128, 1], F32)
        g.wg = _alloc(es, nc, "wg_sb", [128, NT, NE], F32)
        kb.dma("pool", g.ident_bf.t[:], I["ident"][:, :], writes=[g.ident_bf])
        kb.dma("sp", g.ident_f.t[:], I["ident"][:, :], writes=[g.ident_f])
        kb.dma("sp", g.ones_f.t[:], I["ones"][:, :], writes=[g.ones_f])
        kb.dma("pool", g.ones_bf.t[:], I["ones"][:, :], writes=[g.ones_bf])
        kb.dma("sp", g.bones_f.t[:], I["bones"][:, :], writes=[g.bones_f])
        kb.dma("sp", g.trif.t[:], I["trif"][:, :], writes=[g.trif])
        kb.dma("sp", g.trib.t[:], I["trib"][:, :], writes=[g.trib])
        kb.dma("sp", g.cflag.t[:], I["cflag"][:, :], writes=[g.cflag])

        only = os.environ.get("KONLY", "")
        only = set(only.split(",")) if only else None
        nlayers = int(os.environ.get("KLAYERS", DEPTH))
        nch = int(os.environ.get("KCHUNKS", NCH))
        for l in range(nlayers):
            for k in range(nch):
                g.DB = {kk: [Buf() for _ in range(NST)] for kk in list(S.keys()) + ["y", "aff"]}
                xsrc = (I["x"] if l == 0 else y)[k * T:(k + 1) * T, :]
                g.yc = y[k * T:(k + 1) * T, :]
                g.memc = I["mem"][k]
                g.affc = S["affall"][k * T:(k + 1) * T, :]
                g.h2rc = S["h2r"][k * T:(k + 1) * T, :]
                plan = [("A0", lambda: stage_A(g, l, xsrc, 0)), ("A1", lambda: stage_A(g, l, xsrc, 1)), ("B", lambda: stage_B(g, l)),
                        ("C", lambda: stage_C(g, l)), ("D", lambda: stage_D(g, l)), ("E", lambda: stage_E(g, l, xsrc)),
                        ("F", lambda: stage_F(g, l))]
                for nm, fn in plan:
                    if only is not None and nm not in only:
                        continue
                    fn()
                    kb.barrier()
                print("stage", l, k, "A-F ninst", kb.ninst, flush=True)
            for nm, fn in (("H", lambda: stage_H2(g, l)), ("I", lambda: stage_I2(g, l))):
                if only is not None and nm not in only:
                    continue
                fn()
                kb.barrier()
                print("stage", l, nm, "ninst", kb.ninst, flush=True)
        kb.barrier()
    return nc


def load_bcast_row(g, es, name, src_row, n):
    t = _alloc(es, g.nc, name, [128, n], F32)
    g.kb.dma("sp", t.t[:], src_row.partition_broadcast(128), writes=[t])
    return t


def load_col(g, es, name, src_vec, nchunks):
    t = _alloc(es, g.nc, name, [128, nchunks], F32)
    with g.nc.allow_non_contiguous_dma(reason="tiny param load"):
        g.kb.dma("sp", t.t[:], src_vec.rearrange("(c p) -> p c", p=128), writes=[t])
    return t


def rms_rows(g, xa, ss, rstd, junk, ntb):
    kb = g.kb
    for tb in range(ntb):
        kb.op("act", lambda e, tb=tb: e.activation(out=junk.t[:], in_=xa[tb].t[:], func=AF.Square,
                                                  accum_out=ss.t[:, tb:tb + 1]),
              reads=[xa[tb]], writes=[junk, ss])
    kb.op("act", lambda e: e.activation(out=rstd.t[:, 0:ntb], in_=ss.t[:, 0:ntb], func=AF.Ln, scale=1.0 / D, bias=EPS),
          reads=[ss], writes=[rstd])
    kb.op("act", lambda e: e.activation(out=rstd.t[:, 0:ntb], in_=rstd.t[:, 0:ntb], func=AF.Exp, scale=-0.5),
          reads=[rstd], writes=[rstd])


def norm_transpose(g, src, st, gbc, W, out_hT, also_store=None):
    kb = g.kb
    t0 = st * ST
    for tb in range(4):
        kb.dma("sp", W.xa[tb].t[:], src[t0 + tb * 128:t0 + (tb + 1) * 128, :],
               reads=[g.DB["y"][st]], writes=[W.xa[tb]])
    rms_rows(g, W.xa, W.ss, W.rstd, W.junk, 4)
    for tb in range(4):
        kb.op("dve", lambda e, tb=tb: e.scalar_tensor_tensor(out=W.hb[tb].t[:], in0=W.xa[tb].t[:],
                                                            scalar=W.rstd.t[:, tb:tb + 1], in1=gbc.t[:],
                                                            op0=ALU.mult, op1=ALU.mult),
              reads=[W.xa[tb], W.rstd, gbc], writes=[W.hb[tb]])
        for kc in range(8):
            kb.op("pe", lambda e, tb=tb, kc=kc: e.transpose(out=W.ptr.t[:, kc, :], in_=W.hb[tb].t[:, kc * 128:(kc + 1) * 128],
                                                           identity=g.ident_bf.t[:]),
                  reads=[W.hb[tb], g.ident_bf], writes=[W.ptr])
        kb.op("act", lambda e, tb=tb: e.copy(out=out_hT.t[:, :, tb * 128:(tb + 1) * 128], in_=W.ptr.t[:]),
              reads=[W.ptr], writes=[out_hT])


class NormWork:
    def __init__(self, g, es, pfx):
        nc = g.nc
        self.xa = [_alloc(es, nc, pfx + "xa%d" % i, [128, D], F32) for i in range(4)]
        self.hb = [_alloc(es, nc, pfx + "hb%d" % i, [128, D], BF16) for i in range(2)] * 2
        self.ss = _alloc(es, nc, pfx + "ss", [128, 4], F32)
        self.rstd = _alloc(es, nc, pfx + "rstd", [128, 4], F32)
        self.junk = _alloc(es, nc, pfx + "junk", [128, D], BF16)
        self.ptr = _alloc(es, nc, pfx + "ptr", [128, 8, 128], BF16, psum=True)


def stage_A(g, l, xsrc, half):
    nc, kb, I, S = g.nc, g.kb, g.I, g.S
    with ExitStack() as es:
        W = NormWork(g, es, "A")
        gbc = load_bcast_row(g, es, "A_gbc", I["norm_mix"][l, :], D)
        hT = _alloc(es, nc, "A_hT", [128, 8, ST], BF16)
        wsb = _alloc(es, nc, "A_w", [128, 8, 4096], BF16)
        for kc in range(8):
            kb.dma("pool", wsb.t[:, kc, :], I["w_in"][l, kc * 128:(kc + 1) * 128, half * 4096:(half + 1) * 4096],
                   writes=[wsb])
        ps = [_alloc(es, nc, "A_ps%d" % i, [128, ST], F32, psum=True) for i in range(4)]
        ps2 = [_alloc(es, nc, "A_ps2%d" % i, [128, ST], F32, psum=True) for i in range(2)]
        f32t = [_alloc(es, nc, "A_f%d" % i, [128, ST], F32) for i in range(6)]
        bft = [_alloc(es, nc, "A_b%d" % i, [128, ST], BF16) for i in range(6)]
        cnt = {"ps": 0, "ps2": 0, "f": 0, "b": 0}

        def nxt(lst, key):
            r = lst[cnt[key] % len(lst)]
            cnt[key] += 1
            return r

        if half == 0:
            qg = _alloc(es, nc, "A_qg", [128, 2], F32)
            with nc.allow_non_contiguous_dma(reason="tiny"):
                for hh in range(2):
                    kb.dma("sp", qg.t[hh * 64:(hh + 1) * 64, 0:1], I["na_q_norm"][l, :].rearrange("(p o) -> p o", o=1), writes=[qg])
                    kb.dma("sp", qg.t[hh * 64:(hh + 1) * 64, 1:2], I["na_k_norm"][l, :].rearrange("(p o) -> p o", o=1), writes=[qg])
            kb.op("dve", lambda e: e.tensor_scalar(out=qg.t[:, 0:1], in0=qg.t[:, 0:1], scalar1=0.125, scalar2=None, op0=ALU.mult),
                  reads=[qg], writes=[qg])
            lbv = _alloc(es, nc, "A_lb", [128, 8], F32)
            oml = _alloc(es, nc, "A_oml", [128, 8], F32)
            noml = _alloc(es, nc, "A_noml", [128, 8], F32)
            if l == 0:
                kb.op("dve", lambda e: e.memset(lbv.t[:], 0.0), writes=[lbv])
            else:
                b0 = _alloc(es, nc, "A_b0", [128, 8], F32)
                b1 = _alloc(es, nc, "A_b1", [128, 8], F32)
                with nc.allow_non_contiguous_dma(reason="tiny"):
                    for d_ in range(2):
                        kb.dma("sp", b0.t[:, d_ * 4:(d_ + 1) * 4], I["hg_lb"][d_, 0, :].rearrange("(c p) -> p c", p=128), writes=[b0])
                        kb.dma("sp", b1.t[:, d_ * 4:(d_ + 1) * 4], I["hg_lb"][d_, 1, :].rearrange("(c p) -> p c", p=128), writes=[b1])
                kb.op("dve", lambda e: e.tensor_tensor(out=b1.t[:], in0=b1.t[:], in1=b0.t[:], op=ALU.subtract), reads=[b0, b1], writes=[b1])
                kb.op("act", lambda e: e.activation(out=lbv.t[:], in_=b1.t[:], func=AF.Sigmoid), reads=[b1], writes=[lbv])
            kb.op("dve", lambda e: e.tensor_scalar(out=oml.t[:], in0=lbv.t[:], scalar1=-1.0, scalar2=1.0, op0=ALU.mult, op1=ALU.add),
                  reads=[lbv], writes=[oml])
            kb.op("dve", lambda e: e.tensor_scalar(out=noml.t[:], in0=oml.t[:], scalar1=-1.0, scalar2=None, op0=ALU.mult),
                  reads=[oml], writes=[noml])

        def proj_fm(cb):
            p = nxt(ps, "ps")
            for kc in range(8):
                kb.op("pe", lambda e, kc=kc: e.matmul(p.t[:], wsb.t[:, kc, cb * 128:(cb + 1) * 128], hT.t[:, kc, :],
                                                     start=(kc == 0), stop=(kc == 7)),
                      reads=[wsb, hT], writes=[p])
            return p

        def store(q, dst, src_tt, dbufs):
            kb.dma(q, dst, src_tt.t[:], reads=[src_tt], writes=dbufs)

        for st in range(NST):
            t0 = st * ST
            norm_transpose(g, xsrc, st, gbc, W, hT)
            if half == 0:
                for cb in range(8):
                    p = proj_fm(cb)
                    sq = nxt(f32t, "f")
                    kb.op("act", lambda e: e.activation(out=sq.t[:], in_=p.t[:], func=AF.Square), reads=[p], writes=[sq])
                    p2 = nxt(ps2, "ps2")
                    kb.op("pe", lambda e: e.matmul(p2.t[:], g.bones_f.t[:], sq.t[:], start=True, stop=True),
                          reads=[g.bones_f, sq], writes=[p2])
                    rs = nxt(f32t, "f")
                    kb.op("act", lambda e: e.activation(out=rs.t[:], in_=p2.t[:], func=AF.Ln, scale=1.0 / 64, bias=EPS),
                          reads=[p2], writes=[rs])
                    kb.op("act", lambda e: e.activation(out=rs.t[:], in_=rs.t[:], func=AF.Exp, scale=-0.5), reads=[rs], writes=[rs])
                    ob = nxt(bft, "b")
                    gi = 0 if cb < 4 else 1
                    kb.op("dve", lambda e: e.scalar_tensor_tensor(out=ob.t[:], in0=p.t[:], scalar=qg.t[:, gi:gi + 1], in1=rs.t[:],
                                                                 op0=ALU.mult, op1=ALU.mult),
                          reads=[p, qg, rs], writes=[ob])
                    dn = "qT" if cb < 4 else "kT"
                    c = cb % 4
                    store("sp", S[dn][c * 128:(c + 1) * 128, t0:t0 + ST], ob, [g.DB[dn][st]])
                for (c0, dn) in ((1024, "v"), (3072, "vh")):
                    for tb in range(4):
                        p = nxt(ps, "ps")
                        for kc in range(8):
                            kb.op("pe", lambda e, kc=kc: e.matmul(p.t[:], hT.t[:, kc, tb * 128:(tb + 1) * 128],
                                                                 wsb.t[:, kc, c0:c0 + 512], start=(kc == 0), stop=(kc == 7)),
                                  reads=[wsb, hT], writes=[p])
                        ob = nxt(bft, "b")
                        kb.op("act", lambda e: e.copy(out=ob.t[:], in_=p.t[:]), reads=[p], writes=[ob])
                        store("sp", S[dn][t0 + tb * 128:t0 + (tb + 1) * 128, :], ob, [g.DB[dn][st]])
                for cb in list(range(12, 16)) + list(range(28, 32)):
                    p = proj_fm(cb)
                    sgm = nxt(f32t, "f")
                    kb.op("act", lambda e: e.activation(out=sgm.t[:], in_=p.t[:], func=AF.Sigmoid), reads=[p], writes=[sgm])
                    ob = nxt(bft, "b")
                    sc = (128.0 ** -0.5) if cb < 16 else 1.0
                    kb.op("dve", lambda e: e.scalar_tensor_tensor(out=ob.t[:], in0=p.t[:], scalar=sc, in1=sgm.t[:],
                                                                 op0=ALU.mult, op1=ALU.mult),
                          reads=[p, sgm], writes=[ob])
                    dn = "qh" if cb < 16 else "gs"
                    c = cb % 4
                    store("sp", S[dn][c * 128:(c + 1) * 128, t0:t0 + ST], ob, [g.DB[dn][st]])
                for cb in range(16, 24):
                    d_ = (cb - 16) // 4
                    h = cb % 4
                    j = d_ * 4 + h
                    p = proj_fm(cb)
                    sgm = nxt(f32t, "f")
                    kb.op("act", lambda e: e.activation(out=sgm.t[:], in_=p.t[:], func=AF.Sigmoid), reads=[p], writes=[sgm])
                    ff = nxt(f32t, "f")
                    kb.op("dve", lambda e: e.tensor_scalar(out=ff.t[:], in0=sgm.t[:], scalar1=oml.t[:, j:j + 1], scalar2=lbv.t[:, j:j + 1],
                                                          op0=ALU.mult, op1=ALU.add), reads=[sgm, oml, lbv], writes=[ff])
                    kb.op("pool", lambda e: e.tensor_scalar(out=ff.t[:], in0=ff.t[:], scalar1=1e-20, scalar2=None, op0=ALU.max), reads=[ff], writes=[ff])
                    kb.op("act", lambda e: e.activation(out=ff.t[:], in_=ff.t[:], func=AF.Ln), reads=[ff], writes=[ff])
                    store("sp", S["lf"][d_, h * 128:(h + 1) * 128, t0:t0 + ST], ff, [g.DB["lf"][st]])
                    ob = nxt(bft, "b")
                    kb.op("dve", lambda e: e.tensor_scalar(out=ob.t[:], in0=sgm.t[:], scalar1=noml.t[:, j:j + 1], scalar2=oml.t[:, j:j + 1],
                                                          op0=ALU.mult, op1=ALU.add), reads=[sgm, noml, oml], writes=[ob])
                    store("sp", S["kk"][d_, h * 128:(h + 1) * 128, t0:t0 + ST], ob, [g.DB["kk"][st]])
            else:
                for c in range(4):
                    pb = proj_fm(4 + c)
                    sgm = nxt(f32t, "f")
                    kb.op("act", lambda e: e.activation(out=sgm.t[:], in_=pb.t[:], func=AF.Sigmoid), reads=[pb], writes=[sgm])
                    pa = proj_fm(c)
                    uu = nxt(f32t, "f")
                    kb.op("dve", lambda e: e.tensor_tensor(out=uu.t[:], in0=pa.t[:], in1=sgm.t[:], op=ALU.mult), reads=[pa, sgm], writes=[uu])
                    store("sp", S["u"][c * 128:(c + 1) * 128, t0:t0 + ST], uu, [g.DB["u"][st]])
                for cb in range(8, 32):
                    p = proj_fm(cb)
                    ob = nxt(bft, "b")
                    kb.op("act", lambda e: e.activation(out=ob.t[:], in_=p.t[:], func=AF.Sigmoid), reads=[p], writes=[ob])
                    store("sp", S["sg"][(cb - 8) * 128:(cb - 7) * 128, t0:t0 + ST], ob, [g.DB["sg"][st]])


def na_window(r, typ):
    if typ == 0:
        base = (r // 64) * 64
        r0 = min(max((r % 64) - 4, 0), 56) + base
    else:
        r0 = min(max(r - 4, 0), 120)
    return r0


def stage_B(g, l):
    nc, kb, I, S = g.nc, g.kb, g.I, g.S
    NR = 24
    with ExitStack() as es:
        bt = _alloc(es, nc, "B_bt", [128, 4, 31, 64], F32)
        kb.dma("sp", bt.t[:], I["bt"][l].rearrange("p (c e q) -> p c e q", c=4, e=31), writes=[bt])
        smask = _alloc(es, nc, "B_sm", [128, 128], F32)
        kb.dma("sp", smask.t[:], I["smask"][:, :], writes=[smask])
        kbd = [_alloc(es, nc, "B_k%d" % i, [128, 4, 128], BF16) for i in range(NR)]
        vbd = [_alloc(es, nc, "B_v%d" % i, [128, 4, 130], BF16) for i in range(NR)]
        qr = [_alloc(es, nc, "B_q%d" % i, [128, 4, 64], BF16) for i in range(6)]
        for i in range(NR):
            kb.op("pool", lambda e, i=i: e.memset(kbd[i].t[:], 0.0), writes=[kbd[i]])
            kb.op("pool", lambda e, i=i: e.memset(vbd[i].t[:], 0.0), writes=[vbd[i]])
            kb.op("pool", lambda e, i=i: e.memset(vbd[i].t[0:64, :, 64:65], 1.0), writes=[vbd[i]])
            kb.op("pool", lambda e, i=i: e.memset(vbd[i].t[64:128, :, 129:130], 1.0), writes=[vbd[i]])
        pss = [_alloc(es, nc, "B_ps%d" % i, [128, 8, 64], F32, psum=True) for i in range(5)]
        pos = [_alloc(es, nc, "B_po%d" % i, [128, 512], F32, psum=True) for i in range(2)]
        ptt = _alloc(es, nc, "B_pt", [128, 4, 64], BF16, psum=True)
        sct = [_alloc(es, nc, "B_sc%d" % i, [128, 8, 64], F32) for i in range(4)]
        ptl = [_alloc(es, nc, "B_pT%d" % i, [128, 8, 64], BF16) for i in range(8)]
        rcp = [_alloc(es, nc, "B_rc%d" % i, [64, 4], F32) for i in range(4)]
        obf = [_alloc(es, nc, "B_ob%d" % i, [64, 512], BF16) for i in range(2)]
        outb = [_alloc(es, nc, "B_out%d" % i, [128, 4, ST], BF16) for i in range(2)]
        kTv = S["kT"].rearrange("(c p) t -> p c t", p=128)
        qTv = S["qT"].rearrange("(c p) t -> p c t", p=128)
        vv = S["v"].rearrange("t (c x) -> t c x", x=128)
        loaded = [-1]
        cn = {"ps": 0, "sc": 0, "pT": 0, "po": 0}

        def load_row(rr):
            sl = rr % NR
            st = (rr * 64) // ST
            cs = slice(rr * 64, rr * 64 + 64)
            with nc.allow_non_contiguous_dma(reason="row blocks"):
                kb.dma("sp", kbd[sl].t[0:64, :, 0:64], kTv[0:64, :, cs], reads=[g.DB["kT"][st]], writes=[kbd[sl]])
                kb.dma("sp", kbd[sl].t[64:128, :, 64:128], kTv[64:128, :, cs], reads=[g.DB["kT"][st]], writes=[kbd[sl]])
                kb.dma("pool", vbd[sl].t[0:64, :, 0:64], vv[cs, :, 0:64], reads=[g.DB["v"][st]], writes=[vbd[sl]])
                kb.dma("pool", vbd[sl].t[64:128, :, 65:129], vv[cs, :, 64:128], reads=[g.DB["v"][st]], writes=[vbd[sl]])

        for r in range(g.lim if g.lim else 128):
            special = 60 <= r <= 67
            if special:
                krows = list(range(56, 72))
            else:
                r0 = na_window(r, 1)
                krows = list(range(r0, r0 + 8))
            while loaded[0] < krows[-1]:
                loaded[0] += 1
                load_row(loaded[0])
            q = qr[r % len(qr)]
            with nc.allow_non_contiguous_dma(reason="row blocks"):
                kb.dma("sp", q.t[:], qTv[:, :, r * 64:(r + 1) * 64], reads=[g.DB["qT"][(r * 64) // ST]], writes=[q])
            groups = [krows[i:i + 8] for i in range(0, len(krows), 8)]
            pot = [pos[0], pos[1]]
            allpts = []
            for c in range(4):
                pts = []
                for gi, grp in enumerate(groups):
                    p = pss[cn["ps"] % len(pss)]
                    cn["ps"] += 1
                    for j, rr in enumerate(grp):
                        kb.op("pe", lambda e, j=j, rr=rr, p=p, c=c: e.matmul(p.t[:, j, :], kbd[rr % NR].t[:, c, :], q.t[:, c, :], start=True, stop=True),
                              reads=[kbd[rr % NR], q], writes=[p])
                    sc = sct[cn["sc"] % len(sct)]
                    cn["sc"] += 1
                    e0 = (grp[0] - r + 7) + 8
                    kb.op("dve", lambda e, sc=sc, p=p, c=c, e0=e0: e.tensor_tensor(out=sc.t[:], in0=p.t[:], in1=bt.t[:, c, e0:e0 + 8, :], op=ALU.add),
                          reads=[p, bt], writes=[sc])
                    if special:
                        a = (r - 60) * 16 + gi * 8
                        kb.op("dve", lambda e, sc=sc, a=a: e.tensor_tensor(out=sc.t[:], in0=sc.t[:],
                                                                          in1=smask.t[:, a:a + 8].unsqueeze(2).broadcast_to([128, 8, 64]), op=ALU.add),
                              reads=[sc, smask], writes=[sc])
                    pT = ptl[cn["pT"] % len(ptl)]
                    cn["pT"] += 1
                    kb.op("act", lambda e, pT=pT, sc=sc: e.activation(out=pT.t[:], in_=sc.t[:], func=AF.Exp), reads=[sc], writes=[pT])
                    pts.append((pT, grp))
                allpts.append(pts)
            for c in range(4):
                pts = allpts[c]
                po = pot[c // 2]
                n_mm = sum(len(grp) for _, grp in pts)
                i_mm = 0
                for pT, grp in pts:
                    for j, rr in enumerate(grp):
                        kb.op("pe", lambda e, j=j, rr=rr, i_mm=i_mm, pT=pT, po=po, c=c, n_mm=n_mm: e.matmul(
                            po.t[0:64, (c % 2) * 130:(c % 2) * 130 + 130], pT.t[:, j, :], vbd[rr % NR].t[:, c, :],
                            start=(i_mm == 0), stop=(i_mm == n_mm - 1)), reads=[pT, vbd[rr % NR]], writes=[po])
                        i_mm += 1
            rc = rcp[r % 4]
            ob = obf[r % 2]
            for hf in range(2):
                pv = pot[hf].t[0:64, 0:260].rearrange("p (a x) -> p a x", x=65)
                kb.op("dve", lambda e: e.reciprocal(out=rc.t[:, :].unsqueeze(2), in_=pv[:, :, 64:65]), reads=[pot[hf]], writes=[rc])
                kb.op("dve", lambda e: e.tensor_tensor(out=ob.t[:, hf * 256:(hf + 1) * 256].rearrange("p (a x) -> p a x", x=64),
                                                      in0=pv[:, :, 0:64], in1=rc.t[:, :].unsqueeze(2).broadcast_to([64, 4, 64]), op=ALU.mult),
                      reads=[pot[hf], rc], writes=[ob])
            for fc in range(4):
                kb.op("pe", lambda e, fc=fc: e.transpose(out=ptt.t[:, fc, :], in_=ob.t[:, fc * 128:(fc + 1) * 128], identity=g.ident_bf.t[0:64, 0:64]),
                      reads=[ob, g.ident_bf], writes=[ptt])
            st = (r * 64) // ST
            o = outb[st % 2]
            off = (r * 64) % ST
            kb.op("act", lambda e: e.copy(out=o.t[:, :, off:off + 64], in_=ptt.t[:]), reads=[ptt], writes=[o])
            if off + 64 == ST:
                kb.dma("pool", S["naT"].rearrange("(c p) t -> p c t", p=128)[:, :, st * ST:(st + 1) * ST], o.t[:],
                       reads=[o], writes=[g.DB["naT"][st]])


def stage_C(g, l):
    nc, kb, I, S = g.nc, g.kb, g.I, g.S
    with ExitStack() as es:
        og = load_col(g, es, "C_og", I["hg_out_norm"][l, :], 1)
        NB_ = 3
        lf = [_alloc(es, nc, "C_lf%d" % i, [128, 4, 128], F32) for i in range(NB_)]
        kk = [_alloc(es, nc, "C_kk%d" % i, [128, 4, 128], BF16) for i in range(NB_)]
        qh = [_alloc(es, nc, "C_qh%d" % i, [128, 4, 128], BF16) for i in range(NB_)]
        vh = [_alloc(es, nc, "C_vh%d" % i, [128, 512], BF16) for i in range(NB_)]
        gsb = [_alloc(es, nc, "C_gs%d" % i, [128, 4, 128], BF16) for i in range(NB_)]
        ofb = [_alloc(es, nc, "C_of%d" % i, [128, 4, 128], F32) for i in range(NB_)]
        cb_ = [_alloc(es, nc, "C_c%d" % i, [128, 132], F32) for i in range(4)]
        cm = [_alloc(es, nc, "C_cm%d" % i, [128, 128], F32) for i in range(4)]
        e1 = [_alloc(es, nc, "C_e1%d" % i, [128, 128], F32) for i in range(4)]
        e2 = [_alloc(es, nc, "C_e2%d" % i, [128, 128], F32) for i in range(4)]
        qt = [_alloc(es, nc, "C_qt%d" % i, [128, 128], BF16) for i in range(4)]
        kt = [_alloc(es, nc, "C_kt%d" % i, [128, 128], BF16) for i in range(4)]
        ktok = [_alloc(es, nc, "C_ktok%d" % i, [128, 128], BF16) for i in range(4)]
        attm = [_alloc(es, nc, "C_att%d" % i, [128, 128], BF16) for i in range(4)]
        t6 = [_alloc(es, nc, "C_t6%d" % i, [128, 2, 3], F32) for i in range(4)]
        x6 = [_alloc(es, nc, "C_x6%d" % i, [128, 2, 3], F32) for i in range(4)]
        bs = [_alloc(es, nc, "C_bs%d" % i, [128, 2], F32) for i in range(4)]
        Sst = [_alloc(es, nc, "C_S%d" % i, [128, 128], F32) for i in range(4)]
        Smid = [_alloc(es, nc, "C_Sm%d" % i, [128, 128], BF16) for i in range(8)]
        tmp = [_alloc(es, nc, "C_tmp%d" % i, [128, 128], F32) for i in range(4)]
        osb = [_alloc(es, nc, "C_o%d" % i, [128, 4, 128], F32) for i in range(2)]
        sqb = [_alloc(es, nc, "C_sq%d" % i, [128, 128], F32) for i in range(2)]
        rsb = [_alloc(es, nc, "C_rs%d" % i, [128, 128], F32) for i in range(2)]
        hgo = [_alloc(es, nc, "C_hg%d" % i, [128, 4, 128], BF16) for i in range(2)]
        zeros = _alloc(es, nc, "C_z", [128, 128], F32)
        kb.op("dve", lambda e: e.memset(zeros.t[:], 0.0), writes=[zeros])
        p_att = [_alloc(es, nc, "C_pa%d" % i, [128, 128], F32, psum=True) for i in range(2)]
        p_tr = [_alloc(es, nc, "C_pt%d" % i, [128, 128], BF16, psum=True) for i in range(1)]
        p_o = [_alloc(es, nc, "C_po%d" % i, [128, 128], F32, psum=True) for i in range(2)]
        p_u = [_alloc(es, nc, "C_pu%d" % i, [128, 128], F32, psum=True) for i in range(2)]
        p_n = [_alloc(es, nc, "C_pn%d" % i, [128, 128], F32, psum=True) for i in range(1)]
        cnt = {"i": 0}
        fm = lambda nm: S[nm].rearrange("(h p) t -> p h t", p=128)

        for d_ in range(2):
            tri = g.trif if d_ == 0 else g.trib
            for h in range(4):
                kb.op("dve", lambda e, h=h: e.memset(Sst[h].t[:], 0.0), writes=[Sst[h]])
                kb.op("dve", lambda e, h=h: e.memset(Smid[h].t[:], 0.0), writes=[Smid[h]])
            smi = [0, 0, 0, 0]
            tiles = range(NT) if d_ == 0 else range(NT - 1, -1, -1)
            if g.lim:
                tiles = list(tiles)[:g.lim]
            for ti in tiles:
                i = cnt["i"] % NB_
                cnt["i"] += 1
                st = (ti * 128) // ST
                ts_ = slice(ti * 128, (ti + 1) * 128)
                with nc.allow_non_contiguous_dma(reason="head blocks"):
                    kb.dma("sp", lf[i].t[:], S["lf"][d_].rearrange("(h p) t -> p h t", p=128)[:, :, ts_], reads=[g.DB["lf"][st]], writes=[lf[i]])
                    kb.dma("sp", kk[i].t[:], S["kk"][d_].rearrange("(h p) t -> p h t", p=128)[:, :, ts_], reads=[g.DB["kk"][st]], writes=[kk[i]])
                    kb.dma("sp", qh[i].t[:], fm("qh")[:, :, ts_], reads=[g.DB["qh"][st]], writes=[qh[i]])
                    kb.dma("sp", vh[i].t[:], S["vh"][ts_, :], reads=[g.DB["vh"][st]], writes=[vh[i]])
                    if d_ == 1:
                        kb.dma("sp", gsb[i].t[:], fm("gs")[:, :, ts_], reads=[g.DB["gs"][st]], writes=[gsb[i]])
                        kb.dma("sp", ofb[i].t[:], fm("of")[:, :, ts_], reads=[g.DB["of"][st]], writes=[ofb[i]])
                ob = osb[ti % 2]
                for h in range(4):
                    C = cb_[h]
                    kb.op("dve", lambda e: e.memset(C.t[:, 0:1], 0.0), writes=[C])
                    kb.op("dve", lambda e: e.tensor_tensor_scan(out=C.t[:, 1:129], data0=lf[i].t[:, h, :], data1=zeros.t[:],
                                                               initial=0.0, op0=ALU.add, op1=ALU.add),
                          reads=[lf[i], zeros], writes=[C])
                    Cv = C.t[:, 1:129].rearrange("p (a x) -> p a x", x=64)
                    Z = Cv if d_ == 0 else C.t[:, 0:128].rearrange("p (a x) -> p a x", x=64)
                    endv = Cv[:, :, 63:64]
                    midv = Cv[:, :, 31:32]
                    B_ = bs[h]
                    kb.op("dve", lambda e: e.memset(B_.t[:, 0:1], 0.0), writes=[B_])
                    kb.op("dve", lambda e: e.tensor_copy(out=B_.t[:, 1:2], in_=C.t[:, 64:65]), reads=[C], writes=[B_])
                    T6 = t6[h]
                    bv = B_.t[:, :].unsqueeze(2)
                    kb.op("dve", lambda e: e.tensor_tensor(out=T6.t[:, :, 0:1], in0=endv, in1=bv, op=ALU.subtract), reads=[C, B_], writes=[T6])
                    kb.op("dve", lambda e: e.tensor_tensor(out=T6.t[:, :, 1:2], in0=endv, in1=midv, op=ALU.subtract), reads=[C], writes=[T6])
                    kb.op("dve", lambda e: e.tensor_tensor(out=T6.t[:, :, 2:3], in0=midv, in1=bv, op=ALU.subtract), reads=[C, B_], writes=[T6])
                    X6 = x6[h]
                    kb.op("act", lambda e: e.activation(out=X6.t[:], in_=T6.t[:], func=AF.Exp), reads=[T6], writes=[X6])
                    i_d2 = 1 if d_ == 0 else 2
                    i_m = 2 if d_ == 0 else 1
                    CM = cm[h]
                    kb.op("dve", lambda e: e.tensor_tensor(out=CM.t[:].rearrange("p (a x) -> p a x", x=64), in0=Z,
                                                          in1=midv.broadcast_to([128, 2, 64]), op=ALU.subtract), reads=[C], writes=[CM])
                    sgn = 1.0 if d_ == 0 else -1.0
                    kb.op("act", lambda e: e.activation(out=e1[h].t[:], in_=CM.t[:], func=AF.Exp, scale=sgn), reads=[CM], writes=[e1[h]])
                    kb.op("act", lambda e: e.activation(out=e2[h].t[:], in_=CM.t[:], func=AF.Exp, scale=-sgn), reads=[CM], writes=[e2[h]])
                    kb.op("pool", lambda e: e.tensor_tensor(out=qt[h].t[:], in0=qh[i].t[:, h, :], in1=e1[h].t[:], op=ALU.mult),
                          reads=[qh[i], e1[h]], writes=[qt[h]])
                    kb.op("dve", lambda e: e.tensor_tensor(out=kt[h].t[:], in0=kk[i].t[:, h, :], in1=e2[h].t[:], op=ALU.mult),
                          reads=[kk[i], e2[h]], writes=[kt[h]])
                    pa = p_att[h % 2]
                    kb.op("pe", lambda e: e.matmul(pa.t[:], kt[h].t[:], qt[h].t[:], start=True, stop=True), reads=[kt[h], qt[h]], writes=[pa])
                    kb.op("dve", lambda e: e.tensor_tensor(out=attm[h].t[:], in0=pa.t[:], in1=tri.t[:], op=ALU.mult), reads=[pa, tri], writes=[attm[h]])
                    ptr_ = p_tr[0]
                    kb.op("pe", lambda e: e.transpose(out=ptr_.t[:], in_=kt[h].t[:], identity=g.ident_bf.t[:]), reads=[kt[h], g.ident_bf], writes=[ptr_])
                    kb.op("act", lambda e: e.copy(out=ktok[h].t[:], in_=ptr_.t[:]), reads=[ptr_], writes=[ktok[h]])
                    po = p_o[h % 2]
                    vslice = vh[i].t[:, h * 128:(h + 1) * 128]
                    kb.op("pe", lambda e: e.matmul(po.t[:], vslice, attm[h].t[:], start=True, stop=False), reads=[vh[i], attm[h]], writes=[po])
                    chunks = (0, 1) if d_ == 0 else (1, 0)
                    smi[h] ^= 1
                    sm0 = Smid[h + 4 * smi[h]]
                    kb.op("dve", lambda e, sm0=sm0: e.tensor_scalar(out=sm0.t[:], in0=Sst[h].t[:], scalar1=X6.t[:, chunks[0], i_m:i_m + 1],
                                                                  scalar2=None, op0=ALU.mult), reads=[Sst[h], X6], writes=[sm0])
                    for ci, a in enumerate(chunks):
                        sm = Smid[h + 4 * smi[h]]
                        kb.op("pe", lambda e, a=a, sm=sm, ci=ci: e.matmul(po.t[:, a * 64:(a + 1) * 64], sm.t[:], qt[h].t[:, a * 64:(a + 1) * 64],
                                                                         start=False, stop=(ci == 1)),
                              reads=[sm, qt[h]], writes=[po])
                        pu = p_u[(h + a) % 2]
                        kb.op("pe", lambda e, a=a: e.matmul(pu.t[:], ktok[h].t[a * 64:(a + 1) * 64, :], vh[i].t[a * 64:(a + 1) * 64, h * 128:(h + 1) * 128],
                                                           start=True, stop=True), reads=[ktok[h], vh[i]], writes=[pu])
                        tm = tmp[h]
                        kb.op("dve", lambda e, a=a: e.tensor_scalar(out=tm.t[:], in0=pu.t[:], scalar1=X6.t[:, a, i_d2:i_d2 + 1], scalar2=None, op0=ALU.mult),
                              reads=[pu, X6], writes=[tm])
                        kb.op("dve", lambda e, a=a: e.scalar_tensor_tensor(out=Sst[h].t[:], in0=Sst[h].t[:], scalar=X6.t[:, a, 0:1], in1=tm.t[:],
                                                                          op0=ALU.mult, op1=ALU.add), reads=[Sst[h], X6, tm], writes=[Sst[h]])
                        chunk_id = ti * 2 + a
                        if (d_ == 0 and chunk_id == 63) or (d_ == 1 and chunk_id == 64):
                            kb.op("dve", lambda e: e.tensor_scalar(out=Sst[h].t[:], in0=Sst[h].t[:], scalar1=g.cflag.t[:, 0:1], scalar2=None, op0=ALU.mult),
                                  reads=[Sst[h], g.cflag], writes=[Sst[h]])
                        if ci == 0:
                            na_ = chunks[1]
                            smi[h] ^= 1
                            sm2 = Smid[h + 4 * smi[h]]
                            kb.op("dve", lambda e, na_=na_, sm2=sm2: e.tensor_scalar(out=sm2.t[:], in0=Sst[h].t[:], scalar1=X6.t[:, na_, i_m:i_m + 1],
                                                                                  scalar2=None, op0=ALU.mult), reads=[Sst[h], X6], writes=[sm2])
                    if d_ == 0:
                        kb.op("act", lambda e: e.copy(out=ob.t[:, h, :], in_=po.t[:]), reads=[po], writes=[ob])
                    else:
                        kb.op("dve", lambda e: e.tensor_tensor(out=ob.t[:, h, :], in0=po.t[:], in1=ofb[i].t[:, h, :], op=ALU.add),
                              reads=[po, ofb[i]], writes=[ob])
                        sq = sqb[h % 2]
                        kb.op("act", lambda e: e.activation(out=sq.t[:], in_=ob.t[:, h, :], func=AF.Square), reads=[ob], writes=[sq])
                        pn = p_n[0]
                        kb.op("pe", lambda e: e.matmul(pn.t[:], g.ones_f.t[:], sq.t[:], start=True, stop=True), reads=[g.ones_f, sq], writes=[pn])
                        rs = rsb[h % 2]
                        kb.op("act", lambda e: e.activation(out=rs.t[:], in_=pn.t[:], func=AF.Ln, scale=1.0 / 128, bias=EPS), reads=[pn], writes=[rs])
                        kb.op("act", lambda e: e.activation(out=rs.t[:], in_=rs.t[:], func=AF.Exp, scale=-0.5), reads=[rs], writes=[rs])
                        kb.op("dve", lambda e: e.scalar_tensor_tensor(out=rs.t[:], in0=ob.t[:, h, :], scalar=og.t[:, 0:1], in1=rs.t[:],
                                                                     op0=ALU.mult, op1=ALU.mult), reads=[ob, og, rs], writes=[rs])
                        kb.op("pool", lambda e: e.tensor_tensor(out=hgo[ti % 2].t[:, h, :], in0=rs.t[:], in1=gsb[i].t[:, h, :], op=ALU.mult),
                              reads=[rs, gsb[i]], writes=[hgo[ti % 2]])
                with nc.allow_non_contiguous_dma(reason="head blocks"):
                    if d_ == 0:
                        kb.dma("pool", fm("of")[:, :, ts_], ob.t[:], reads=[ob], writes=[g.DB["of"][st]])
                    else:
                        kb.dma("pool", fm("hgT")[:, :, ts_], hgo[ti % 2].t[:], reads=[hgo[ti % 2]], writes=[g.DB["hgT"][st]])


def stage_D(g, l):
    nc, kb, I, S = g.nc, g.kb, g.I, g.S
    TB = 1024
    with ExitStack() as es:
        cw = _alloc(es, nc, "D_cw", [128, 4, 31], F32)
        with nc.allow_non_contiguous_dma(reason="tiny conv weights"):
            for c in range(4):
                kb.dma("sp", cw.t[:, c, :], I["conv_w"][l, :, c * 128:(c + 1) * 128].rearrange("j p -> p j"), writes=[cw])
        cbias = load_col(g, es, "D_cb", I["conv_b"][l, :], 4)
        lng = load_col(g, es, "D_lg", I["conv_ln_g"][l, :], 4)
        lnb = load_col(g, es, "D_lb", I["conv_ln_b"][l, :], 4)
        ub = [_alloc(es, nc, "D_u%d" % i, [128, TB + 30], F32) for i in range(3)]
        yb = [_alloc(es, nc, "D_y%d" % i, [128, 4, TB], F32) for i in range(2)]
        sqt = [_alloc(es, nc, "D_sq%d" % i, [128, ST], F32) for i in range(2)]
        mean = [_alloc(es, nc, "D_mean%d" % i, [128, ST], F32) for i in range(2)]
        rstd = [_alloc(es, nc, "D_rstd%d" % i, [128, ST], F32) for i in range(2)]
        tt = [_alloc(es, nc, "D_t%d" % i, [128, ST], F32) for i in range(3)]
        ob = [_alloc(es, nc, "D_o%d" % i, [128, 4, ST], BF16) for i in range(2)]
        ps1 = [_alloc(es, nc, "D_p1%d" % i, [128, ST], F32, psum=True) for i in range(2)]
        ps2 = [_alloc(es, nc, "D_p2%d" % i, [128, ST], F32, psum=True) for i in range(2)]
        k = 0
        nsb = 0
        for blk in range(T // TB):
            t0 = blk * TB
            yv = yb[blk % 2]
            for c in range(4):
                u = ub[k % 3]
                k += 1
                lo, hi = t0 - 15, t0 + TB + 15
                a, b = max(lo, 0), min(hi, T)
                if lo < 0:
                    kb.op("pool", lambda e: e.memset(u.t[:, 0:15], 0.0), writes=[u])
                if hi > T:
                    kb.op("pool", lambda e: e.memset(u.t[:, TB + 15:TB + 30], 0.0), writes=[u])
                rd = [g.DB["u"][s_] for s_ in range(a // ST, (b - 1) // ST + 1)]
                kb.dma("sp", u.t[:, a - lo:b - lo], S["u"][c * 128:(c + 1) * 128, a:b], reads=rd, writes=[u])
                if t0 == 4096:
                    kb.op("pool", lambda e: e.tensor_scalar(out=u.t[:, 0:15], in0=u.t[:, 0:15], scalar1=g.cflag.t[:, 0:1], scalar2=None, op0=ALU.mult),
                          reads=[u, g.cflag], writes=[u])
                if t0 + TB == 4096:
                    kb.op("pool", lambda e: e.tensor_scalar(out=u.t[:, TB + 15:TB + 30], in0=u.t[:, TB + 15:TB + 30], scalar1=g.cflag.t[:, 0:1],
                                                           scalar2=None, op0=ALU.mult), reads=[u, g.cflag], writes=[u])
                kb.op("dve", lambda e: e.tensor_scalar(out=yv.t[:, c, :], in0=u.t[:, 0:TB], scalar1=cw.t[:, c, 0:1], scalar2=cbias.t[:, c:c + 1],
                                                      op0=ALU.mult, op1=ALU.add), reads=[u, cw, cbias], writes=[yv])
                for j in range(1, 31):
                    kb.op("dve", lambda e, j=j: e.scalar_tensor_tensor(out=yv.t[:, c, :], in0=u.t[:, j:j + TB], scalar=cw.t[:, c, j:j + 1], in1=yv.t[:, c, :],
                                                                      op0=ALU.mult, op1=ALU.add), reads=[u, cw, yv], writes=[yv])
            for sb_ in range(TB // ST):
                cs = slice(sb_ * ST, (sb_ + 1) * ST)
                p1 = ps1[nsb % 2]
                p2 = ps2[nsb % 2]
                for c in range(4):
                    sq = sqt[c % 2]
                    kb.op("act", lambda e, c=c: e.activation(out=sq.t[:], in_=yv.t[:, c, cs], func=AF.Square), reads=[yv], writes=[sq])
                    kb.op("pe", lambda e, c=c: e.matmul(p1.t[:], g.ones_f.t[:], yv.t[:, c, cs], start=(c == 0), stop=(c == 3)),
                          reads=[g.ones_f, yv], writes=[p1])
                    kb.op("pe", lambda e, c=c: e.matmul(p2.t[:], g.ones_f.t[:], sq.t[:], start=(c == 0), stop=(c == 3)),
                          reads=[g.ones_f, sq], writes=[p2])
                mn = mean[nsb % 2]
                rs = rstd[nsb % 2]
                kb.op("act", lambda e: e.activation(out=mn.t[:], in_=p1.t[:], func=AF.Copy, scale=1.0 / 512), reads=[p1], writes=[mn])
                msq = tt[2]
                kb.op("dve", lambda e: e.tensor_tensor(out=msq.t[:], in0=mn.t[:], in1=mn.t[:], op=ALU.mult), reads=[mn], writes=[msq])
                kb.op("dve", lambda e: e.scalar_tensor_tensor(out=rs.t[:], in0=p2.t[:], scalar=1.0 / 512, in1=msq.t[:], op0=ALU.mult, op1=ALU.subtract),
                      reads=[p2, msq], writes=[rs])
                kb.op("act", lambda e: e.activation(out=rs.t[:], in_=rs.t[:], func=AF.Ln, bias=EPS), reads=[rs], writes=[rs])
                kb.op("act", lambda e: e.activation(out=rs.t[:], in_=rs.t[:], func=AF.Exp, scale=-0.5), reads=[rs], writes=[rs])
                o = ob[nsb % 2]
                for c in range(4):
                    t_ = tt[c % 2]
                    kb.op("dve", lambda e, c=c: e.tensor_tensor(out=t_.t[:], in0=yv.t[:, c, cs], in1=mn.t[:], op=ALU.subtract), reads=[yv, mn], writes=[t_])
                    kb.op("pool", lambda e: e.tensor_tensor(out=t_.t[:], in0=t_.t[:], in1=rs.t[:], op=ALU.mult), reads=[t_, rs], writes=[t_])
                    kb.op("act", lambda e, c=c: e.activation(out=o.t[:, c, :], in_=t_.t[:], func=AF.Silu, scale=lng.t[:, c:c + 1], bias=lnb.t[:, c:c + 1]),
                          reads=[t_, lng, lnb], writes=[o])
                st = (t0 + sb_ * ST) // ST
                kb.dma("pool", S["cvT"].rearrange("(c p) t -> p c t", p=128)[:, :, st * ST:(st + 1) * ST], o.t[:], reads=[o], writes=[g.DB["cvT"][st]])
                nsb += 1


def stage_E(g, l, xsrc):
    nc, kb, I, S = g.nc, g.kb, g.I, g.S
    with ExitStack() as es:
        wbr = _alloc(es, nc, "E_wbr", [128, 12, D], BF16)
        for bi, nm in enumerate(("w_na_br", "w_hg_br", "w_cv_br")):
            kb.dma("pool", wbr.t[:, bi * 4:(bi + 1) * 4, :], I[nm][l].rearrange("(c p) n -> p c n", p=128), writes=[wbr])
        wout = _alloc(es, nc, "E_wout", [128, 8, D], BF16)
        kb.dma("pool", wout.t[:], I["w_out"][l].rearrange("(c p) n -> p c n", p=128), writes=[wout])
        brT = [[_alloc(es, nc, "E_br%d_%d" % (b, i), [128, 4, ST], BF16) for b in range(3)] for i in range(2)]
        sgt = [_alloc(es, nc, "E_sg%d" % i, [128, 24, ST], BF16) for i in range(2)]
        mT = _alloc(es, nc, "E_mT", [128, 8, ST], BF16)
        tmp = [_alloc(es, nc, "E_t%d" % i, [128, ST], F32) for i in range(6)]
        xa = [_alloc(es, nc, "E_x%d" % i, [128, D], F32) for i in range(3)]
        pb = [_alloc(es, nc, "E_pb%d" % i, [128, ST], F32, psum=True) for i in range(6)]
        pq = [_alloc(es, nc, "E_pq%d" % i, [128, ST], F32, psum=True) for i in range(2)]
        fmv = lambda nm: S[nm].rearrange("(c p) t -> p c t", p=128)
        n_pb = 0
        n_x = 0
        n_pq = 0
        for st in range(NST):
            t0 = st * ST
            br = brT[st % 2]
            sg = sgt[st % 2]
            for b, nm in enumerate(("naT", "hgT", "cvT")):
                kb.dma("sp", br[b].t[:], fmv(nm)[:, :, t0:t0 + ST], reads=[g.DB[nm][st]], writes=[br[b]])
            kb.dma("sp", sg.t[:], fmv("sg")[:, :, t0:t0 + ST], reads=[g.DB["sg"][st]], writes=[sg])
            for m in range(8):
                ps = []
                for b in range(3):
                    p = pb[n_pb % 6]
                    n_pb += 1
                    for kc in range(4):
                        kb.op("pe", lambda e, b=b, kc=kc, p=p: e.matmul(p.t[:], wbr.t[:, b * 4 + kc, m * 128:(m + 1) * 128], br[b].t[:, kc, :],
                                                                       start=(kc == 0), stop=(kc == 3)), reads=[wbr, br[b]], writes=[p])
                    ps.append(p)
                t1, t2, t3 = tmp[(m % 2) * 3], tmp[(m % 2) * 3 + 1], tmp[(m % 2) * 3 + 2]
                kb.op("dve", lambda e: e.tensor_tensor(out=t1.t[:], in0=ps[0].t[:], in1=sg.t[:, m, :], op=ALU.mult), reads=[ps[0], sg], writes=[t1])
                kb.op("dve", lambda e: e.tensor_tensor(out=t2.t[:], in0=ps[1].t[:], in1=sg.t[:, 8 + m, :], op=ALU.mult), reads=[ps[1], sg], writes=[t2])
                kb.op("dve", lambda e: e.tensor_tensor(out=t3.t[:], in0=ps[2].t[:], in1=sg.t[:, 16 + m, :], op=ALU.mult), reads=[ps[2], sg], writes=[t3])
                kb.op("pool", lambda e: e.tensor_tensor(out=t1.t[:], in0=t1.t[:], in1=t2.t[:], op=ALU.add), reads=[t1, t2], writes=[t1])
                kb.op("pool", lambda e: e.tensor_tensor(out=mT.t[:, m, :], in0=t1.t[:], in1=t3.t[:], op=ALU.add), reads=[t1, t3], writes=[mT])
            for tb in range(4):
                x = xa[n_x % 3]
                n_x += 1
                rows = slice(t0 + tb * 128, t0 + (tb + 1) * 128)
                kb.dma("sp", x.t[:], xsrc[rows, :], reads=[g.DB["y"][st]], writes=[x])
                for hf in range(2):
                    p = pq[n_pq % 2]
                    n_pq += 1
                    for m in range(8):
                        kb.op("pe", lambda e, m=m, p=p: e.matmul(p.t[:], mT.t[:, m, tb * 128:(tb + 1) * 128], wout.t[:, m, hf * 512:(hf + 1) * 512],
                                                                start=(m == 0), stop=(m == 7)), reads=[mT, wout], writes=[p])
                    kb.op("dve", lambda e, p=p: e.tensor_tensor(out=x.t[:, hf * 512:(hf + 1) * 512], in0=p.t[:], in1=x.t[:, hf * 512:(hf + 1) * 512], op=ALU.add),
                          reads=[p, x], writes=[x])
                kb.dma("pool", g.yc[rows, :], x.t[:], reads=[x], writes=[g.DB["y"][st]])


def stage_F(g, l):
    nc, kb, I, S = g.nc, g.kb, g.I, g.S
    y = g.yc
    with ExitStack() as es:
        W = NormWork(g, es, "F")
        gmem = load_bcast_row(g, es, "F_gmem", I["norm_mem"][l, :], D)
        gkv = load_bcast_row(g, es, "F_gkv", I["mem_kv_norm"][l, :], D)
        gffn = load_bcast_row(g, es, "F_gffn", I["norm_ffn"][l, :], D)
        qn = load_col(g, es, "F_qn", I["mem_q_norm"][l, :], 1)
        kn = load_col(g, es, "F_kn", I["mem_k_norm"][l, :], 1)
        kb.op("dve", lambda e: e.tensor_scalar(out=qn.t[:], in0=qn.t[:], scalar1=128.0 ** -0.5, scalar2=None, op0=ALU.mult), reads=[qn], writes=[qn])
        wq = _alloc(es, nc, "F_wq", [128, 8, 512], BF16)
        kb.dma("pool", wq.t[:], I["wq_mem"][l].rearrange("(c p) n -> p c n", p=128), writes=[wq])
        wkv = _alloc(es, nc, "F_wkv", [128, 8, D], BF16)
        kb.dma("pool", wkv.t[:], I["wkv_mem"][l].rearrange("(c p) n -> p c n", p=128), writes=[wkv])
        wo = _alloc(es, nc, "F_wo", [128, 4, D], BF16)
        kb.dma("pool", wo.t[:], I["wo_mem"][l].rearrange("(c p) n -> p c n", p=128), writes=[wo])
        wr = _alloc(es, nc, "F_wr", [128, 8, NE], BF16)
        with nc.allow_non_contiguous_dma(reason="router weights"):
            kb.dma("pool", wr.t[:], I["w_router"][l].rearrange("(c p) n -> p c n", p=128), writes=[wr])
        hT = _alloc(es, nc, "F_hT", [128, 8, ST], BF16)
        h2T = [_alloc(es, nc, "F_h2T%d" % i, [128, 8, ST], BF16) for i in range(2)]
        KT = [_alloc(es, nc, "F_KT%d" % i, [128, 4, 256], BF16) for i in range(2)]
        V = [_alloc(es, nc, "F_V%d" % i, [128, 2, 512], BF16) for i in range(2)]
        f32t = [_alloc(es, nc, "F_f%d" % i, [128, ST], F32) for i in range(4)]
        qT = [_alloc(es, nc, "F_qT%d" % i, [128, ST], BF16) for i in range(2)]
        PT = [_alloc(es, nc, "F_PT%d" % i, [128, ST], BF16) for i in range(4)]
        oT = _alloc(es, nc, "F_oT", [128, 4, ST], BF16)
        sm = [_alloc(es, nc, "F_sm%d" % i, [128, 4], F32) for i in range(2)]
        ex = [_alloc(es, nc, "F_ex%d" % i, [128, NE], F32) for i in range(2)]
        af = [_alloc(es, nc, "F_af%d" % i, [128, NE], F32) for i in range(2)]
        p_q = _alloc(es, nc, "F_pq", [128, ST], F32, psum=True)
        p_n = _alloc(es, nc, "F_pn", [128, ST], F32, psum=True)
        p_s = [_alloc(es, nc, "F_ps%d" % i, [128, ST], F32, psum=True) for i in range(2)]
        p_o = _alloc(es, nc, "F_po", [128, ST], F32, psum=True)
        p_d = _alloc(es, nc, "F_pd", [128, ST], F32, psum=True)
        p_x = _alloc(es, nc, "F_px", [128, ST], F32, psum=True)

        def chan_norm(p, n, gain_col, out_bf, out_tt):
            sq = f32t[0]
            kb.op("act", lambda e: e.activation(out=sq.t[:, 0:n], in_=p.t[:, 0:n], func=AF.Square), reads=[p], writes=[sq])
            kb.op("pe", lambda e: e.matmul(p_n.t[:, 0:n], g.ones_f.t[:], sq.t[:, 0:n], start=True, stop=True), reads=[g.ones_f, sq], writes=[p_n])
            rs = f32t[1]
            kb.op("act", lambda e: e.activation(out=rs.t[:, 0:n], in_=p_n.t[:, 0:n], func=AF.Ln, scale=1.0 / 128, bias=EPS), reads=[p_n], writes=[rs])
            kb.op("act", lambda e: e.activation(out=rs.t[:, 0:n], in_=rs.t[:, 0:n], func=AF.Exp, scale=-0.5), reads=[rs], writes=[rs])
            kb.op("dve", lambda e: e.scalar_tensor_tensor(out=out_bf, in0=p.t[:, 0:n], scalar=gain_col, in1=rs.t[:, 0:n], op0=ALU.mult, op1=ALU.mult),
                  reads=[p, rs, qn, kn], writes=[out_tt])

        for s in range(2):
            for tb in range(2):
                kb.dma("sp", W.xa[tb].t[:], g.memc[s, tb * 128:(tb + 1) * 128, :], writes=[W.xa[tb]])
            rms_rows(g, W.xa, W.ss, W.rstd, W.junk, 2)
            for tb in range(2):
                kb.op("dve", lambda e, tb=tb: e.scalar_tensor_tensor(out=W.hb[tb].t[:], in0=W.xa[tb].t[:], scalar=W.rstd.t[:, tb:tb + 1], in1=gkv.t[:],
                                                                    op0=ALU.mult, op1=ALU.mult), reads=[W.xa[tb], W.rstd, gkv], writes=[W.hb[tb]])
                for kc in range(8):
                    kb.op("pe", lambda e, tb=tb, kc=kc: e.transpose(out=W.ptr.t[:, kc, :], in_=W.hb[tb].t[:, kc * 128:(kc + 1) * 128], identity=g.ident_bf.t[:]),
                          reads=[W.hb[tb], g.ident_bf], writes=[W.ptr])
                kb.op("act", lambda e, tb=tb: e.copy(out=hT.t[:, :, tb * 128:(tb + 1) * 128], in_=W.ptr.t[:]), reads=[W.ptr], writes=[hT])
            for h in range(4):
                for kc in range(8):
                    kb.op("pe", lambda e, kc=kc: e.matmul(p_q.t[:, 0:256], wkv.t[:, kc, h * 128:(h + 1) * 128], hT.t[:, kc, 0:256], start=(kc == 0), stop=(kc == 7)),
                          reads=[wkv, hT], writes=[p_q])
                chan_norm(p_q, 256, kn.t[:, 0:1], KT[s].t[:, h, :], KT[s])
            for mc in range(2):
                for kc in range(8):
                    kb.op("pe", lambda e, kc=kc: e.matmul(p_o.t[:], hT.t[:, kc, mc * 128:(mc + 1) * 128], wkv.t[:, kc, 512:1024], start=(kc == 0), stop=(kc == 7)),
                          reads=[wkv, hT], writes=[p_o])
                kb.op("act", lambda e: e.copy(out=V[s].t[:, mc, :], in_=p_o.t[:]), reads=[p_o], writes=[V[s]])

        for st in range(NST):
            t0 = st * ST
            s = st // (NST // 2)
            norm_transpose(g, y, st, gmem, W, hT)
            for h in range(4):
                for kc in range(8):
                    kb.op("pe", lambda e, kc=kc: e.matmul(p_q.t[:], wq.t[:, kc, h * 128:(h + 1) * 128], hT.t[:, kc, :], start=(kc == 0), stop=(kc == 7)),
                          reads=[wq, hT], writes=[p_q])
                q_ = qT[h % 2]
                chan_norm(p_q, ST, qn.t[:, 0:1], q_.t[:], q_)
                pts = []
                for mc in range(2):
                    ps_ = p_s[mc]
                    kb.op("pe", lambda e: e.matmul(ps_.t[:], KT[s].t[:, h, mc * 128:(mc + 1) * 128], q_.t[:], start=True, stop=True), reads=[KT[s], q_], writes=[ps_])
                    pt = PT[(h % 2) * 2 + mc]
                    kb.op("act", lambda e: e.activation(out=pt.t[:], in_=ps_.t[:], func=AF.Exp), reads=[ps_], writes=[pt])
                    pts.append(pt)
                for mc in range(2):
                    kb.op("pe", lambda e: e.matmul(p_o.t[:], V[s].t[:, mc, h * 128:(h + 1) * 128], pts[mc].t[:], start=(mc == 0), stop=(mc == 1)),
                          reads=[V[s], pts[mc]], writes=[p_o])
                for mc in range(2):
                    kb.op("pe", lambda e: e.matmul(p_d.t[:], g.ones_bf.t[:], pts[mc].t[:], start=(mc == 0), stop=(mc == 1)), reads=[g.ones_bf, pts[mc]], writes=[p_d])
                rc = f32t[2 + h % 2]
                kb.op("dve", lambda e: e.reciprocal(out=rc.t[:], in_=p_d.t[:]), reads=[p_d], writes=[rc])
                kb.op("dve", lambda e: e.tensor_tensor(out=oT.t[:, h, :], in0=p_o.t[:], in1=rc.t[:], op=ALU.mult), reads=[p_o, rc], writes=[oT])
            for tb in range(4):
                x = W.xa[tb]
                rows = slice(t0 + tb * 128, t0 + (tb + 1) * 128)
                for hf in range(2):
                    for h in range(4):
                        kb.op("pe", lambda e, h=h: e.matmul(p_x.t[:], oT.t[:, h, tb * 128:(tb + 1) * 128], wo.t[:, h, hf * 512:(hf + 1) * 512], start=(h == 0), stop=(h == 3)),
                              reads=[oT, wo], writes=[p_x])
                    kb.op("dve", lambda e: e.tensor_tensor(out=x.t[:, hf * 512:(hf + 1) * 512], in0=p_x.t[:], in1=x.t[:, hf * 512:(hf + 1) * 512], op=ALU.add),
                          reads=[p_x, x], writes=[x])
                kb.dma("pool", y[rows, :], x.t[:], reads=[x], writes=[g.DB["y"][st]])
            rms_rows(g, W.xa, W.ss, W.rstd, W.junk, 4)
            h2 = h2T[st % 2]
            for tb in range(4):
                kb.op("dve", lambda e, tb=tb: e.scalar_tensor_tensor(out=W.hb[tb].t[:], in0=W.xa[tb].t[:], scalar=W.rstd.t[:, tb:tb + 1], in1=gffn.t[:],
                                                                    op0=ALU.mult, op1=ALU.mult), reads=[W.xa[tb], W.rstd, gffn], writes=[W.hb[tb]])
                for kc in range(8):
                    kb.op("pe", lambda e, tb=tb, kc=kc: e.transpose(out=W.ptr.t[:, kc, :], in_=W.hb[tb].t[:, kc * 128:(kc + 1) * 128], identity=g.ident_bf.t[:]),
                          reads=[W.hb[tb], g.ident_bf], writes=[W.ptr])
                kb.op("act", lambda e, tb=tb: e.copy(out=h2.t[:, :, tb * 128:(tb + 1) * 128], in_=W.ptr.t[:]), reads=[W.ptr], writes=[h2])
                kb.dma("sp", g.h2rc[t0 + tb * 128:t0 + (tb + 1) * 128, :], W.hb[tb].t[:], reads=[W.hb[tb]], writes=[g.DBall["h2r"]])
            for tb in range(4):
                for kc in range(8):
                    kb.op("pe", lambda e, kc=kc: e.matmul(p_x.t[:, 0:NE], h2.t[:, kc, tb * 128:(tb + 1) * 128], wr.t[:, kc, :], start=(kc == 0), stop=(kc == 7)),
                          reads=[h2, wr], writes=[p_x])
                s_ = sm[tb % 2]
                kb.op("dve", lambda e: e.tensor_reduce(out=s_.t[:, 0:1], in_=p_x.t[:, 0:NE], axis=AX.X, op=ALU.max), reads=[p_x], writes=[s_])
                kb.op("dve", lambda e: e.tensor_scalar(out=s_.t[:, 1:2], in0=s_.t[:, 0:1], scalar1=-1.0, scalar2=None, op0=ALU.mult), reads=[s_], writes=[s_])
                e_ = ex[tb % 2]
                kb.op("act", lambda e: e.activation(out=e_.t[:], in_=p_x.t[:, 0:NE], func=AF.Exp, bias=s_.t[:, 1:2], accum_out=s_.t[:, 2:3]),
                      reads=[p_x, s_], writes=[e_, s_])
                kb.op("dve", lambda e: e.reciprocal(out=s_.t[:, 3:4], in_=s_.t[:, 2:3]), reads=[s_], writes=[s_])
                a_ = af[tb % 2]
                kb.op("dve", lambda e: e.tensor_scalar(out=a_.t[:], in0=e_.t[:], scalar1=s_.t[:, 3:4], scalar2=None, op0=ALU.mult), reads=[e_, s_], writes=[a_])
                kb.dma("sp", g.affc[t0 + tb * 128:t0 + (tb + 1) * 128, :], a_.t[:], reads=[a_], writes=[g.DBall["affall"]])


def stage_H(g, l):
    nc, kb, I, S = g.nc, g.kb, g.I, g.S
    rd = [b for b in g.DB["aff"]]
    coll(g, "AllGather", ALU.bypass, [[0, 1, 2, 3], [4, 5, 6, 7]], S["aff"][:, :], S["affall"][:, :], rd, g.DBall["affall"])
    with ExitStack() as es:
        A = _alloc(es, nc, "H_A", [128, 256, NE], F32)
        kb.dma("sp", A.t[:], S["affall"].rearrange("(p j) e -> p j e", p=128), reads=[g.DBall["affall"]], writes=[A])
        cmp_ = _alloc(es, nc, "H_cmp", [128, 256, NE], BF16)
        lo = _alloc(es, nc, "H_lo", [128, NE], F32)
        hi = _alloc(es, nc, "H_hi", [128, NE], F32)
        mid = _alloc(es, nc, "H_mid", [128, NE], F32)
        cp = _alloc(es, nc, "H_cp", [128, NE], F32)
        ge = _alloc(es, nc, "H_ge", [128, NE], F32)
        d1 = _alloc(es, nc, "H_d1", [128, NE], F32)
        d2 = _alloc(es, nc, "H_d2", [128, NE], F32)
        pt = _alloc(es, nc, "H_pt", [128, NE], F32, psum=True)
        kb.op("dve", lambda e: e.memset(lo.t[:], 0.0), writes=[lo])
        kb.op("dve", lambda e: e.memset(hi.t[:], 1.0), writes=[hi])
        for it in range(NBIS):
            kb.op("dve", lambda e: e.tensor_tensor(out=mid.t[:], in0=lo.t[:], in1=hi.t[:], op=ALU.add), reads=[lo, hi], writes=[mid])
            kb.op("dve", lambda e: e.tensor_scalar(out=mid.t[:], in0=mid.t[:], scalar1=0.5, scalar2=None, op0=ALU.mult), reads=[mid], writes=[mid])
            kb.op("dve", lambda e: e.tensor_tensor(out=cmp_.t[:], in0=A.t[:], in1=mid.t[:, :].unsqueeze(1).broadcast_to([128, 256, NE]), op=ALU.is_gt),
                  reads=[A, mid], writes=[cmp_])
            kb.op("dve", lambda e: e.tensor_reduce(out=cp.t[:], in_=cmp_.t[:].rearrange("p j e -> p e j"), axis=AX.X, op=ALU.add), reads=[cmp_], writes=[cp])
            kb.op("pe", lambda e: e.matmul(pt.t[:], g.ones_f.t[:], cp.t[:], start=True, stop=True), reads=[g.ones_f, cp], writes=[pt])
            kb.op("dve", lambda e: e.tensor_scalar(out=ge.t[:], in0=pt.t[:], scalar1=float(CAPK) - 0.5, scalar2=None, op0=ALU.is_gt), reads=[pt], writes=[ge])
            kb.op("dve", lambda e: e.tensor_tensor(out=d1.t[:], in0=mid.t[:], in1=lo.t[:], op=ALU.subtract), reads=[mid, lo], writes=[d1])
            kb.op("dve", lambda e: e.tensor_tensor(out=d1.t[:], in0=d1.t[:], in1=ge.t[:], op=ALU.mult), reads=[d1, ge], writes=[d1])
            kb.op("dve", lambda e: e.tensor_tensor(out=d2.t[:], in0=hi.t[:], in1=mid.t[:], op=ALU.subtract), reads=[hi, mid], writes=[d2])
            kb.op("dve", lambda e: e.tensor_tensor(out=d2.t[:], in0=d2.t[:], in1=ge.t[:], op=ALU.mult), reads=[d2, ge], writes=[d2])
            kb.op("dve", lambda e: e.tensor_tensor(out=lo.t[:], in0=lo.t[:], in1=d1.t[:], op=ALU.add), reads=[lo, d1], writes=[lo])
            kb.op("dve", lambda e: e.tensor_tensor(out=hi.t[:], in0=mid.t[:], in1=d2.t[:], op=ALU.add), reads=[mid, d2], writes=[hi])
        own = _alloc(es, nc, "H_own", [128, NT, NE], F32)
        with nc.allow_non_contiguous_dma(reason="aff rows"):
            kb.dma("sp", own.t[:], S["aff"].rearrange("(j p) e -> p j e", p=128), reads=rd, writes=[own])
        msk = _alloc(es, nc, "H_msk", [128, NT, NE], F32)
        kb.op("dve", lambda e: e.tensor_tensor(out=msk.t[:], in0=own.t[:], in1=lo.t[:, :].unsqueeze(1).broadcast_to([128, NT, NE]), op=ALU.is_gt),
              reads=[own, lo], writes=[msk])
        kb.op("dve", lambda e: e.tensor_tensor(out=g.wg.t[:], in0=own.t[:], in1=msk.t[:], op=ALU.mult), reads=[own, msk], writes=[g.wg])


def coll(g, kind, op, groups, in_ap, out_ap, reads, out_buf):
    kb = g.kb
    i = kb.next_dsem("pool")
    deps_ = kb._deps(reads, [out_buf])
    if kb.dcnt[i] > 0:
        deps_[("d", i)] = max(deps_.get(("d", i), 0), kb.dcnt[i])
    kb._wait("pool", deps_)
    inst = g.nc.gpsimd.collective_compute(kind, op, replica_groups=groups, ins=[in_ap], outs=[out_ap])
    kb.dcnt[i] += 16
    inst.then_inc(kb.semh[("d", i)], 16)
    for b in reads:
        b.r[("d", i)] = kb.dcnt[i]
    out_buf.w = (("d", i), kb.dcnt[i])
    out_buf.r = {}


def stage_W(g):
    nc, kb, I = g.nc, g.kb, g.I
    g.wall = {}
    allc = [list(range(8))]
    for l in range(DEPTH):
        for nm, rows, cols in (("w_gate", D, DFF), ("w_up", D, DFF), ("w_down", DFF, D)):
            loc = nc.dram_tensor("%s_loc%d" % (nm, l), [2 * rows, cols], F32, kind="Internal").ap()
            al = nc.dram_tensor("%s_all%d" % (nm, l), [NE * rows, cols], F32, kind="Internal").ap()
            bl, ba = Buf(), Buf()
            kb.dma("sp", loc[:, :], I[nm][l], writes=[bl])
            coll(g, "AllGather", ALU.bypass, allc, loc[:, :], al[:, :], [bl], ba)
            g.wall[(nm, l)] = (al, ba)


def stage_I(g, l):
    nc, kb, I, S = g.nc, g.kb, g.I, g.S
    y = g.y
    NB = 256
    NFC = (DFF + 127) // 128
    with ExitStack() as es:
        wgu = _alloc(es, nc, "I_wgu", [128, 8, 2 * DFF], BF16)
        wd = _alloc(es, nc, "I_wd", [128, NFC, D], BF16)
        h2 = [_alloc(es, nc, "I_h2%d" % i, [128, 8, NB], BF16) for i in range(2)]
        aT = _alloc(es, nc, "I_aT", [128, NFC, NB], BF16)
        sgt = [_alloc(es, nc, "I_sg%d" % i, [128, NB], F32) for i in range(3)]
        xa = [_alloc(es, nc, "I_x%d" % i, [128, D], F32) for i in range(2)]
        pg = [_alloc(es, nc, "I_pg%d" % i, [128, NB], F32, psum=True) for i in range(2)]
        pu = [_alloc(es, nc, "I_pu%d" % i, [128, NB], F32, psum=True) for i in range(2)]
        pq = [_alloc(es, nc, "I_pq%d" % i, [128, 512], F32, psum=True) for i in range(2)]
        n = {"h": 0, "f": 0, "x": 0, "q": 0}
        for ex in range(NE):
            for kc in range(8):
                for hi_, nm_ in enumerate(("w_gate", "w_up")):
                    al_, ba_ = g.wall[(nm_, l)]
                    kb.dma("pool", wgu.t[:, kc, hi_ * DFF:(hi_ + 1) * DFF], al_[ex * D + kc * 128:ex * D + (kc + 1) * 128, :], reads=[ba_], writes=[wgu])
            for fc in range(NFC):
                fw = min(128, DFF - fc * 128)
                al_, ba_ = g.wall[("w_down", l)]
                kb.dma("pool", wd.t[0:fw, fc, :], al_[ex * DFF + fc * 128:ex * DFF + fc * 128 + fw, :], reads=[ba_], writes=[wd])
            for bk in range(T // NB):
                t0 = bk * NB
                st = t0 // ST
                hh = h2[n["h"] % 2]
                n["h"] += 1
                kb.dma("sp", hh.t[:], S["h2T"].rearrange("(c p) t -> p c t", p=128)[:, :, t0:t0 + NB], reads=[g.DB["h2T"][st]], writes=[hh])
                for fc in range(NFC):
                    fw = min(128, DFF - fc * 128)
                    p_g = pg[n["f"] % 2]
                    p_u = pu[n["f"] % 2]
                    s_ = sgt[n["f"] % 3]
                    n["f"] += 1
                    for kc in range(8):
                        kb.op("pe", lambda e, kc=kc: e.matmul(p_g.t[0:fw, :], wgu.t[:, kc, fc * 128:fc * 128 + fw], hh.t[:, kc, :], start=(kc == 0), stop=(kc == 7)),
                              reads=[wgu, hh], writes=[p_g])
                    for kc in range(8):
                        kb.op("pe", lambda e, kc=kc: e.matmul(p_u.t[0:fw, :], wgu.t[:, kc, DFF + fc * 128:DFF + fc * 128 + fw], hh.t[:, kc, :], start=(kc == 0), stop=(kc == 7)),
                              reads=[wgu, hh], writes=[p_u])
                    kb.op("act", lambda e: e.activation(out=s_.t[0:fw, :], in_=p_g.t[0:fw, :], func=AF.Sigmoid), reads=[p_g], writes=[s_])
                    kb.op("dve", lambda e: e.tensor_tensor(out=s_.t[0:fw, :], in0=p_g.t[0:fw, :], in1=s_.t[0:fw, :], op=ALU.mult), reads=[p_g, s_], writes=[s_])
                    kb.op("dve", lambda e: e.tensor_tensor(out=aT.t[0:fw, fc, :], in0=p_u.t[0:fw, :], in1=s_.t[0:fw, :], op=ALU.mult), reads=[p_u, s_], writes=[aT])
                for tb in range(NB // 128):
                    tile_i = (t0 + tb * 128) // 128
                    rows = slice(t0 + tb * 128, t0 + (tb + 1) * 128)
                    x = xa[n["x"] % 2]
                    n["x"] += 1
                    kb.dma("sp", x.t[:], y[rows, :], reads=[g.DB["y"][st]], writes=[x])
                    for hf in range(2):
                        p = pq[n["q"] % 2]
                        n["q"] += 1
                        for fc in range(NFC):
                            fw = min(128, DFF - fc * 128)
                            kb.op("pe", lambda e, fc=fc, fw=fw: e.matmul(p.t[:], aT.t[0:fw, fc, tb * 128:(tb + 1) * 128], wd.t[0:fw, fc, hf * 512:(hf + 1) * 512],
                                                                        start=(fc == 0), stop=(fc == NFC - 1)), reads=[aT, wd], writes=[p])
                        kb.op("dve", lambda e: e.scalar_tensor_tensor(out=x.t[:, hf * 512:(hf + 1) * 512], in0=p.t[:], scalar=g.wg.t[:, tile_i, ex:ex + 1],
                                                                     in1=x.t[:, hf * 512:(hf + 1) * 512], op0=ALU.mult, op1=ALU.add), reads=[p, g.wg, x], writes=[x])
                    kb.dma("pool", y[rows, :], x.t[:], reads=[x], writes=[g.DB["y"][st]])


def stage_H2(g, l):
    nc, kb, I, S = g.nc, g.kb, g.I, g.S
    with ExitStack() as es:
        A = _alloc(es, nc, "H_A", [128, 256, NE], F32)
        kb.dma("sp", A.t[:], S["affall"].rearrange("(p j) e -> p j e", p=128), reads=[g.DBall["affall"]], writes=[A])
        cmp_ = _alloc(es, nc, "H_cmp", [128, 256, NE], BF16)
        lo = _alloc(es, nc, "H_lo", [128, NE], F32)
        hi = _alloc(es, nc, "H_hi", [128, NE], F32)
        mid = _alloc(es, nc, "H_mid", [128, NE], F32)
        cp = _alloc(es, nc, "H_cp", [128, NE], F32)
        ge = _alloc(es, nc, "H_ge", [128, NE], F32)
        d1 = _alloc(es, nc, "H_d1", [128, NE], F32)
        d2 = _alloc(es, nc, "H_d2", [128, NE], F32)
        pt = _alloc(es, nc, "H_pt", [128, NE], F32, psum=True)
        kb.op("dve", lambda e: e.memset(lo.t[:], 0.0), writes=[lo])
        kb.op("dve", lambda e: e.memset(hi.t[:], 1.0), writes=[hi])
        for it in range(NBIS):
            kb.op("dve", lambda e: e.tensor_tensor(out=mid.t[:], in0=lo.t[:], in1=hi.t[:], op=ALU.add), reads=[lo, hi], writes=[mid])
            kb.op("dve", lambda e: e.tensor_scalar(out=mid.t[:], in0=mid.t[:], scalar1=0.5, scalar2=None, op0=ALU.mult), reads=[mid], writes=[mid])
            kb.op("dve", lambda e: e.tensor_tensor(out=cmp_.t[:], in0=A.t[:], in1=mid.t[:, :].unsqueeze(1).broadcast_to([128, 256, NE]), op=ALU.is_gt),
                  reads=[A, mid], writes=[cmp_])
            kb.op("dve", lambda e: e.tensor_reduce(out=cp.t[:], in_=cmp_.t[:].rearrange("p j e -> p e j"), axis=AX.X, op=ALU.add), reads=[cmp_], writes=[cp])
            kb.op("pe", lambda e: e.matmul(pt.t[:], g.ones_f.t[:], cp.t[:], start=True, stop=True), reads=[g.ones_f, cp], writes=[pt])
            kb.op("dve", lambda e: e.tensor_scalar(out=ge.t[:], in0=pt.t[:], scalar1=float(CAPK) - 0.5, scalar2=None, op0=ALU.is_gt), reads=[pt], writes=[ge])
            kb.op("dve", lambda e: e.tensor_tensor(out=d1.t[:], in0=mid.t[:], in1=lo.t[:], op=ALU.subtract), reads=[mid, lo], writes=[d1])
            kb.op("dve", lambda e: e.tensor_tensor(out=d1.t[:], in0=d1.t[:], in1=ge.t[:], op=ALU.mult), reads=[d1, ge], writes=[d1])
            kb.op("dve", lambda e: e.tensor_tensor(out=d2.t[:], in0=hi.t[:], in1=mid.t[:], op=ALU.subtract), reads=[hi, mid], writes=[d2])
            kb.op("dve", lambda e: e.tensor_tensor(out=d2.t[:], in0=d2.t[:], in1=ge.t[:], op=ALU.mult), reads=[d2, ge], writes=[d2])
            kb.op("dve", lambda e: e.tensor_tensor(out=lo.t[:], in0=lo.t[:], in1=d1.t[:], op=ALU.add), reads=[lo, d1], writes=[lo])
            kb.op("dve", lambda e: e.tensor_tensor(out=hi.t[:], in0=mid.t[:], in1=d2.t[:], op=ALU.add), reads=[mid, d2], writes=[hi])
        M = _alloc(es, nc, "H_M", [128, 256, NE], F32)
        wgt = _alloc(es, nc, "H_wgt", [128, 256, NE], F32)
        Pin = _alloc(es, nc, "H_Pin", [128, NE, 256], F32)
        idx = _alloc(es, nc, "H_idx", [128, NE, 256], U32)
        zer = _alloc(es, nc, "H_zer", [128, 256], F32)
        cnt = _alloc(es, nc, "H_cnt", [128, NE], F32)
        offs = _alloc(es, nc, "H_offs", [128, NE], F32)
        sut = _alloc(es, nc, "H_sut", [128, 128], F32)
        kb.dma("sp", sut.t[:], I["sut"][:, :], writes=[sut])
        kb.op("dve", lambda e: e.memset(zer.t[:], 0.0), writes=[zer])
        kb.op("dve", lambda e: e.tensor_tensor(out=M.t[:], in0=A.t[:], in1=lo.t[:, :].unsqueeze(1).broadcast_to([128, 256, NE]), op=ALU.is_gt),
              reads=[A, lo], writes=[M])
        kb.op("dve", lambda e: e.tensor_tensor(out=wgt.t[:], in0=A.t[:], in1=M.t[:], op=ALU.mult), reads=[A, M], writes=[wgt])
        Mt = M.t[:].rearrange("p j e -> p e j")
        for ex in range(NE):
            kb.op("dve", lambda e, ex=ex: e.tensor_tensor_scan(out=Pin.t[:, ex, :], data0=Mt[:, ex, :], data1=zer.t[:], initial=0.0, op0=ALU.add, op1=ALU.add),
                  reads=[M, zer], writes=[Pin])
        kb.op("dve", lambda e: e.tensor_copy(out=cnt.t[:, :].unsqueeze(2), in_=Pin.t[:, :, 255:256]), reads=[Pin], writes=[cnt])
        kb.op("pe", lambda e: e.matmul(pt.t[:], sut.t[:], cnt.t[:], start=True, stop=True), reads=[sut, cnt], writes=[pt])
        kb.op("dve", lambda e: e.tensor_scalar(out=offs.t[:], in0=pt.t[:], scalar1=-BIG, scalar2=None, op0=ALU.add), reads=[pt], writes=[offs])
        kb.op("dve", lambda e: e.tensor_tensor(out=Pin.t[:], in0=Pin.t[:], in1=Mt, op=ALU.subtract), reads=[Pin, M], writes=[Pin])
        kb.op("dve", lambda e: e.tensor_tensor(out=Pin.t[:], in0=Pin.t[:], in1=offs.t[:, :].unsqueeze(2).broadcast_to([128, NE, 256]), op=ALU.add),
              reads=[Pin, offs], writes=[Pin])
        kb.op("dve", lambda e: e.tensor_tensor(out=Pin.t[:], in0=Pin.t[:], in1=Mt, op=ALU.mult), reads=[Pin, M], writes=[Pin])
        kb.op("dve", lambda e: e.tensor_scalar(out=Pin.t[:], in0=Pin.t[:], scalar1=BIG, scalar2=None, op0=ALU.add), reads=[Pin], writes=[Pin])
        kb.op("dve", lambda e: e.tensor_copy(out=idx.t[:], in_=Pin.t[:]), reads=[Pin], writes=[idx])
        for ex in range(NE):
            kb.dma("pool", g.G[ex][:, :], I["zrows"][:, :], writes=[g.Gbuf[ex]])
        xj = [_alloc(es, nc, "H_xj%d" % i, [128, ROW], BF16) for i in range(4)]
        h2v = S["h2r"].rearrange("(p j) d -> p j d", j=256)
        for j in range(256):
            X = xj[j % 4]
            kb.dma("sp", X.t[:, 0:D], h2v[:, j, :], reads=[g.DBall["h2r"]], writes=[X])
            with nc.allow_non_contiguous_dma(reason="token ids"):
                kb.dma("sp", X.t[:, D + 32:D + 34].bitcast(F32), I["tokid"][:, j:j + 1], writes=[X])
            kb.op("dve", lambda e, j=j: e.tensor_copy(out=X.t[:, D:D + 32].bitcast(F32), in_=wgt.t[:, j, :]), reads=[wgt], writes=[X])
            for ex in range(NE):
                kb.idma(reads=[X, idx], writes=[g.Gbuf[ex]], out=g.G[ex][:, :],
                        out_offset=bass.IndirectOffsetOnAxis(ap=idx.t[:, ex, j:j + 1], axis=0), in_=X.t[:, :], in_offset=None,
                        bounds_check=CAPK - 1, oob_is_err=False)


def stage_I2(g, l):
    nc, kb, I, S = g.nc, g.kb, g.I, g.S
    y = g.y
    NB = 256
    NFC = (DFF + 127) // 128
    with ExitStack() as es:
        wgu = _alloc(es, nc, "I_wgu", [128, 8, 2 * DFF], BF16)
        wd = _alloc(es, nc, "I_wd", [128, NFC, D], BF16)
        Gt = [_alloc(es, nc, "I_G%d" % i, [128, ROW], BF16) for i in range(4)]
        h2 = [_alloc(es, nc, "I_h2%d" % i, [128, 8, NB], BF16) for i in range(2)]
        aT = _alloc(es, nc, "I_aT", [128, NFC, NB], BF16)
        sgt = [_alloc(es, nc, "I_sg%d" % i, [128, NB], F32) for i in range(3)]
        xo = [_alloc(es, nc, "I_x%d" % i, [128, D], F32) for i in range(2)]
        ptr = _alloc(es, nc, "I_ptr", [128, 8, 128], BF16, psum=True)
        pg = [_alloc(es, nc, "I_pg%d" % i, [128, NB], F32, psum=True) for i in range(2)]
        pu = [_alloc(es, nc, "I_pu%d" % i, [128, NB], F32, psum=True) for i in range(2)]
        pq = [_alloc(es, nc, "I_pq%d" % i, [128, 512], F32, psum=True) for i in range(2)]
        n = {"h": 0, "f": 0, "x": 0, "q": 0, "g": 0}
        nblk = int(os.environ.get("KBLK", CAPK // NB))
        for ex in range(NE):
            for kc in range(8):
                kb.dma("pool", wgu.t[:, kc, :], I["w_gate_up"][l, ex, kc * 128:(kc + 1) * 128, :], writes=[wgu])
            for fc in range(NFC):
                fw = min(128, DFF - fc * 128)
                kb.dma("pool", wd.t[0:fw, fc, :], I["w_down"][l, ex, fc * 128:fc * 128 + fw, :], writes=[wd])
            for bk in range(nblk):
                s0 = bk * NB
                hh = h2[n["h"] % 2]
                n["h"] += 1
                gts = []
                for tb in range(NB // 128):
                    G_ = Gt[n["g"] % 4]
                    n["g"] += 1
                    kb.dma("sp", G_.t[:], g.G[ex][s0 + tb * 128:s0 + (tb + 1) * 128, :], reads=[g.Gbuf[ex]], writes=[G_])
                    for kc in range(8):
                        kb.op("pe", lambda e, kc=kc, G_=G_: e.transpose(out=ptr.t[:, kc, :], in_=G_.t[:, kc * 128:(kc + 1) * 128], identity=g.ident_bf.t[:]),
                              reads=[G_, g.ident_bf], writes=[ptr])
                    kb.op("act", lambda e, tb=tb: e.copy(out=hh.t[:, :, tb * 128:(tb + 1) * 128], in_=ptr.t[:]), reads=[ptr], writes=[hh])
                    gts.append(G_)
                for fc in range(NFC):
                    fw = min(128, DFF - fc * 128)
                    p_g = pg[n["f"] % 2]
                    p_u = pu[n["f"] % 2]
                    s_ = sgt[n["f"] % 3]
                    n["f"] += 1
                    for kc in range(8):
                        kb.op("pe", lambda e, kc=kc: e.matmul(p_g.t[0:fw, :], wgu.t[:, kc, fc * 128:fc * 128 + fw], hh.t[:, kc, :], start=(kc == 0), stop=(kc == 7)),
                              reads=[wgu, hh], writes=[p_g])
                    for kc in range(8):
                        kb.op("pe", lambda e, kc=kc: e.matmul(p_u.t[0:fw, :], wgu.t[:, kc, DFF + fc * 128:DFF + fc * 128 + fw], hh.t[:, kc, :], start=(kc == 0), stop=(kc == 7)),
                              reads=[wgu, hh], writes=[p_u])
                    kb.op("act", lambda e: e.activation(out=s_.t[0:fw, :], in_=p_g.t[0:fw, :], func=AF.Sigmoid), reads=[p_g], writes=[s_])
                    kb.op("dve", lambda e: e.tensor_tensor(out=s_.t[0:fw, :], in0=p_g.t[0:fw, :], in1=s_.t[0:fw, :], op=ALU.mult), reads=[p_g, s_], writes=[s_])
                    kb.op("dve", lambda e: e.tensor_tensor(out=aT.t[0:fw, fc, :], in0=p_u.t[0:fw, :], in1=s_.t[0:fw, :], op=ALU.mult), reads=[p_u, s_], writes=[aT])
                for tb in range(NB // 128):
                    G_ = gts[tb]
                    x = xo[n["x"] % 2]
                    n["x"] += 1
                    gate = G_.t[:, D:D + 32].bitcast(F32)[:, ex:ex + 1]
                    for hf in range(2):
                        p = pq[n["q"] % 2]
                        n["q"] += 1
                        for fc in range(NFC):
                            fw = min(128, DFF - fc * 128)
                            kb.op("pe", lambda e, fc=fc, fw=fw: e.matmul(p.t[:], aT.t[0:fw, fc, tb * 128:(tb + 1) * 128], wd.t[0:fw, fc, hf * 512:(hf + 1) * 512],
                                                                        start=(fc == 0), stop=(fc == NFC - 1)), reads=[aT, wd], writes=[p])
                        kb.op("dve", lambda e: e.tensor_scalar(out=x.t[:, hf * 512:(hf + 1) * 512], in0=p.t[:], scalar1=gate, scalar2=None, op0=ALU.mult),
                              reads=[p, G_], writes=[x])
                    kb.idma(reads=[x, G_], writes=[g.ybuf], out=y[:, :],
                            out_offset=bass.IndirectOffsetOnAxis(ap=G_.t[:, D + 32:D + 34].bitcast(U32), axis=0), in_=x.t[:, :], in_offset=None,
                            bounds_check=GT - 1, oob_is_err=False, compute_op=ALU.add)


def host_consts():
    c = {}
    c["ident"] = np.eye(128, dtype=np.float32)
    c["ones"] = np.ones((128, 128), np.float32)
    bo = np.zeros((128, 128), np.float32)
    bo[:64, :64] = 1
    bo[64:, 64:] = 1
    c["bones"] = bo
    s = np.arange(128)[:, None]
    t = np.arange(128)[None, :]
    same = (s // 64) == (t // 64)
    c["trif"] = (same & (s <= t)).astype(np.float32)
    c["trib"] = (same & (s >= t)).astype(np.float32)
    return c


def host_smask(typ):
    m = np.zeros((8, 16), np.float32)
    for i, r in enumerate(range(60, 68)):
        r0 = na_window(r, typ)
        for j, rr in enumerate(range(56, 72)):
            m[i, j] = 0.0 if (r0 <= rr < r0 + 8) else NEG
    return np.ascontiguousarray(np.broadcast_to(m.reshape(1, 128), (128, 128))).astype(np.float32)


def host_bt(rel_bias):
    out = np.zeros((DEPTH, 128, 4, 31, 64), np.float32)
    qc = np.arange(64)
    c0 = np.clip(qc - 8, 0, 48)
    kc = np.arange(64)
    inwin = (kc[:, None] >= c0[None, :]) & (kc[:, None] < c0[None, :] + 16)
    dc = np.clip(kc[:, None] - qc[None, :] + 15, 0, 30)
    for c in range(4):
        for hh in range(2):
            h = 2 * c + hh
            for dr in range(15):
                gath = rel_bias[:, h, dr][:, dc]
                out[:, hh * 64:(hh + 1) * 64, c, dr + 8, :] = np.where(inwin[None], gath, np.float32(NEG))
    return np.ascontiguousarray(out.reshape(DEPTH, 128, 4 * 31 * 64))


_NC_CACHE = {}


def _run(inp, cores):
    if "nc" not in _NC_CACHE:
        _NC_CACHE["nc"] = build_program()
    nc = _NC_CACHE["nc"]
    used = list(nc._k_inputs.keys())
    consts = host_consts()
    bt = host_bt(np.asarray(inp["na_rel_bias"], dtype=np.float32))
    in_maps = []
    for c in cores:
        m = {}
        if c == 0:
            m["x"] = np.ascontiguousarray(inp["x_prompt"].reshape(GT, D))
            m["mem"] = np.ascontiguousarray(inp["mem_prompt"].reshape(NCH, 2, 256, D))
            typ = 0
        else:
            m["x"] = np.ascontiguousarray(inp["x_sample"].reshape(GT, D))
            m["mem"] = np.ascontiguousarray(np.stack([inp["mem_sample"], inp["mem_sample"]], axis=1))
            typ = 1
        m["cflag"] = np.full((128, 1), float(typ), np.float32)
        m["smask"] = host_smask(typ)
        m["bt"] = bt
        m["tokid"] = (np.arange(128, dtype=np.uint32)[:, None] * 256 + np.arange(256, dtype=np.uint32)[None, :]).view(np.float32)
        m["zrows"] = np.zeros((CAPK, ROW), np.float32)
        m["sut"] = (np.arange(128)[:, None] < np.arange(128)[None, :]).astype(np.float32)
        m.update(consts)
        mm = {}
        for k in used:
            mm[k] = np.ascontiguousarray(m[k] if k in m else inp[k], dtype=np.float32)
        in_maps.append(mm)
    res = run_bass_kernel_spmd(nc, in_maps, core_ids=list(range(len(cores))))
    return res


def kernel(**inputs):
    inp = {k: np.asarray(v) for k, v in inputs.items()}
    res = _run(inp, [0, 1])
    ys = [np.asarray(r["y"]) for r in res.results]
    y_prompt = ys[0].reshape(8, 4096, D).astype(np.float32)
    y_sample = ys[1].reshape(4, 8192, D).astype(np.float32)
    return (y_prompt, y_sample)
```

```python
import os
import numpy as np
import concourse.bass as bass
import concourse.mybir as mybir
from concourse.bass_utils import run_bass_kernel_spmd
from contextlib import ExitStack

F32 = mybir.dt.float32
BF16 = mybir.dt.bfloat16
U32 = mybir.dt.uint32
AF = mybir.ActivationFunctionType
ALU = mybir.AluOpType
AX = mybir.AxisListType

T = 8192
ST = 512
NST = T // ST
NT = T // 128
D = 1024
DIN = 8192
DEPTH = 2
NE = 16
DFF = 2752
CAPK = 4096
EPS = 1e-6
NEG = -30000.0
NBIS = 26
NCH = 4
GT = NCH * T
ROW = 1060
BIG = 1.0e6


class Buf:
    __slots__ = ("w", "r")

    def __init__(self):
        self.w = None
        self.r = {}


class TT:
    __slots__ = ("t", "b")

    def __init__(self, t):
        self.t = t
        self.b = Buf()


class KB:
    def __init__(self, nc, es):
        self.nc = nc
        self.E = {"pe": nc.tensor, "dve": nc.vector, "act": nc.scalar, "pool": nc.gpsimd, "sp": nc.sync}
        self.semh = {}
        self.cnt = {}
        for e in ("pe", "dve", "act", "pool"):
            self.semh[e] = es.enter_context(nc.semaphore("s_" + e))
            self.cnt[e] = 0
        self.nd = 26
        self.qsems = {"sp": list(range(0, 16)), "pool": list(range(16, 26))}
        self.qnext = {"sp": 0, "pool": 0}
        self.dcnt = [0] * self.nd
        for i in range(self.nd):
            self.semh[("d", i)] = es.enter_context(nc.semaphore("d%d" % i))
        self.seen = {e: {} for e in self.E}
        self.ninst = 0
        self.bregs = {}

    def _deps(self, reads, writes):
        deps = {}
        for b in reads:
            if b.w is not None and deps.get(b.w[0], 0) < b.w[1]:
                deps[b.w[0]] = b.w[1]
        for b in writes:
            if b.w is not None and deps.get(b.w[0], 0) < b.w[1]:
                deps[b.w[0]] = b.w[1]
            for k, v in b.r.items():
                if deps.get(k, 0) < v:
                    deps[k] = v
        return deps

    def _wait(self, eng, deps):
        E = self.E[eng]
        seen = self.seen[eng]
        for k, v in deps.items():
            if eng == "pe" and k == "pe":
                continue
            if seen.get(k, 0) >= v:
                continue
            E.wait_ge(self.semh[k], v)
            seen[k] = v

    def op(self, eng, fn, reads=(), writes=()):
        reads = [x.b if isinstance(x, TT) else x for x in reads]
        writes = [x.b if isinstance(x, TT) else x for x in writes]
        self._wait(eng, self._deps(reads, writes))
        inst = fn(self.E[eng])
        self.cnt[eng] += 1
        c = self.cnt[eng]
        inst.then_inc(self.semh[eng], 1)
        for b in reads:
            b.r[eng] = c
        for b in writes:
            b.w = (eng, c)
            b.r = {}
        self.ninst += 1
        return inst

    def dma(self, q, out, in_, reads=(), writes=(), **kw):
        reads = [x.b if isinstance(x, TT) else x for x in reads]
        writes = [x.b if isinstance(x, TT) else x for x in writes]
        i = self.next_dsem(q)
        key = ("d", i)
        deps = self._deps(reads, writes)
        if self.dcnt[i] > 0 and deps.get(key, 0) < self.dcnt[i]:
            deps[key] = self.dcnt[i]
        self._wait(q, deps)
        inst = self.E[q].dma_start(out=out, in_=in_, **kw)
        self.dcnt[i] += 16
        inst.then_inc(self.semh[key], 16)
        for b in reads:
            b.r[key] = self.dcnt[i]
        for b in writes:
            b.w = (key, self.dcnt[i])
            b.r = {}
        self.ninst += 1
        return inst

    def idma(self, reads=(), writes=(), **kw):
        reads = [x.b if isinstance(x, TT) else x for x in reads]
        writes = [x.b if isinstance(x, TT) else x for x in writes]
        i = self.next_dsem("pool")
        key = ("d", i)
        deps = self._deps(reads, writes)
        if self.dcnt[i] > 0 and deps.get(key, 0) < self.dcnt[i]:
            deps[key] = self.dcnt[i]
        self._wait("pool", deps)
        bc = kw.pop("bounds_check")
        if bc not in self.bregs:
            self.bregs[bc] = self.nc.gpsimd.to_reg(bc)
        inst = self.nc.gpsimd.indirect_dma_start(bounds_check=self.bregs[bc], **kw)
        self.dcnt[i] += 16
        inst.then_inc(self.semh[key], 16)
        for b in reads:
            b.r[key] = self.dcnt[i]
        for b in writes:
            b.w = (key, self.dcnt[i])
            b.r = {}
        self.ninst += 1
        return inst

    def next_dsem(self, q):
        lst = self.qsems[q]
        i = lst[self.qnext[q] % len(lst)]
        self.qnext[q] += 1
        return i

    def barrier(self):
        allk = {e: self.cnt[e] for e in self.cnt if self.cnt[e] > 0}
        for i in range(self.nd):
            if self.dcnt[i] > 0:
                allk[("d", i)] = self.dcnt[i]
        for eng in self.E:
            self._wait(eng, {k: v for k, v in allk.items() if k != eng})


class Ctx:
    pass


_UID = [0]


def _alloc(es, nc, name, shape, dt, psum=False):
    f = nc.psum_tensor if psum else nc.sbuf_tensor
    _UID[0] += 1
    return TT(es.enter_context(f("t%d_%s" % (_UID[0], name), shape, dt)))


def build_program():
    nc = bass.Bass("TRN2", target_bir_lowering=False)
    g = Ctx()
    g.nc = nc

    debug = os.environ.get("KDEBUG", "").split(",")

    def dscr(name, shape, dt):
        dbg = ("1" in debug) or (name in debug)
        return nc.dram_tensor(name, shape, dt, kind=("ExternalOutput" if dbg else "Internal")).ap()

    ishapes = {
        "x": [GT, D], "mem": [NCH, 2, 256, D], "tokid": [128, 256], "zrows": [CAPK, ROW], "sut": [128, 128], "cflag": [128, 1], "smask": [128, 128], "bt": [DEPTH, 128, 4 * 31 * 64],
        "ident": [128, 128], "ones": [128, 128], "bones": [128, 128], "trif": [128, 128], "trib": [128, 128],
        "norm_mix": [DEPTH, D], "w_in": [DEPTH, D, DIN], "na_q_norm": [DEPTH, 64], "na_k_norm": [DEPTH, 64],
        "w_na_br": [DEPTH, 512, D], "hg_lb": [2, DEPTH, 512], "hg_out_norm": [DEPTH, 128],
        "w_hg_br": [DEPTH, 512, D], "conv_w": [DEPTH, 31, 512], "conv_b": [DEPTH, 512],
        "conv_ln_g": [DEPTH, 512], "conv_ln_b": [DEPTH, 512], "w_cv_br": [DEPTH, 512, D],
        "w_out": [DEPTH, D, D], "norm_mem": [DEPTH, D], "mem_kv_norm": [DEPTH, D], "wq_mem": [DEPTH, D, 512],
        "wkv_mem": [DEPTH, D, D], "mem_q_norm": [DEPTH, 128], "mem_k_norm": [DEPTH, 128],
        "wo_mem": [DEPTH, 512, D], "norm_ffn": [DEPTH, D], "w_router": [DEPTH, D, NE],
        "w_gate_up": [DEPTH, NE, D, 2 * DFF], "w_down": [DEPTH, NE, DFF, D],
    }

    class LazyIn(dict):
        def __missing__(self, k):
            ap = nc.dram_tensor(k, ishapes[k], F32, kind="ExternalInput").ap()
            self[k] = ap
            return ap

    I = LazyIn()
    y = nc.dram_tensor("y", [GT, D], F32, kind="ExternalOutput").ap()
    S = {}
    S["qT"] = dscr("qT_d", [512, T], BF16)
    S["kT"] = dscr("kT_d", [512, T], BF16)
    S["v"] = dscr("v_d", [T, 512], BF16)
    S["qh"] = dscr("qh_d", [512, T], BF16)
    S["lf"] = dscr("lf_d", [2, 512, T], F32)
    S["kk"] = dscr("kk_d", [2, 512, T], BF16)
    S["vh"] = dscr("vh_d", [T, 512], BF16)
    S["gs"] = dscr("gs_d", [512, T], BF16)
    S["u"] = dscr("u_d", [512, T], F32)
    S["sg"] = dscr("sg_d", [3072, T], BF16)
    S["naT"] = dscr("naT_d", [512, T], BF16)
    S["of"] = dscr("of_d", [512, T], F32)
    S["hgT"] = dscr("hgT_d", [512, T], BF16)
    S["cvT"] = dscr("cvT_d", [512, T], BF16)
    S["h2T"] = dscr("h2T_d", [D, T], BF16)
    S["affall"] = dscr("affall_d", [GT, NE], F32)
    S["h2r"] = dscr("h2r_d", [GT, D], BF16)
    g.G = [nc.dram_tensor("G%d" % e, [CAPK, ROW], BF16, kind="Internal").ap() for e in range(NE)]
    g.Gbuf = [Buf() for _ in range(NE)]
    g.ybuf = Buf()
    g.I, g.S, g.y = I, S, y
    nc._k_inputs = I
    nc._k_scratch = S
    g.DBall = {k: Buf() for k in ["affall", "h2r"]}

    g.lim = int(os.environ.get("KLIM", "0"))
    with ExitStack() as es:
        kb = KB(nc, es)
        g.kb = kb
        g.ident_bf = _alloc(es, nc, "ident_bf", [128, 128], BF16)
        g.ident_f = _alloc(es, nc, "ident_f", [128, 128], F32)
        g.ones_f = _alloc(es, nc, "ones_f", [128, 128], F32)
        g.ones_bf = _alloc(es, nc, "ones_bf", [128, 128], BF16)
        g.bones_f = _alloc(es, nc, "bones_f", [128, 128], F32)
        g.trif = _alloc(es, nc, "trif", [128, 128], F32)
        g.trib = _alloc(es, nc, "trib", [128, 128], F32)
        g.cflag = _alloc(es, nc, "cflag_sb", [128, 1], F32)
        g.wg = _alloc(es, nc, "wg_sb", [128, NT, NE], F32)
        kb.dma("pool", g.ident_bf.t[:], I["ident"][:, :], writes=[g.ident_bf])
        kb.dma("sp", g.ident_f.t[:], I["ident"][:, :], writes=[g.ident_f])
        kb.dma("sp", g.ones_f.t[:], I["ones"][:, :], writes=[g.ones_f])
        kb.dma("pool", g.ones_bf.t[:], I["ones"][:, :], writes=[g.ones_bf])
        kb.dma("sp", g.bones_f.t[:], I["bones"][:, :], writes=[g.bones_f])
        kb.dma("sp", g.trif.t[:], I["trif"][:, :], writes=[g.trif])
        kb.dma("sp", g.trib.t[:], I["trib"][:, :], writes=[g.trib])
        kb.dma("sp", g.cflag.t[:], I["cflag"][:, :], writes=[g.cflag])

        only = os.environ.get("KONLY", "")
        only = set(only.split(",")) if only else None
        nlayers = int(os.environ.get("KLAYERS", DEPTH))
        nch = int(os.environ.get("KCHUNKS", NCH))
        for l in range(nlayers):
            for k in range(nch):
                g.DB = {kk: [Buf() for _ in range(NST)] for kk in list(S.keys()) + ["y", "aff"]}
                xsrc = (I["x"] if l == 0 else y)[k * T:(k + 1) * T, :]
                g.yc = y[k * T:(k + 1) * T, :]
                g.memc = I["mem"][k]
                g.affc = S["affall"][k * T:(k + 1) * T, :]
                g.h2rc = S["h2r"][k * T:(k + 1) * T, :]
                plan = [("A0", lambda: stage_A(g, l, xsrc, 0)), ("A1", lambda: stage_A(g, l, xsrc, 1)), ("B", lambda: stage_B(g, l)),
                        ("C", lambda: stage_C(g, l)), ("D", lambda: stage_D(g, l)), ("E", lambda: stage_E(g, l, xsrc)),
                        ("F", lambda: stage_F(g, l))]
                for nm, fn in plan:
                    if only is not None and nm not in only:
                        continue
                    fn()
                    kb.barrier()
                print("stage", l, k, "A-F ninst", kb.ninst, flush=True)
            for nm, fn in (("H", lambda: stage_H2(g, l)), ("I", lambda: stage_I2(g, l))):
                if only is not None and nm not in only:
                    continue
                fn()
                kb.barrier()
                print("stage", l, nm, "ninst", kb.ninst, flush=True)
        kb.barrier()
    return nc


def load_bcast_row(g, es, name, src_row, n):
    t = _alloc(es, g.nc, name, [128, n], F32)
    g.kb.dma("sp", t.t[:], src_row.partition_broadcast(128), writes=[t])
    return t


def load_col(g, es, name, src_vec, nchunks):
    t = _alloc(es, g.nc, name, [128, nchunks], F32)
    with g.nc.allow_non_contiguous_dma(reason="tiny param load"):
        g.kb.dma("sp", t.t[:], src_vec.rearrange("(c p) -> p c", p=128), writes=[t])
    return t


def rms_rows(g, xa, ss, rstd, junk, ntb):
    kb = g.kb
    for tb in range(ntb):
        kb.op("act", lambda e, tb=tb: e.activation(out=junk.t[:], in_=xa[tb].t[:], func=AF.Square,
                                                  accum_out=ss.t[:, tb:tb + 1]),
              reads=[xa[tb]], writes=[junk, ss])
    kb.op("act", lambda e: e.activation(out=rstd.t[:, 0:ntb], in_=ss.t[:, 0:ntb], func=AF.Ln, scale=1.0 / D, bias=EPS),
          reads=[ss], writes=[rstd])
    kb.op("act", lambda e: e.activation(out=rstd.t[:, 0:ntb], in_=rstd.t[:, 0:ntb], func=AF.Exp, scale=-0.5),
          reads=[rstd], writes=[rstd])


def norm_transpose(g, src, st, gbc, W, out_hT, also_store=None):
    kb = g.kb
    t0 = st * ST
    for tb in range(4):
        kb.dma("sp", W.xa[tb].t[:], src[t0 + tb * 128:t0 + (tb + 1) * 128, :],
               reads=[g.DB["y"][st]], writes=[W.xa[tb]])
    rms_rows(g, W.xa, W.ss, W.rstd, W.junk, 4)
    for tb in range(4):
        kb.op("dve", lambda e, tb=tb: e.scalar_tensor_tensor(out=W.hb[tb].t[:], in0=W.xa[tb].t[:],
                                                            scalar=W.rstd.t[:, tb:tb + 1], in1=gbc.t[:],
                                                            op0=ALU.mult, op1=ALU.mult),
              reads=[W.xa[tb], W.rstd, gbc], writes=[W.hb[tb]])
        for kc in range(8):
            kb.op("pe", lambda e, tb=tb, kc=kc: e.transpose(out=W.ptr.t[:, kc, :], in_=W.hb[tb].t[:, kc * 128:(kc + 1) * 128],
                                                           identity=g.ident_bf.t[:]),
                  reads=[W.hb[tb], g.ident_bf], writes=[W.ptr])
        kb.op("act", lambda e, tb=tb: e.copy(out=out_hT.t[:, :, tb * 128:(tb + 1) * 128], in_=W.ptr.t[:]),
              reads=[W.ptr], writes=[out_hT])


class NormWork:
    def __init__(self, g, es, pfx):
        nc = g.nc
        self.xa = [_alloc(es, nc, pfx + "xa%d" % i, [128, D], F32) for i in range(4)]
        self.hb = [_alloc(es, nc, pfx + "hb%d" % i, [128, D], BF16) for i in range(2)] * 2
        self.ss = _alloc(es, nc, pfx + "ss", [128, 4], F32)
        self.rstd = _alloc(es, nc, pfx + "rstd", [128, 4], F32)
        self.junk = _alloc(es, nc, pfx + "junk", [128, D], BF16)
        self.ptr = _alloc(es, nc, pfx + "ptr", [128, 8, 128], BF16, psum=True)


def stage_A(g, l, xsrc, half):
    nc, kb, I, S = g.nc, g.kb, g.I, g.S
    with ExitStack() as es:
        W = NormWork(g, es, "A")
        gbc = load_bcast_row(g, es, "A_gbc", I["norm_mix"][l, :], D)
        hT = _alloc(es, nc, "A_hT", [128, 8, ST], BF16)
        wsb = _alloc(es, nc, "A_w", [128, 8, 4096], BF16)
        for kc in range(8):
            kb.dma("pool", wsb.t[:, kc, :], I["w_in"][l, kc * 128:(kc + 1) * 128, half * 4096:(half + 1) * 4096],
                   writes=[wsb])
        ps = [_alloc(es, nc, "A_ps%d" % i, [128, ST], F32, psum=True) for i in range(4)]
        ps2 = [_alloc(es, nc, "A_ps2%d" % i, [128, ST], F32, psum=True) for i in range(2)]
        f32t = [_alloc(es, nc, "A_f%d" % i, [128, ST], F32) for i in range(6)]
        bft = [_alloc(es, nc, "A_b%d" % i, [128, ST], BF16) for i in range(6)]
        cnt = {"ps": 0, "ps2": 0, "f": 0, "b": 0}

        def nxt(lst, key):
            r = lst[cnt[key] % len(lst)]
            cnt[key] += 1
            return r

        if half == 0:
            qg = _alloc(es, nc, "A_qg", [128, 2], F32)
            with nc.allow_non_contiguous_dma(reason="tiny"):
                for hh in range(2):
                    kb.dma("sp", qg.t[hh * 64:(hh + 1) * 64, 0:1], I["na_q_norm"][l, :].rearrange("(p o) -> p o", o=1), writes=[qg])
                    kb.dma("sp", qg.t[hh * 64:(hh + 1) * 64, 1:2], I["na_k_norm"][l, :].rearrange("(p o) -> p o", o=1), writes=[qg])
            kb.op("dve", lambda e: e.tensor_scalar(out=qg.t[:, 0:1], in0=qg.t[:, 0:1], scalar1=0.125, scalar2=None, op0=ALU.mult),
                  reads=[qg], writes=[qg])
            lbv = _alloc(es, nc, "A_lb", [128, 8], F32)
            oml = _alloc(es, nc, "A_oml", [128, 8], F32)
            noml = _alloc(es, nc, "A_noml", [128, 8], F32)
            if l == 0:
                kb.op("dve", lambda e: e.memset(lbv.t[:], 0.0), writes=[lbv])
            else:
                b0 = _alloc(es, nc, "A_b0", [128, 8], F32)
                b1 = _alloc(es, nc, "A_b1", [128, 8], F32)
                with nc.allow_non_contiguous_dma(reason="tiny"):
                    for d_ in range(2):
                        kb.dma("sp", b0.t[:, d_ * 4:(d_ + 1) * 4], I["hg_lb"][d_, 0, :].rearrange("(c p) -> p c", p=128), writes=[b0])
                        kb.dma("sp", b1.t[:, d_ * 4:(d_ + 1) * 4], I["hg_lb"][d_, 1, :].rearrange("(c p) -> p c", p=128), writes=[b1])
                kb.op("dve", lambda e: e.tensor_tensor(out=b1.t[:], in0=b1.t[:], in1=b0.t[:], op=ALU.subtract), reads=[b0, b1], writes=[b1])
                kb.op("act", lambda e: e.activation(out=lbv.t[:], in_=b1.t[:], func=AF.Sigmoid), reads=[b1], writes=[lbv])
            kb.op("dve", lambda e: e.tensor_scalar(out=oml.t[:], in0=lbv.t[:], scalar1=-1.0, scalar2=1.0, op0=ALU.mult, op1=ALU.add),
                  reads=[lbv], writes=[oml])
            kb.op("dve", lambda e: e.tensor_scalar(out=noml.t[:], in0=oml.t[:], scalar1=-1.0, scalar2=None, op0=ALU.mult),
                  reads=[oml], writes=[noml])

        def proj_fm(cb):
            p = nxt(ps, "ps")
            for kc in range(8):
                kb.op("pe", lambda e, kc=kc: e.matmul(p.t[:], wsb.t[:, kc, cb * 128:(cb + 1) * 128], hT.t[:, kc, :],
                                                     start=(kc == 0), stop=(kc == 7)),
                      reads=[wsb, hT], writes=[p])
            return p

        def store(q, dst, src_tt, dbufs):
            kb.dma(q, dst, src_tt.t[:], reads=[src_tt], writes=dbufs)

        for st in range(NST):
            t0 = st * ST
            norm_transpose(g, xsrc, st, gbc, W, hT)
            if half == 0:
                for cb in range(8):
                    p = proj_fm(cb)
                    sq = nxt(f32t, "f")
                    kb.op("act", lambda e: e.activation(out=sq.t[:], in_=p.t[:], func=AF.Square), reads=[p], writes=[sq])
                    p2 = nxt(ps2, "ps2")
                    kb.op("pe", lambda e: e.matmul(p2.t[:], g.bones_f.t[:], sq.t[:], start=True, stop=True),
                          reads=[g.bones_f, sq], writes=[p2])
                    rs = nxt(f32t, "f")
                    kb.op("act", lambda e: e.activation(out=rs.t[:], in_=p2.t[:], func=AF.Ln, scale=1.0 / 64, bias=EPS),
                          reads=[p2], writes=[rs])
                    kb.op("act", lambda e: e.activation(out=rs.t[:], in_=rs.t[:], func=AF.Exp, scale=-0.5), reads=[rs], writes=[rs])
                    ob = nxt(bft, "b")
                    gi = 0 if cb < 4 else 1
                    kb.op("dve", lambda e: e.scalar_tensor_tensor(out=ob.t[:], in0=p.t[:], scalar=qg.t[:, gi:gi + 1], in1=rs.t[:],
                                                                 op0=ALU.mult, op1=ALU.mult),
                          reads=[p, qg, rs], writes=[ob])
                    dn = "qT" if cb < 4 else "kT"
                    c = cb % 4
                    store("sp", S[dn][c * 128:(c + 1) * 128, t0:t0 + ST], ob, [g.DB[dn][st]])
                for (c0, dn) in ((1024, "v"), (3072, "vh")):
                    for tb in range(4):
                        p = nxt(ps, "ps")
                        for kc in range(8):
                            kb.op("pe", lambda e, kc=kc: e.matmul(p.t[:], hT.t[:, kc, tb * 128:(tb + 1) * 128],
                                                                 wsb.t[:, kc, c0:c0 + 512], start=(kc == 0), stop=(kc == 7)),
                                  reads=[wsb, hT], writes=[p])
                        ob = nxt(bft, "b")
                        kb.op("act", lambda e: e.copy(out=ob.t[:], in_=p.t[:]), reads=[p], writes=[ob])
                        store("sp", S[dn][t0 + tb * 128:t0 + (tb + 1) * 128, :], ob, [g.DB[dn][st]])
                for cb in list(range(12, 16)) + list(range(28, 32)):
                    p = proj_fm(cb)
                    sgm = nxt(f32t, "f")
                    kb.op("act", lambda e: e.activation(out=sgm.t[:], in_=p.t[:], func=AF.Sigmoid), reads=[p], writes=[sgm])
                    ob = nxt(bft, "b")
                    sc = (128.0 ** -0.5) if cb < 16 else 1.0
                    kb.op("dve", lambda e: e.scalar_tensor_tensor(out=ob.t[:], in0=p.t[:], scalar=sc, in1=sgm.t[:],
                                                                 op0=ALU.mult, op1=ALU.mult),
                          reads=[p, sgm], writes=[ob])
                    dn = "qh" if cb < 16 else "gs"
                    c = cb % 4
                    store("sp", S[dn][c * 128:(c + 1) * 128, t0:t0 + ST], ob, [g.DB[dn][st]])
                for cb in range(16, 24):
                    d_ = (cb - 16) // 4
                    h = cb % 4
                    j = d_ * 4 + h
                    p = proj_fm(cb)
                    sgm = nxt(f32t, "f")
                    kb.op("act", lambda e: e.activation(out=sgm.t[:], in_=p.t[:], func=AF.Sigmoid), reads=[p], writes=[sgm])
                    ff = nxt(f32t, "f")
                    kb.op("dve", lambda e: e.tensor_scalar(out=ff.t[:], in0=sgm.t[:], scalar1=oml.t[:, j:j + 1], scalar2=lbv.t[:, j:j + 1],
                                                          op0=ALU.mult, op1=ALU.add), reads=[sgm, oml, lbv], writes=[ff])
                    kb.op("pool", lambda e: e.tensor_scalar(out=ff.t[:], in0=ff.t[:], scalar1=1e-20, scalar2=None, op0=ALU.max), reads=[ff], writes=[ff])
                    kb.op("act", lambda e: e.activation(out=ff.t[:], in_=ff.t[:], func=AF.Ln), reads=[ff], writes=[ff])
                    store("sp", S["lf"][d_, h * 128:(h + 1) * 128, t0:t0 + ST], ff, [g.DB["lf"][st]])
                    ob = nxt(bft, "b")
                    kb.op("dve", lambda e: e.tensor_scalar(out=ob.t[:], in0=sgm.t[:], scalar1=noml.t[:, j:j + 1], scalar2=oml.t[:, j:j + 1],
                                                          op0=ALU.mult, op1=ALU.add), reads=[sgm, noml, oml], writes=[ob])
                    store("sp", S["kk"][d_, h * 128:(h + 1) * 128, t0:t0 + ST], ob, [g.DB["kk"][st]])
            else:
                for c in range(4):
                    pb = proj_fm(4 + c)
                    sgm = nxt(f32t, "f")
                    kb.op("act", lambda e: e.activation(out=sgm.t[:], in_=pb.t[:], func=AF.Sigmoid), reads=[pb], writes=[sgm])
                    pa = proj_fm(c)
                    uu = nxt(f32t, "f")
                    kb.op("dve", lambda e: e.tensor_tensor(out=uu.t[:], in0=pa.t[:], in1=sgm.t[:], op=ALU.mult), reads=[pa, sgm], writes=[uu])
                    store("sp", S["u"][c * 128:(c + 1) * 128, t0:t0 + ST], uu, [g.DB["u"][st]])
                for cb in range(8, 32):
                    p = proj_fm(cb)
                    ob = nxt(bft, "b")
                    kb.op("act", lambda e: e.activation(out=ob.t[:], in_=p.t[:], func=AF.Sigmoid), reads=[p], writes=[ob])
                    store("sp", S["sg"][(cb - 8) * 128:(cb - 7) * 128, t0:t0 + ST], ob, [g.DB["sg"][st]])


def na_window(r, typ):
    if typ == 0:
        base = (r // 64) * 64
        r0 = min(max((r % 64) - 4, 0), 56) + base
    else:
        r0 = min(max(r - 4, 0), 120)
    return r0


def stage_B(g, l):
    nc, kb, I, S = g.nc, g.kb, g.I, g.S
    NR = 24
    with ExitStack() as es:
        bt = _alloc(es, nc, "B_bt", [128, 4, 31, 64], F32)
        kb.dma("sp", bt.t[:], I["bt"][l].rearrange("p (c e q) -> p c e q", c=4, e=31), writes=[bt])
        smask = _alloc(es, nc, "B_sm", [128, 128], F32)
        kb.dma("sp", smask.t[:], I["smask"][:, :], writes=[smask])
        kbd = [_alloc(es, nc, "B_k%d" % i, [128, 4, 128], BF16) for i in range(NR)]
        vbd = [_alloc(es, nc, "B_v%d" % i, [128, 4, 130], BF16) for i in range(NR)]
        qr = [_alloc(es, nc, "B_q%d" % i, [128, 4, 64], BF16) for i in range(6)]
        for i in range(NR):
            kb.op("pool", lambda e, i=i: e.memset(kbd[i].t[:], 0.0), writes=[kbd[i]])
            kb.op("pool", lambda e, i=i: e.memset(vbd[i].t[:], 0.0), writes=[vbd[i]])
            kb.op("pool", lambda e, i=i: e.memset(vbd[i].t[0:64, :, 64:65], 1.0), writes=[vbd[i]])
            kb.op("pool", lambda e, i=i: e.memset(vbd[i].t[64:128, :, 129:130], 1.0), writes=[vbd[i]])
        pss = [_alloc(es, nc, "B_ps%d" % i, [128, 8, 64], F32, psum=True) for i in range(5)]
        pos = [_alloc(es, nc, "B_po%d" % i, [128, 512], F32, psum=True) for i in range(2)]
        ptt = _alloc(es, nc, "B_pt", [128, 4, 64], BF16, psum=True)
        sct = [_alloc(es, nc, "B_sc%d" % i, [128, 8, 64], F32) for i in range(4)]
        ptl = [_alloc(es, nc, "B_pT%d" % i, [128, 8, 64], BF16) for i in range(8)]
        rcp = [_alloc(es, nc, "B_rc%d" % i, [64, 4], F32) for i in range(4)]
        obf = [_alloc(es, nc, "B_ob%d" % i, [64, 512], BF16) for i in range(2)]
        outb = [_alloc(es, nc, "B_out%d" % i, [128, 4, ST], BF16) for i in range(2)]
        kTv = S["kT"].rearrange("(c p) t -> p c t", p=128)
        qTv = S["qT"].rearrange("(c p) t -> p c t", p=128)
        vv = S["v"].rearrange("t (c x) -> t c x", x=128)
        loaded = [-1]
        cn = {"ps": 0, "sc": 0, "pT": 0, "po": 0}

        def load_row(rr):
            sl = rr % NR
            st = (rr * 64) // ST
            cs = slice(rr * 64, rr * 64 + 64)
            with nc.allow_non_contiguous_dma(reason="row blocks"):
                kb.dma("sp", kbd[sl].t[0:64, :, 0:64], kTv[0:64, :, cs], reads=[g.DB["kT"][st]], writes=[kbd[sl]])
                kb.dma("sp", kbd[sl].t[64:128, :, 64:128], kTv[64:128, :, cs], reads=[g.DB["kT"][st]], writes=[kbd[sl]])
                kb.dma("pool", vbd[sl].t[0:64, :, 0:64], vv[cs, :, 0:64], reads=[g.DB["v"][st]], writes=[vbd[sl]])
                kb.dma("pool", vbd[sl].t[64:128, :, 65:129], vv[cs, :, 64:128], reads=[g.DB["v"][st]], writes=[vbd[sl]])

        for r in range(g.lim if g.lim else 128):
            special = 60 <= r <= 67
            if special:
                krows = list(range(56, 72))
            else:
                r0 = na_window(r, 1)
                krows = list(range(r0, r0 + 8))
            while loaded[0] < krows[-1]:
                loaded[0] += 1
                load_row(loaded[0])
            q = qr[r % len(qr)]
            with nc.allow_non_contiguous_dma(reason="row blocks"):
                kb.dma("sp", q.t[:], qTv[:, :, r * 64:(r + 1) * 64], reads=[g.DB["qT"][(r * 64) // ST]], writes=[q])
            groups = [krows[i:i + 8] for i in range(0, len(krows), 8)]
            pot = [pos[0], pos[1]]
            allpts = []
            for c in range(4):
                pts = []
                for gi, grp in enumerate(groups):
                    p = pss[cn["ps"] % len(pss)]
                    cn["ps"] += 1
                    for j, rr in enumerate(grp):
                        kb.op("pe", lambda e, j=j, rr=rr, p=p, c=c: e.matmul(p.t[:, j, :], kbd[rr % NR].t[:, c, :], q.t[:, c, :], start=True, stop=True),
                              reads=[kbd[rr % NR], q], writes=[p])
                    sc = sct[cn["sc"] % len(sct)]
                    cn["sc"] += 1
                    e0 = (grp[0] - r + 7) + 8
                    kb.op("dve", lambda e, sc=sc, p=p, c=c, e0=e0: e.tensor_tensor(out=sc.t[:], in0=p.t[:], in1=bt.t[:, c, e0:e0 + 8, :], op=ALU.add),
                          reads=[p, bt], writes=[sc])
                    if special:
                        a = (r - 60) * 16 + gi * 8
                        kb.op("dve", lambda e, sc=sc, a=a: e.tensor_tensor(out=sc.t[:], in0=sc.t[:],
                                                                          in1=smask.t[:, a:a + 8].unsqueeze(2).broadcast_to([128, 8, 64]), op=ALU.add),
                              reads=[sc, smask], writes=[sc])
                    pT = ptl[cn["pT"] % len(ptl)]
                    cn["pT"] += 1
                    kb.op("act", lambda e, pT=pT, sc=sc: e.activation(out=pT.t[:], in_=sc.t[:], func=AF.Exp), reads=[sc], writes=[pT])
                    pts.append((pT, grp))
                allpts.append(pts)
            for c in range(4):
                pts = allpts[c]
                po = pot[c // 2]
                n_mm = sum(len(grp) for _, grp in pts)
                i_mm = 0
                for pT, grp in pts:
                    for j, rr in enumerate(grp):
                        kb.op("pe", lambda e, j=j, rr=rr, i_mm=i_mm, pT=pT, po=po, c=c, n_mm=n_mm: e.matmul(
                            po.t[0:64, (c % 2) * 130:(c % 2) * 130 + 130], pT.t[:, j, :], vbd[rr % NR].t[:, c, :],
                            start=(i_mm == 0), stop=(i_mm == n_mm - 1)), reads=[pT, vbd[rr % NR]], writes=[po])
                        i_mm += 1
            rc = rcp[r % 4]
            ob = obf[r % 2]
            for hf in range(2):
                pv = pot[hf].t[0:64, 0:260].rearrange("p (a x) -> p a x", x=65)
                kb.op("dve", lambda e: e.reciprocal(out=rc.t[:, :].unsqueeze(2), in_=pv[:, :, 64:65]), reads=[pot[hf]], writes=[rc])
                kb.op("dve", lambda e: e.tensor_tensor(out=ob.t[:, hf * 256:(hf + 1) * 256].rearrange("p (a x) -> p a x", x=64),
                                                      in0=pv[:, :, 0:64], in1=rc.t[:, :].unsqueeze(2).broadcast_to([64, 4, 64]), op=ALU.mult),
                      reads=[pot[hf], rc], writes=[ob])
            for fc in range(4):
                kb.op("pe", lambda e, fc=fc: e.transpose(out=ptt.t[:, fc, :], in_=ob.t[:, fc * 128:(fc + 1) * 128], identity=g.ident_bf.t[0:64, 0:64]),
                      reads=[ob, g.ident_bf], writes=[ptt])
            st = (r * 64) // ST
            o = outb[st % 2]
            off = (r * 64) % ST
            kb.op("act", lambda e: e.copy(out=o.t[:, :, off:off + 64], in_=ptt.t[:]), reads=[ptt], writes=[o])
            if off + 64 == ST:
                kb.dma("pool", S["naT"].rearrange("(c p) t -> p c t", p=128)[:, :, st * ST:(st + 1) * ST], o.t[:],
                       reads=[o], writes=[g.DB["naT"][st]])


def stage_C(g, l):
    nc, kb, I, S = g.nc, g.kb, g.I, g.S
    with ExitStack() as es:
        og = load_col(g, es, "C_og", I["hg_out_norm"][l, :], 1)
        NB_ = 4
        lf = [_alloc(es, nc, "C_lf%d" % i, [128, 4, 128], F32) for i in range(NB_)]
        kk = [_alloc(es, nc, "C_kk%d" % i, [128, 4, 128], BF16) for i in range(NB_)]
        qh = [_alloc(es, nc, "C_qh%d" % i, [128, 4, 128], BF16) for i in range(NB_)]
        vh = [_alloc(es, nc, "C_vh%d" % i, [128, 512], BF16) for i in range(NB_)]
        gsb = [_alloc(es, nc, "C_gs%d" % i, [128, 4, 128], BF16) for i in range(NB_)]
        ofb = [_alloc(es, nc, "C_of%d" % i, [128, 4, 128], F32) for i in range(NB_)]

        def two(name, shape, dt, psum=False):
            return [[_alloc(es, nc, "%s%d_%d" % (name, p_, h_), shape, dt, psum=psum) for h_ in range(4)] for p_ in range(2)]

        cb_ = two("C_c", [128, 132], F32)
        cm = two("C_cm", [128, 128], F32)
        e1 = two("C_e1", [128, 128], F32)
        e2 = two("C_e2", [128, 128], F32)
        qt = two("C_qt", [128, 128], BF16)
        kt = two("C_kt", [128, 128], BF16)
        ktok = two("C_ktok", [128, 128], BF16)
        attm = two("C_att", [128, 128], BF16)
        t6 = two("C_t6", [128, 2, 3], F32)
        x6 = two("C_x6", [128, 2, 3], F32)
        bs = two("C_bs", [128, 2], F32)
        Sst = [_alloc(es, nc, "C_S%d" % i, [128, 128], F32) for i in range(4)]
        Smid = [_alloc(es, nc, "C_Sm%d" % i, [128, 128], BF16) for i in range(8)]
        tmp = [_alloc(es, nc, "C_tmp%d" % i, [128, 128], F32) for i in range(4)]
        osb = [_alloc(es, nc, "C_o%d" % i, [128, 4, 128], F32) for i in range(2)]
        sqb = [_alloc(es, nc, "C_sq%d" % i, [128, 128], F32) for i in range(2)]
        rsb = [_alloc(es, nc, "C_rs%d" % i, [128, 128], F32) for i in range(2)]
        hgo = [_alloc(es, nc, "C_hg%d" % i, [128, 4, 128], BF16) for i in range(2)]
        zeros = _alloc(es, nc, "C_z", [128, 128], F32)
        kb.op("dve", lambda e: e.memset(zeros.t[:], 0.0), writes=[zeros])
        p_att = [_alloc(es, nc, "C_pa%d" % i, [128, 128], F32, psum=True) for i in range(2)]
        p_tr = [_alloc(es, nc, "C_pt%d" % i, [128, 128], BF16, psum=True) for i in range(1)]
        p_o = [_alloc(es, nc, "C_po%d" % i, [128, 128], F32, psum=True) for i in range(2)]
        p_u = [_alloc(es, nc, "C_pu%d" % i, [128, 128], F32, psum=True) for i in range(2)]
        p_n = [_alloc(es, nc, "C_pn%d" % i, [128, 128], F32, psum=True) for i in range(1)]
        fm = lambda nm: S[nm].rearrange("(h p) t -> p h t", p=128)

        for d_ in range(2):
            tri = g.trif if d_ == 0 else g.trib
            for h in range(4):
                kb.op("dve", lambda e, h=h: e.memset(Sst[h].t[:], 0.0), writes=[Sst[h]])
            smi = [0, 0, 0, 0]
            tiles = list(range(NT)) if d_ == 0 else list(range(NT - 1, -1, -1))
            if g.lim:
                tiles = tiles[:g.lim]
            i_d2 = 1 if d_ == 0 else 2
            i_m = 2 if d_ == 0 else 1
            sgn = 1.0 if d_ == 0 else -1.0
            chunks = (0, 1) if d_ == 0 else (1, 0)

            def prep(n, d_=d_, tri=tri, sgn=sgn):
                ti = tiles[n]
                i = n % NB_
                par = n % 2
                st = (ti * 128) // ST
                ts_ = slice(ti * 128, (ti + 1) * 128)
                with nc.allow_non_contiguous_dma(reason="head blocks"):
                    kb.dma("sp", lf[i].t[:], S["lf"][d_].rearrange("(h p) t -> p h t", p=128)[:, :, ts_], reads=[g.DB["lf"][st]], writes=[lf[i]])
                    kb.dma("sp", kk[i].t[:], S["kk"][d_].rearrange("(h p) t -> p h t", p=128)[:, :, ts_], reads=[g.DB["kk"][st]], writes=[kk[i]])
                    kb.dma("sp", qh[i].t[:], fm("qh")[:, :, ts_], reads=[g.DB["qh"][st]], writes=[qh[i]])
                    kb.dma("sp", vh[i].t[:], S["vh"][ts_, :], reads=[g.DB["vh"][st]], writes=[vh[i]])
                    if d_ == 1:
                        kb.dma("sp", gsb[i].t[:], fm("gs")[:, :, ts_], reads=[g.DB["gs"][st]], writes=[gsb[i]])
                        kb.dma("sp", ofb[i].t[:], fm("of")[:, :, ts_], reads=[g.DB["of"][st]], writes=[ofb[i]])
                for h in range(4):
                    C = cb_[par][h]
                    kb.op("dve", lambda e, C=C: e.memset(C.t[:, 0:1], 0.0), writes=[C])
                    kb.op("dve", lambda e, C=C, h=h: e.tensor_tensor_scan(out=C.t[:, 1:129], data0=lf[i].t[:, h, :], data1=zeros.t[:],
                                                                         initial=0.0, op0=ALU.add, op1=ALU.add),
                          reads=[lf[i], zeros], writes=[C])
                    Cv = C.t[:, 1:129].rearrange("p (a x) -> p a x", x=64)
                    Z = Cv if d_ == 0 else C.t[:, 0:128].rearrange("p (a x) -> p a x", x=64)
                    endv = Cv[:, :, 63:64]
                    midv = Cv[:, :, 31:32]
                    B_ = bs[par][h]
                    kb.op("dve", lambda e, B_=B_: e.memset(B_.t[:, 0:1], 0.0), writes=[B_])
                    kb.op("dve", lambda e, B_=B_, C=C: e.tensor_copy(out=B_.t[:, 1:2], in_=C.t[:, 64:65]), reads=[C], writes=[B_])
                    T6 = t6[par][h]
                    bv = B_.t[:, :].unsqueeze(2)
                    kb.op("dve", lambda e, T6=T6, endv=endv, bv=bv: e.tensor_tensor(out=T6.t[:, :, 0:1], in0=endv, in1=bv, op=ALU.subtract), reads=[C, B_], writes=[T6])
                    kb.op("dve", lambda e, T6=T6, endv=endv, midv=midv: e.tensor_tensor(out=T6.t[:, :, 1:2], in0=endv, in1=midv, op=ALU.subtract), reads=[C], writes=[T6])
                    kb.op("dve", lambda e, T6=T6, midv=midv, bv=bv: e.tensor_tensor(out=T6.t[:, :, 2:3], in0=midv, in1=bv, op=ALU.subtract), reads=[C, B_], writes=[T6])
                    X6 = x6[par][h]
                    kb.op("act", lambda e, X6=X6, T6=T6: e.activation(out=X6.t[:], in_=T6.t[:], func=AF.Exp), reads=[T6], writes=[X6])
                    CM = cm[par][h]
                    kb.op("dve", lambda e, CM=CM, Z=Z, midv=midv: e.tensor_tensor(out=CM.t[:].rearrange("p (a x) -> p a x", x=64), in0=Z,
                                                                               in1=midv.broadcast_to([128, 2, 64]), op=ALU.subtract), reads=[C], writes=[CM])
                    E1, E2, QT, KT = e1[par][h], e2[par][h], qt[par][h], kt[par][h]
                    kb.op("act", lambda e, E1=E1, CM=CM: e.activation(out=E1.t[:], in_=CM.t[:], func=AF.Exp, scale=sgn), reads=[CM], writes=[E1])
                    kb.op("act", lambda e, E2=E2, CM=CM: e.activation(out=E2.t[:], in_=CM.t[:], func=AF.Exp, scale=-sgn), reads=[CM], writes=[E2])
                    kb.op("pool", lambda e, QT=QT, E1=E1, h=h: e.tensor_tensor(out=QT.t[:], in0=qh[i].t[:, h, :], in1=E1.t[:], op=ALU.mult),
                          reads=[qh[i], E1], writes=[QT])
                    kb.op("dve", lambda e, KT=KT, E2=E2, h=h: e.tensor_tensor(out=KT.t[:], in0=kk[i].t[:, h, :], in1=E2.t[:], op=ALU.mult),
                          reads=[kk[i], E2], writes=[KT])
                    pa = p_att[h % 2]
                    kb.op("pe", lambda e, pa=pa, KT=KT, QT=QT: e.matmul(pa.t[:], KT.t[:], QT.t[:], start=True, stop=True), reads=[KT, QT], writes=[pa])
                    AM = attm[par][h]
                    kb.op("dve", lambda e, AM=AM, pa=pa: e.tensor_tensor(out=AM.t[:], in0=pa.t[:], in1=tri.t[:], op=ALU.mult), reads=[pa, tri], writes=[AM])
                    ptr_ = p_tr[0]
                    kb.op("pe", lambda e, ptr_=ptr_, KT=KT: e.transpose(out=ptr_.t[:], in_=KT.t[:], identity=g.ident_bf.t[:]), reads=[KT, g.ident_bf], writes=[ptr_])
                    KK = ktok[par][h]
                    kb.op("act", lambda e, KK=KK, ptr_=ptr_: e.copy(out=KK.t[:], in_=ptr_.t[:]), reads=[ptr_], writes=[KK])

            def recur(n, d_=d_):
                ti = tiles[n]
                i = n % NB_
                par = n % 2
                st = (ti * 128) // ST
                ts_ = slice(ti * 128, (ti + 1) * 128)
                ob = osb[n % 2]
                for h in range(4):
                    X6, QT, KK, po, AM = x6[par][h], qt[par][h], ktok[par][h], p_o[h % 2], attm[par][h]
                    kb.op("pe", lambda e, po=po, AM=AM, h=h: e.matmul(po.t[:], vh[i].t[:, h * 128:(h + 1) * 128], AM.t[:], start=True, stop=False),
                          reads=[vh[i], AM], writes=[po])
                    smi[h] ^= 1
                    sm0 = Smid[h + 4 * smi[h]]
                    kb.op("dve", lambda e, sm0=sm0, X6=X6, h=h: e.tensor_scalar(out=sm0.t[:], in0=Sst[h].t[:], scalar1=X6.t[:, chunks[0], i_m:i_m + 1],
                                                                             scalar2=None, op0=ALU.mult), reads=[Sst[h], X6], writes=[sm0])
                    for ci, a in enumerate(chunks):
                        sm = Smid[h + 4 * smi[h]]
                        kb.op("pe", lambda e, a=a, sm=sm, ci=ci, po=po, QT=QT: e.matmul(po.t[:, a * 64:(a + 1) * 64], sm.t[:], QT.t[:, a * 64:(a + 1) * 64],
                                                                                   start=False, stop=(ci == 1)),
                              reads=[sm, QT], writes=[po])
                        pu = p_u[(h + a) % 2]
                        kb.op("pe", lambda e, a=a, pu=pu, KK=KK, h=h: e.matmul(pu.t[:], KK.t[a * 64:(a + 1) * 64, :], vh[i].t[a * 64:(a + 1) * 64, h * 128:(h + 1) * 128],
                                                                          start=True, stop=True), reads=[KK, vh[i]], writes=[pu])
                        tm = tmp[h]
                        kb.op("dve", lambda e, a=a, tm=tm, pu=pu, X6=X6: e.tensor_scalar(out=tm.t[:], in0=pu.t[:], scalar1=X6.t[:, a, i_d2:i_d2 + 1], scalar2=None, op0=ALU.mult),
                              reads=[pu, X6], writes=[tm])
                        kb.op("dve", lambda e, a=a, tm=tm, X6=X6, h=h: e.scalar_tensor_tensor(out=Sst[h].t[:], in0=Sst[h].t[:], scalar=X6.t[:, a, 0:1], in1=tm.t[:],
                                                                                          op0=ALU.mult, op1=ALU.add), reads=[Sst[h], X6, tm], writes=[Sst[h]])
                        chunk_id = ti * 2 + a
                        if (d_ == 0 and chunk_id == 63) or (d_ == 1 and chunk_id == 64):
                            kb.op("dve", lambda e, h=h: e.tensor_scalar(out=Sst[h].t[:], in0=Sst[h].t[:], scalar1=g.cflag.t[:, 0:1], scalar2=None, op0=ALU.mult),
                                  reads=[Sst[h], g.cflag], writes=[Sst[h]])
                        if ci == 0:
                            na_ = chunks[1]
                            smi[h] ^= 1
                            sm2 = Smid[h + 4 * smi[h]]
                            kb.op("dve", lambda e, na_=na_, sm2=sm2, X6=X6, h=h: e.tensor_scalar(out=sm2.t[:], in0=Sst[h].t[:], scalar1=X6.t[:, na_, i_m:i_m + 1],
                                                                                              scalar2=None, op0=ALU.mult), reads=[Sst[h], X6], writes=[sm2])
                    if d_ == 0:
                        kb.op("act", lambda e, h=h, po=po: e.copy(out=ob.t[:, h, :], in_=po.t[:]), reads=[po], writes=[ob])
                    else:
                        kb.op("dve", lambda e, h=h, po=po: e.tensor_tensor(out=ob.t[:, h, :], in0=po.t[:], in1=ofb[i].t[:, h, :], op=ALU.add),
                              reads=[po, ofb[i]], writes=[ob])
                        sq = sqb[h % 2]
                        kb.op("act", lambda e, h=h, sq=sq: e.activation(out=sq.t[:], in_=ob.t[:, h, :], func=AF.Square), reads=[ob], writes=[sq])
                        pn = p_n[0]
                        kb.op("pe", lambda e, sq=sq, pn=pn: e.matmul(pn.t[:], g.ones_f.t[:], sq.t[:], start=True, stop=True), reads=[g.ones_f, sq], writes=[pn])
                        rs = rsb[h % 2]
                        kb.op("act", lambda e, rs=rs, pn=pn: e.activation(out=rs.t[:], in_=pn.t[:], func=AF.Ln, scale=1.0 / 128, bias=EPS), reads=[pn], writes=[rs])
                        kb.op("act", lambda e, rs=rs: e.activation(out=rs.t[:], in_=rs.t[:], func=AF.Exp, scale=-0.5), reads=[rs], writes=[rs])
                        kb.op("dve", lambda e, rs=rs, h=h: e.scalar_tensor_tensor(out=rs.t[:], in0=ob.t[:, h, :], scalar=og.t[:, 0:1], in1=rs.t[:],
                                                                                 op0=ALU.mult, op1=ALU.mult), reads=[ob, og, rs], writes=[rs])
                        kb.op("pool", lambda e, rs=rs, h=h: e.tensor_tensor(out=hgo[n % 2].t[:, h, :], in0=rs.t[:], in1=gsb[i].t[:, h, :], op=ALU.mult),
                              reads=[rs, gsb[i]], writes=[hgo[n % 2]])
                with nc.allow_non_contiguous_dma(reason="head blocks"):
                    if d_ == 0:
                        kb.dma("pool", fm("of")[:, :, ts_], ob.t[:], reads=[ob], writes=[g.DB["of"][st]])
                    else:
                        kb.dma("pool", fm("hgT")[:, :, ts_], hgo[n % 2].t[:], reads=[hgo[n % 2]], writes=[g.DB["hgT"][st]])

            prep(0)
            for n in range(len(tiles)):
                if n + 1 < len(tiles):
                    prep(n + 1)
                recur(n)


def stage_D(g, l):
    nc, kb, I, S = g.nc, g.kb, g.I, g.S
    TB = 1024
    with ExitStack() as es:
        cw = _alloc(es, nc, "D_cw", [128, 4, 31], F32)
        with nc.allow_non_contiguous_dma(reason="tiny conv weights"):
            for c in range(4):
                kb.dma("sp", cw.t[:, c, :], I["conv_w"][l, :, c * 128:(c + 1) * 128].rearrange("j p -> p j"), writes=[cw])
        cbias = load_col(g, es, "D_cb", I["conv_b"][l, :], 4)
        lng = load_col(g, es, "D_lg", I["conv_ln_g"][l, :], 4)
        lnb = load_col(g, es, "D_lb", I["conv_ln_b"][l, :], 4)
        ub = [_alloc(es, nc, "D_u%d" % i, [128, TB + 30], F32) for i in range(3)]
        yb = [_alloc(es, nc, "D_y%d" % i, [128, 4, TB], F32) for i in range(2)]
        sqt = [_alloc(es, nc, "D_sq%d" % i, [128, ST], F32) for i in range(2)]
        mean = [_alloc(es, nc, "D_mean%d" % i, [128, ST], F32) for i in range(2)]
        rstd = [_alloc(es, nc, "D_rstd%d" % i, [128, ST], F32) for i in range(2)]
        tt = [_alloc(es, nc, "D_t%d" % i, [128, ST], F32) for i in range(3)]
        ob = [_alloc(es, nc, "D_o%d" % i, [128, 4, ST], BF16) for i in range(2)]
        ps1 = [_alloc(es, nc, "D_p1%d" % i, [128, ST], F32, psum=True) for i in range(2)]
        ps2 = [_alloc(es, nc, "D_p2%d" % i, [128, ST], F32, psum=True) for i in range(2)]
        k = 0
        nsb = 0
        for blk in range(T // TB):
            t0 = blk * TB
            yv = yb[blk % 2]
            for c in range(4):
                u = ub[k % 3]
                k += 1
                lo, hi = t0 - 15, t0 + TB + 15
                a, b = max(lo, 0), min(hi, T)
                if lo < 0:
                    kb.op("pool", lambda e: e.memset(u.t[:, 0:15], 0.0), writes=[u])
                if hi > T:
                    kb.op("pool", lambda e: e.memset(u.t[:, TB + 15:TB + 30], 0.0), writes=[u])
                rd = [g.DB["u"][s_] for s_ in range(a // ST, (b - 1) // ST + 1)]
                kb.dma("sp", u.t[:, a - lo:b - lo], S["u"][c * 128:(c + 1) * 128, a:b], reads=rd, writes=[u])
                if t0 == 4096:
                    kb.op("pool", lambda e: e.tensor_scalar(out=u.t[:, 0:15], in0=u.t[:, 0:15], scalar1=g.cflag.t[:, 0:1], scalar2=None, op0=ALU.mult),
                          reads=[u, g.cflag], writes=[u])
                if t0 + TB == 4096:
                    kb.op("pool", lambda e: e.tensor_scalar(out=u.t[:, TB + 15:TB + 30], in0=u.t[:, TB + 15:TB + 30], scalar1=g.cflag.t[:, 0:1],
                                                           scalar2=None, op0=ALU.mult), reads=[u, g.cflag], writes=[u])
                kb.op("dve", lambda e: e.tensor_scalar(out=yv.t[:, c, :], in0=u.t[:, 0:TB], scalar1=cw.t[:, c, 0:1], scalar2=cbias.t[:, c:c + 1],
                                                      op0=ALU.mult, op1=ALU.add), reads=[u, cw, cbias], writes=[yv])
                for j in range(1, 31):
                    kb.op("dve", lambda e, j=j: e.scalar_tensor_tensor(out=yv.t[:, c, :], in0=u.t[:, j:j + TB], scalar=cw.t[:, c, j:j + 1], in1=yv.t[:, c, :],
                                                                      op0=ALU.mult, op1=ALU.add), reads=[u, cw, yv], writes=[yv])
            for sb_ in range(TB // ST):
                cs = slice(sb_ * ST, (sb_ + 1) * ST)
                p1 = ps1[nsb % 2]
                p2 = ps2[nsb % 2]
                for c in range(4):
                    sq = sqt[c % 2]
                    kb.op("act", lambda e, c=c: e.activation(out=sq.t[:], in_=yv.t[:, c, cs], func=AF.Square), reads=[yv], writes=[sq])
                    kb.op("pe", lambda e, c=c: e.matmul(p1.t[:], g.ones_f.t[:], yv.t[:, c, cs], start=(c == 0), stop=(c == 3)),
                          reads=[g.ones_f, yv], writes=[p1])
                    kb.op("pe", lambda e, c=c: e.matmul(p2.t[:], g.ones_f.t[:], sq.t[:], start=(c == 0), stop=(c == 3)),
                          reads=[g.ones_f, sq], writes=[p2])
                mn = mean[nsb % 2]
                rs = rstd[nsb % 2]
                kb.op("act", lambda e: e.activation(out=mn.t[:], in_=p1.t[:], func=AF.Copy, scale=1.0 / 512), reads=[p1], writes=[mn])
                msq = tt[2]
                kb.op("dve", lambda e: e.tensor_tensor(out=msq.t[:], in0=mn.t[:], in1=mn.t[:], op=ALU.mult), reads=[mn], writes=[msq])
                kb.op("dve", lambda e: e.scalar_tensor_tensor(out=rs.t[:], in0=p2.t[:], scalar=1.0 / 512, in1=msq.t[:], op0=ALU.mult, op1=ALU.subtract),
                      reads=[p2, msq], writes=[rs])
                kb.op("act", lambda e: e.activation(out=rs.t[:], in_=rs.t[:], func=AF.Ln, bias=EPS), reads=[rs], writes=[rs])
                kb.op("act", lambda e: e.activation(out=rs.t[:], in_=rs.t[:], func=AF.Exp, scale=-0.5), reads=[rs], writes=[rs])
                o = ob[nsb % 2]
                for c in range(4):
                    t_ = tt[c % 2]
                    kb.op("dve", lambda e, c=c: e.tensor_tensor(out=t_.t[:], in0=yv.t[:, c, cs], in1=mn.t[:], op=ALU.subtract), reads=[yv, mn], writes=[t_])
                    kb.op("pool", lambda e: e.tensor_tensor(out=t_.t[:], in0=t_.t[:], in1=rs.t[:], op=ALU.mult), reads=[t_, rs], writes=[t_])
                    kb.op("act", lambda e, c=c: e.activation(out=o.t[:, c, :], in_=t_.t[:], func=AF.Silu, scale=lng.t[:, c:c + 1], bias=lnb.t[:, c:c + 1]),
                          reads=[t_, lng, lnb], writes=[o])
                st = (t0 + sb_ * ST) // ST
                kb.dma("pool", S["cvT"].rearrange("(c p) t -> p c t", p=128)[:, :, st * ST:(st + 1) * ST], o.t[:], reads=[o], writes=[g.DB["cvT"][st]])
                nsb += 1


def stage_E(g, l, xsrc):
    nc, kb, I, S = g.nc, g.kb, g.I, g.S
    with ExitStack() as es:
        wbr = _alloc(es, nc, "E_wbr", [128, 12, D], BF16)
        for bi, nm in enumerate(("w_na_br", "w_hg_br", "w_cv_br")):
            kb.dma("pool", wbr.t[:, bi * 4:(bi + 1) * 4, :], I[nm][l].rearrange("(c p) n -> p c n", p=128), writes=[wbr])
        wout = _alloc(es, nc, "E_wout", [128, 8, D], BF16)
        kb.dma("pool", wout.t[:], I["w_out"][l].rearrange("(c p) n -> p c n", p=128), writes=[wout])
        brT = [[_alloc(es, nc, "E_br%d_%d" % (b, i), [128, 4, ST], BF16) for b in range(3)] for i in range(2)]
        sgt = [_alloc(es, nc, "E_sg%d" % i, [128, 24, ST], BF16) for i in range(2)]
        mT = _alloc(es, nc, "E_mT", [128, 8, ST], BF16)
        tmp = [_alloc(es, nc, "E_t%d" % i, [128, ST], F32) for i in range(6)]
        xa = [_alloc(es, nc, "E_x%d" % i, [128, D], F32) for i in range(3)]
        pb = [_alloc(es, nc, "E_pb%d" % i, [128, ST], F32, psum=True) for i in range(6)]
        pq = [_alloc(es, nc, "E_pq%d" % i, [128, ST], F32, psum=True) for i in range(2)]
        fmv = lambda nm: S[nm].rearrange("(c p) t -> p c t", p=128)
        n_pb = 0
        n_x = 0
        n_pq = 0
        for st in range(NST):
            t0 = st * ST
            br = brT[st % 2]
            sg = sgt[st % 2]
            for b, nm in enumerate(("naT", "hgT", "cvT")):
                kb.dma("sp", br[b].t[:], fmv(nm)[:, :, t0:t0 + ST], reads=[g.DB[nm][st]], writes=[br[b]])
            kb.dma("sp", sg.t[:], fmv("sg")[:, :, t0:t0 + ST], reads=[g.DB["sg"][st]], writes=[sg])
            for m in range(8):
                ps = []
                for b in range(3):
                    p = pb[n_pb % 6]
                    n_pb += 1
                    for kc in range(4):
                        kb.op("pe", lambda e, b=b, kc=kc, p=p: e.matmul(p.t[:], wbr.t[:, b * 4 + kc, m * 128:(m + 1) * 128], br[b].t[:, kc, :],
                                                                       start=(kc == 0), stop=(kc == 3)), reads=[wbr, br[b]], writes=[p])
                    ps.append(p)
                t1, t2, t3 = tmp[(m % 2) * 3], tmp[(m % 2) * 3 + 1], tmp[(m % 2) * 3 + 2]
                kb.op("dve", lambda e: e.tensor_tensor(out=t1.t[:], in0=ps[0].t[:], in1=sg.t[:, m, :], op=ALU.mult), reads=[ps[0], sg], writes=[t1])
                kb.op("dve", lambda e: e.tensor_tensor(out=t2.t[:], in0=ps[1].t[:], in1=sg.t[:, 8 + m, :], op=ALU.mult), reads=[ps[1], sg], writes=[t2])
                kb.op("dve", lambda e: e.tensor_tensor(out=t3.t[:], in0=ps[2].t[:], in1=sg.t[:, 16 + m, :], op=ALU.mult), reads=[ps[2], sg], writes=[t3])
                kb.op("pool", lambda e: e.tensor_tensor(out=t1.t[:], in0=t1.t[:], in1=t2.t[:], op=ALU.add), reads=[t1, t2], writes=[t1])
                kb.op("pool", lambda e: e.tensor_tensor(out=mT.t[:, m, :], in0=t1.t[:], in1=t3.t[:], op=ALU.add), reads=[t1, t3], writes=[mT])
            for tb in range(4):
                x = xa[n_x % 3]
                n_x += 1
                rows = slice(t0 + tb * 128, t0 + (tb + 1) * 128)
                kb.dma("sp", x.t[:], xsrc[rows, :], reads=[g.DB["y"][st]], writes=[x])
                for hf in range(2):
                    p = pq[n_pq % 2]
                    n_pq += 1
                    for m in range(8):
                        kb.op("pe", lambda e, m=m, p=p: e.matmul(p.t[:], mT.t[:, m, tb * 128:(tb + 1) * 128], wout.t[:, m, hf * 512:(hf + 1) * 512],
                                                                start=(m == 0), stop=(m == 7)), reads=[mT, wout], writes=[p])
                    kb.op("dve", lambda e, p=p: e.tensor_tensor(out=x.t[:, hf * 512:(hf + 1) * 512], in0=p.t[:], in1=x.t[:, hf * 512:(hf + 1) * 512], op=ALU.add),
                          reads=[p, x], writes=[x])
                kb.dma("pool", g.yc[rows, :], x.t[:], reads=[x], writes=[g.DB["y"][st]])


def stage_F(g, l):
    nc, kb, I, S = g.nc, g.kb, g.I, g.S
    y = g.yc
    with ExitStack() as es:
        W = NormWork(g, es, "F")
        gmem = load_bcast_row(g, es, "F_gmem", I["norm_mem"][l, :], D)
        gkv = load_bcast_row(g, es, "F_gkv", I["mem_kv_norm"][l, :], D)
        gffn = load_bcast_row(g, es, "F_gffn", I["norm_ffn"][l, :], D)
        qn = load_col(g, es, "F_qn", I["mem_q_norm"][l, :], 1)
        kn = load_col(g, es, "F_kn", I["mem_k_norm"][l, :], 1)
        kb.op("dve", lambda e: e.tensor_scalar(out=qn.t[:], in0=qn.t[:], scalar1=128.0 ** -0.5, scalar2=None, op0=ALU.mult), reads=[qn], writes=[qn])
        wq = _alloc(es, nc, "F_wq", [128, 8, 512], BF16)
        kb.dma("pool", wq.t[:], I["wq_mem"][l].rearrange("(c p) n -> p c n", p=128), writes=[wq])
        wkv = _alloc(es, nc, "F_wkv", [128, 8, D], BF16)
        kb.dma("pool", wkv.t[:], I["wkv_mem"][l].rearrange("(c p) n -> p c n", p=128), writes=[wkv])
        wo = _alloc(es, nc, "F_wo", [128, 4, D], BF16)
        kb.dma("pool", wo.t[:], I["wo_mem"][l].rearrange("(c p) n -> p c n", p=128), writes=[wo])
        wr = _alloc(es, nc, "F_wr", [128, 8, NE], BF16)
        with nc.allow_non_contiguous_dma(reason="router weights"):
            kb.dma("pool", wr.t[:], I["w_router"][l].rearrange("(c p) n -> p c n", p=128), writes=[wr])
        hT = _alloc(es, nc, "F_hT", [128, 8, ST], BF16)
        h2T = [_alloc(es, nc, "F_h2T%d" % i, [128, 8, ST], BF16) for i in range(2)]
        KT = [_alloc(es, nc, "F_KT%d" % i, [128, 4, 256], BF16) for i in range(2)]
        V = [_alloc(es, nc, "F_V%d" % i, [128, 2, 512], BF16) for i in range(2)]
        f32t = [_alloc(es, nc, "F_f%d" % i, [128, ST], F32) for i in range(4)]
        qT = [_alloc(es, nc, "F_qT%d" % i, [128, ST], BF16) for i in range(4)]
        rcb = [_alloc(es, nc, "F_rc%d" % i, [128, ST], F32) for i in range(2)]
        PT = [_alloc(es, nc, "F_PT%d" % i, [128, ST], BF16) for i in range(4)]
        oT = _alloc(es, nc, "F_oT", [128, 4, ST], BF16)
        sm = [_alloc(es, nc, "F_sm%d" % i, [128, 4], F32) for i in range(2)]
        ex = [_alloc(es, nc, "F_ex%d" % i, [128, NE], F32) for i in range(2)]
        af = [_alloc(es, nc, "F_af%d" % i, [128, NE], F32) for i in range(2)]
        p_qs = [_alloc(es, nc, "F_pq%d" % i, [128, ST], F32, psum=True) for i in range(2)]
        p_q = p_qs[0]
        p_n = _alloc(es, nc, "F_pn", [128, ST], F32, psum=True)
        p_s = [_alloc(es, nc, "F_ps%d" % i, [128, ST], F32, psum=True) for i in range(2)]
        p_o = _alloc(es, nc, "F_po", [128, ST], F32, psum=True)
        p_d = _alloc(es, nc, "F_pd", [128, ST], F32, psum=True)
        p_x = p_d

        def chan_norm(p, n, gain_col, out_bf, out_tt, par=0):
            sq = f32t[par]
            kb.op("act", lambda e: e.activation(out=sq.t[:, 0:n], in_=p.t[:, 0:n], func=AF.Square), reads=[p], writes=[sq])
            kb.op("pe", lambda e: e.matmul(p_n.t[:, 0:n], g.ones_f.t[:], sq.t[:, 0:n], start=True, stop=True), reads=[g.ones_f, sq], writes=[p_n])
            rs = f32t[2 + par]
            kb.op("act", lambda e: e.activation(out=rs.t[:, 0:n], in_=p_n.t[:, 0:n], func=AF.Ln, scale=1.0 / 128, bias=EPS), reads=[p_n], writes=[rs])
            kb.op("act", lambda e: e.activation(out=rs.t[:, 0:n], in_=rs.t[:, 0:n], func=AF.Exp, scale=-0.5), reads=[rs], writes=[rs])
            kb.op("dve", lambda e: e.scalar_tensor_tensor(out=out_bf, in0=p.t[:, 0:n], scalar=gain_col, in1=rs.t[:, 0:n], op0=ALU.mult, op1=ALU.mult),
                  reads=[p, rs, qn, kn], writes=[out_tt])

        for s in range(2):
            for tb in range(2):
                kb.dma("sp", W.xa[tb].t[:], g.memc[s, tb * 128:(tb + 1) * 128, :], writes=[W.xa[tb]])
            rms_rows(g, W.xa, W.ss, W.rstd, W.junk, 2)
            for tb in range(2):
                kb.op("dve", lambda e, tb=tb: e.scalar_tensor_tensor(out=W.hb[tb].t[:], in0=W.xa[tb].t[:], scalar=W.rstd.t[:, tb:tb + 1], in1=gkv.t[:],
                                                                    op0=ALU.mult, op1=ALU.mult), reads=[W.xa[tb], W.rstd, gkv], writes=[W.hb[tb]])
                for kc in range(8):
                    kb.op("pe", lambda e, tb=tb, kc=kc: e.transpose(out=W.ptr.t[:, kc, :], in_=W.hb[tb].t[:, kc * 128:(kc + 1) * 128], identity=g.ident_bf.t[:]),
                          reads=[W.hb[tb], g.ident_bf], writes=[W.ptr])
                kb.op("act", lambda e, tb=tb: e.copy(out=hT.t[:, :, tb * 128:(tb + 1) * 128], in_=W.ptr.t[:]), reads=[W.ptr], writes=[hT])
            for h in range(4):
                for kc in range(8):
                    kb.op("pe", lambda e, kc=kc: e.matmul(p_q.t[:, 0:256], wkv.t[:, kc, h * 128:(h + 1) * 128], hT.t[:, kc, 0:256], start=(kc == 0), stop=(kc == 7)),
                          reads=[wkv, hT], writes=[p_q])
                chan_norm(p_q, 256, kn.t[:, 0:1], KT[s].t[:, h, :], KT[s])
            for mc in range(2):
                for kc in range(8):
                    kb.op("pe", lambda e, kc=kc: e.matmul(p_o.t[:], hT.t[:, kc, mc * 128:(mc + 1) * 128], wkv.t[:, kc, 512:1024], start=(kc == 0), stop=(kc == 7)),
                          reads=[wkv, hT], writes=[p_o])
                kb.op("act", lambda e: e.copy(out=V[s].t[:, mc, :], in_=p_o.t[:]), reads=[p_o], writes=[V[s]])

        for st in range(NST):
            t0 = st * ST
            s = st // (NST // 2)
            norm_transpose(g, y, st, gmem, W, hT)
            for h in range(4):
                pq_ = p_qs[h % 2]
                for kc in range(8):
                    kb.op("pe", lambda e, kc=kc, pq_=pq_, h=h: e.matmul(pq_.t[:], wq.t[:, kc, h * 128:(h + 1) * 128], hT.t[:, kc, :], start=(kc == 0), stop=(kc == 7)),
                          reads=[wq, hT], writes=[pq_])
                chan_norm(pq_, ST, qn.t[:, 0:1], qT[h].t[:], qT[h], par=h % 2)
            for h in range(4):
                q_ = qT[h]
                pts = []
                for mc in range(2):
                    ps_ = p_s[mc]
                    kb.op("pe", lambda e, ps_=ps_, mc=mc, h=h, q_=q_: e.matmul(ps_.t[:], KT[s].t[:, h, mc * 128:(mc + 1) * 128], q_.t[:], start=True, stop=True),
                          reads=[KT[s], q_], writes=[ps_])
                    pt = PT[(h % 2) * 2 + mc]
                    kb.op("act", lambda e, pt=pt, ps_=ps_: e.activation(out=pt.t[:], in_=ps_.t[:], func=AF.Exp), reads=[ps_], writes=[pt])
                    pts.append(pt)
                for mc in range(2):
                    kb.op("pe", lambda e, mc=mc, h=h, pts=pts: e.matmul(p_o.t[:], V[s].t[:, mc, h * 128:(h + 1) * 128], pts[mc].t[:], start=(mc == 0), stop=(mc == 1)),
                          reads=[V[s], pts[mc]], writes=[p_o])
                for mc in range(2):
                    kb.op("pe", lambda e, mc=mc, pts=pts: e.matmul(p_d.t[:], g.ones_bf.t[:], pts[mc].t[:], start=(mc == 0), stop=(mc == 1)), reads=[g.ones_bf, pts[mc]], writes=[p_d])
                rc = rcb[h % 2]
                kb.op("dve", lambda e, rc=rc: e.reciprocal(out=rc.t[:], in_=p_d.t[:]), reads=[p_d], writes=[rc])
                kb.op("dve", lambda e, rc=rc, h=h: e.tensor_tensor(out=oT.t[:, h, :], in0=p_o.t[:], in1=rc.t[:], op=ALU.mult), reads=[p_o, rc], writes=[oT])
            for tb in range(4):
                x = W.xa[tb]
                rows = slice(t0 + tb * 128, t0 + (tb + 1) * 128)
                for hf in range(2):
                    for h in range(4):
                        kb.op("pe", lambda e, h=h: e.matmul(p_x.t[:], oT.t[:, h, tb * 128:(tb + 1) * 128], wo.t[:, h, hf * 512:(hf + 1) * 512], start=(h == 0), stop=(h == 3)),
                              reads=[oT, wo], writes=[p_x])
                    kb.op("dve", lambda e: e.tensor_tensor(out=x.t[:, hf * 512:(hf + 1) * 512], in0=p_x.t[:], in1=x.t[:, hf * 512:(hf + 1) * 512], op=ALU.add),
                          reads=[p_x, x], writes=[x])
                kb.dma("pool", y[rows, :], x.t[:], reads=[x], writes=[g.DB["y"][st]])
            rms_rows(g, W.xa, W.ss, W.rstd, W.junk, 4)
            h2 = h2T[st % 2]
            for tb in range(4):
                kb.op("dve", lambda e, tb=tb: e.scalar_tensor_tensor(out=W.hb[tb].t[:], in0=W.xa[tb].t[:], scalar=W.rstd.t[:, tb:tb + 1], in1=gffn.t[:],
                                                                    op0=ALU.mult, op1=ALU.mult), reads=[W.xa[tb], W.rstd, gffn], writes=[W.hb[tb]])
                for kc in range(8):
                    kb.op("pe", lambda e, tb=tb, kc=kc: e.transpose(out=W.ptr.t[:, kc, :], in_=W.hb[tb].t[:, kc * 128:(kc + 1) * 128], identity=g.ident_bf.t[:]),
                          reads=[W.hb[tb], g.ident_bf], writes=[W.ptr])
                kb.op("act", lambda e, tb=tb: e.copy(out=h2.t[:, :, tb * 128:(tb + 1) * 128], in_=W.ptr.t[:]), reads=[W.ptr], writes=[h2])
                kb.dma("sp", g.h2rc[t0 + tb * 128:t0 + (tb + 1) * 128, :], W.hb[tb].t[:], reads=[W.hb[tb]], writes=[g.DBall["h2r"]])
            for tb in range(4):
                for kc in range(8):
                    kb.op("pe", lambda e, kc=kc: e.matmul(p_x.t[:, 0:NE], h2.t[:, kc, tb * 128:(tb + 1) * 128], wr.t[:, kc, :], start=(kc == 0), stop=(kc == 7)),
                          reads=[h2, wr], writes=[p_x])
                s_ = sm[tb % 2]
                kb.op("dve", lambda e: e.tensor_reduce(out=s_.t[:, 0:1], in_=p_x.t[:, 0:NE], axis=AX.X, op=ALU.max), reads=[p_x], writes=[s_])
                kb.op("dve", lambda e: e.tensor_scalar(out=s_.t[:, 1:2], in0=s_.t[:, 0:1], scalar1=-1.0, scalar2=None, op0=ALU.mult), reads=[s_], writes=[s_])
                e_ = ex[tb % 2]
                kb.op("act", lambda e: e.activation(out=e_.t[:], in_=p_x.t[:, 0:NE], func=AF.Exp, bias=s_.t[:, 1:2], accum_out=s_.t[:, 2:3]),
                      reads=[p_x, s_], writes=[e_, s_])
                kb.op("dve", lambda e: e.reciprocal(out=s_.t[:, 3:4], in_=s_.t[:, 2:3]), reads=[s_], writes=[s_])
                a_ = af[tb % 2]
                kb.op("dve", lambda e: e.tensor_scalar(out=a_.t[:], in0=e_.t[:], scalar1=s_.t[:, 3:4], scalar2=None, op0=ALU.mult), reads=[e_, s_], writes=[a_])
                kb.dma("sp", g.affc[t0 + tb * 128:t0 + (tb + 1) * 128, :], a_.t[:], reads=[a_], writes=[g.DBall["affall"]])


def stage_H(g, l):
    nc, kb, I, S = g.nc, g.kb, g.I, g.S
    rd = [b for b in g.DB["aff"]]
    coll(g, "AllGather", ALU.bypass, [[0, 1, 2, 3], [4, 5, 6, 7]], S["aff"][:, :], S["affall"][:, :], rd, g.DBall["affall"])
    with ExitStack() as es:
        A = _alloc(es, nc, "H_A", [128, 256, NE], F32)
        kb.dma("sp", A.t[:], S["affall"].rearrange("(p j) e -> p j e", p=128), reads=[g.DBall["affall"]], writes=[A])
        cmp_ = _alloc(es, nc, "H_cmp", [128, 256, NE], BF16)
        lo = _alloc(es, nc, "H_lo", [128, NE], F32)
        hi = _alloc(es, nc, "H_hi", [128, NE], F32)
        mid = _alloc(es, nc, "H_mid", [128, NE], F32)
        cp = _alloc(es, nc, "H_cp", [128, NE], F32)
        ge = _alloc(es, nc, "H_ge", [128, NE], F32)
        d1 = _alloc(es, nc, "H_d1", [128, NE], F32)
        d2 = _alloc(es, nc, "H_d2", [128, NE], F32)
        pt = _alloc(es, nc, "H_pt", [128, NE], F32, psum=True)
        kb.op("dve", lambda e: e.memset(lo.t[:], 0.0), writes=[lo])
        kb.op("dve", lambda e: e.memset(hi.t[:], 1.0), writes=[hi])
        for it in range(NBIS):
            kb.op("dve", lambda e: e.tensor_tensor(out=mid.t[:], in0=lo.t[:], in1=hi.t[:], op=ALU.add), reads=[lo, hi], writes=[mid])
            kb.op("dve", lambda e: e.tensor_scalar(out=mid.t[:], in0=mid.t[:], scalar1=0.5, scalar2=None, op0=ALU.mult), reads=[mid], writes=[mid])
            kb.op("dve", lambda e: e.tensor_tensor(out=cmp_.t[:], in0=A.t[:], in1=mid.t[:, :].unsqueeze(1).broadcast_to([128, 256, NE]), op=ALU.is_gt),
                  reads=[A, mid], writes=[cmp_])
            kb.op("dve", lambda e: e.tensor_reduce(out=cp.t[:], in_=cmp_.t[:].rearrange("p j e -> p e j"), axis=AX.X, op=ALU.add), reads=[cmp_], writes=[cp])
            kb.op("pe", lambda e: e.matmul(pt.t[:], g.ones_f.t[:], cp.t[:], start=True, stop=True), reads=[g.ones_f, cp], writes=[pt])
            kb.op("dve", lambda e: e.tensor_scalar(out=ge.t[:], in0=pt.t[:], scalar1=float(CAPK) - 0.5, scalar2=None, op0=ALU.is_gt), reads=[pt], writes=[ge])
            kb.op("dve", lambda e: e.tensor_tensor(out=d1.t[:], in0=mid.t[:], in1=lo.t[:], op=ALU.subtract), reads=[mid, lo], writes=[d1])
            kb.op("dve", lambda e: e.tensor_tensor(out=d1.t[:], in0=d1.t[:], in1=ge.t[:], op=ALU.mult), reads=[d1, ge], writes=[d1])
            kb.op("dve", lambda e: e.tensor_tensor(out=d2.t[:], in0=hi.t[:], in1=mid.t[:], op=ALU.subtract), reads=[hi, mid], writes=[d2])
            kb.op("dve", lambda e: e.tensor_tensor(out=d2.t[:], in0=d2.t[:], in1=ge.t[:], op=ALU.mult), reads=[d2, ge], writes=[d2])
            kb.op("dve", lambda e: e.tensor_tensor(out=lo.t[:], in0=lo.t[:], in1=d1.t[:], op=ALU.add), reads=[lo, d1], writes=[lo])
            kb.op("dve", lambda e: e.tensor_tensor(out=hi.t[:], in0=mid.t[:], in1=d2.t[:], op=ALU.add), reads=[mid, d2], writes=[hi])
        own = _alloc(es, nc, "H_own", [128, NT, NE], F32)
        with nc.allow_non_contiguous_dma(reason="aff rows"):
            kb.dma("sp", own.t[:], S["aff"].rearrange("(j p) e -> p j e", p=128), reads=rd, writes=[own])
        msk = _alloc(es, nc, "H_msk", [128, NT, NE], F32)
        kb.op("dve", lambda e: e.tensor_tensor(out=msk.t[:], in0=own.t[:], in1=lo.t[:, :].unsqueeze(1).broadcast_to([128, NT, NE]), op=ALU.is_gt),
              reads=[own, lo], writes=[msk])
        kb.op("dve", lambda e: e.tensor_tensor(out=g.wg.t[:], in0=own.t[:], in1=msk.t[:], op=ALU.mult), reads=[own, msk], writes=[g.wg])


def coll(g, kind, op, groups, in_ap, out_ap, reads, out_buf):
    kb = g.kb
    i = kb.next_dsem("pool")
    deps_ = kb._deps(reads, [out_buf])
    if kb.dcnt[i] > 0:
        deps_[("d", i)] = max(deps_.get(("d", i), 0), kb.dcnt[i])
    kb._wait("pool", deps_)
    inst = g.nc.gpsimd.collective_compute(kind, op, replica_groups=groups, ins=[in_ap], outs=[out_ap])
    kb.dcnt[i] += 16
    inst.then_inc(kb.semh[("d", i)], 16)
    for b in reads:
        b.r[("d", i)] = kb.dcnt[i]
    out_buf.w = (("d", i), kb.dcnt[i])
    out_buf.r = {}


def stage_W(g):
    nc, kb, I = g.nc, g.kb, g.I
    g.wall = {}
    allc = [list(range(8))]
    for l in range(DEPTH):
        for nm, rows, cols in (("w_gate", D, DFF), ("w_up", D, DFF), ("w_down", DFF, D)):
            loc = nc.dram_tensor("%s_loc%d" % (nm, l), [2 * rows, cols], F32, kind="Internal").ap()
            al = nc.dram_tensor("%s_all%d" % (nm, l), [NE * rows, cols], F32, kind="Internal").ap()
            bl, ba = Buf(), Buf()
            kb.dma("sp", loc[:, :], I[nm][l], writes=[bl])
            coll(g, "AllGather", ALU.bypass, allc, loc[:, :], al[:, :], [bl], ba)
            g.wall[(nm, l)] = (al, ba)


def stage_I(g, l):
    nc, kb, I, S = g.nc, g.kb, g.I, g.S
    y = g.y
    NB = 256
    NFC = (DFF + 127) // 128
    with ExitStack() as es:
        wgu = _alloc(es, nc, "I_wgu", [128, 8, 2 * DFF], BF16)
        wd = _alloc(es, nc, "I_wd", [128, NFC, D], BF16)
        h2 = [_alloc(es, nc, "I_h2%d" % i, [128, 8, NB], BF16) for i in range(2)]
        aT = _alloc(es, nc, "I_aT", [128, NFC, NB], BF16)
        sgt = [_alloc(es, nc, "I_sg%d" % i, [128, NB], F32) for i in range(3)]
        xa = [_alloc(es, nc, "I_x%d" % i, [128, D], F32) for i in range(2)]
        pg = [_alloc(es, nc, "I_pg%d" % i, [128, NB], F32, psum=True) for i in range(2)]
        pu = [_alloc(es, nc, "I_pu%d" % i, [128, NB], F32, psum=True) for i in range(2)]
        pq = [_alloc(es, nc, "I_pq%d" % i, [128, 512], F32, psum=True) for i in range(2)]
        n = {"h": 0, "f": 0, "x": 0, "q": 0}
        for ex in range(NE):
            for kc in range(8):
                for hi_, nm_ in enumerate(("w_gate", "w_up")):
                    al_, ba_ = g.wall[(nm_, l)]
                    kb.dma("pool", wgu.t[:, kc, hi_ * DFF:(hi_ + 1) * DFF], al_[ex * D + kc * 128:ex * D + (kc + 1) * 128, :], reads=[ba_], writes=[wgu])
            for fc in range(NFC):
                fw = min(128, DFF - fc * 128)
                al_, ba_ = g.wall[("w_down", l)]
                kb.dma("pool", wd.t[0:fw, fc, :], al_[ex * DFF + fc * 128:ex * DFF + fc * 128 + fw, :], reads=[ba_], writes=[wd])
            for bk in range(T // NB):
                t0 = bk * NB
                st = t0 // ST
                hh = h2[n["h"] % 2]
                n["h"] += 1
                kb.dma("sp", hh.t[:], S["h2T"].rearrange("(c p) t -> p c t", p=128)[:, :, t0:t0 + NB], reads=[g.DB["h2T"][st]], writes=[hh])
                for fc in range(NFC):
                    fw = min(128, DFF - fc * 128)
                    p_g = pg[n["f"] % 2]
                    p_u = pu[n["f"] % 2]
                    s_ = sgt[n["f"] % 3]
                    n["f"] += 1
                    for kc in range(8):
                        kb.op("pe", lambda e, kc=kc: e.matmul(p_g.t[0:fw, :], wgu.t[:, kc, fc * 128:fc * 128 + fw], hh.t[:, kc, :], start=(kc == 0), stop=(kc == 7)),
                              reads=[wgu, hh], writes=[p_g])
                    for kc in range(8):
                        kb.op("pe", lambda e, kc=kc: e.matmul(p_u.t[0:fw, :], wgu.t[:, kc, DFF + fc * 128:DFF + fc * 128 + fw], hh.t[:, kc, :], start=(kc == 0), stop=(kc == 7)),
                              reads=[wgu, hh], writes=[p_u])
                    kb.op("act", lambda e: e.activation(out=s_.t[0:fw, :], in_=p_g.t[0:fw, :], func=AF.Sigmoid), reads=[p_g], writes=[s_])
                    kb.op("dve", lambda e: e.tensor_tensor(out=s_.t[0:fw, :], in0=p_g.t[0:fw, :], in1=s_.t[0:fw, :], op=ALU.mult), reads=[p_g, s_], writes=[s_])
                    kb.op("dve", lambda e: e.tensor_tensor(out=aT.t[0:fw, fc, :], in0=p_u.t[0:fw, :], in1=s_.t[0:fw, :], op=ALU.mult), reads=[p_u, s_], writes=[aT])
                for tb in range(NB // 128):
                    tile_i = (t0 + tb * 128) // 128
                    rows = slice(t0 + tb * 128, t0 + (tb + 1) * 128)
                    x = xa[n["x"] % 2]
                    n["x"] += 1
                    kb.dma("sp", x.t[:], y[rows, :], reads=[g.DB["y"][st]], writes=[x])
                    for hf in range(2):
                        p = pq[n["q"] % 2]
                        n["q"] += 1
                        for fc in range(NFC):
                            fw = min(128, DFF - fc * 128)
                            kb.op("pe", lambda e, fc=fc, fw=fw: e.matmul(p.t[:], aT.t[0:fw, fc, tb * 128:(tb + 1) * 128], wd.t[0:fw, fc, hf * 512:(hf + 1) * 512],
                                                                        start=(fc == 0), stop=(fc == NFC - 1)), reads=[aT, wd], writes=[p])
                        kb.op("dve", lambda e: e.scalar_tensor_tensor(out=x.t[:, hf * 512:(hf + 1) * 512], in0=p.t[:], scalar=g.wg.t[:, tile_i, ex:ex + 1],
                                                                     in1=x.t[:, hf * 512:(hf + 1) * 512], op0=ALU.mult, op1=ALU.add), reads=[p, g.wg, x], writes=[x])
                    kb.dma("pool", y[rows, :], x.t[:], reads=[x], writes=[g.DB["y"][st]])


def stage_H2(g, l):
    nc, kb, I, S = g.nc, g.kb, g.I, g.S
    with ExitStack() as es:
        A = _alloc(es, nc, "H_A", [128, 256, NE], F32)
        kb.dma("sp", A.t[:], S["affall"].rearrange("(p j) e -> p j e", p=128), reads=[g.DBall["affall"]], writes=[A])
        cmp_ = _alloc(es, nc, "H_cmp", [128, 256, NE], BF16)
        lo = _alloc(es, nc, "H_lo", [128, NE], F32)
        hi = _alloc(es, nc, "H_hi", [128, NE], F32)
        mid = _alloc(es, nc, "H_mid", [128, NE], F32)
        cp = _alloc(es, nc, "H_cp", [128, NE], F32)
        ge = _alloc(es, nc, "H_ge", [128, NE], F32)
        d1 = _alloc(es, nc, "H_d1", [128, NE], F32)
        d2 = _alloc(es, nc, "H_d2", [128, NE], F32)
        pt = _alloc(es, nc, "H_pt", [128, NE], F32, psum=True)
        kb.op("dve", lambda e: e.memset(lo.t[:], 0.0), writes=[lo])
        kb.op("dve", lambda e: e.memset(hi.t[:], 1.0), writes=[hi])
        for it in range(NBIS):
            kb.op("dve", lambda e: e.tensor_tensor(out=mid.t[:], in0=lo.t[:], in1=hi.t[:], op=ALU.add), reads=[lo, hi], writes=[mid])
            kb.op("dve", lambda e: e.tensor_scalar(out=mid.t[:], in0=mid.t[:], scalar1=0.5, scalar2=None, op0=ALU.mult), reads=[mid], writes=[mid])
            kb.op("dve", lambda e: e.tensor_tensor(out=cmp_.t[:], in0=A.t[:], in1=mid.t[:, :].unsqueeze(1).broadcast_to([128, 256, NE]), op=ALU.is_gt),
                  reads=[A, mid], writes=[cmp_])
            kb.op("dve", lambda e: e.tensor_reduce(out=cp.t[:], in_=cmp_.t[:].rearrange("p j e -> p e j"), axis=AX.X, op=ALU.add), reads=[cmp_], writes=[cp])
            kb.op("pe", lambda e: e.matmul(pt.t[:], g.ones_f.t[:], cp.t[:], start=True, stop=True), reads=[g.ones_f, cp], writes=[pt])
            kb.op("dve", lambda e: e.tensor_scalar(out=ge.t[:], in0=pt.t[:], scalar1=float(CAPK) - 0.5, scalar2=None, op0=ALU.is_gt), reads=[pt], writes=[ge])
            kb.op("dve", lambda e: e.tensor_tensor(out=d1.t[:], in0=mid.t[:], in1=lo.t[:], op=ALU.subtract), reads=[mid, lo], writes=[d1])
            kb.op("dve", lambda e: e.tensor_tensor(out=d1.t[:], in0=d1.t[:], in1=ge.t[:], op=ALU.mult), reads=[d1, ge], writes=[d1])
            kb.op("dve", lambda e: e.tensor_tensor(out=d2.t[:], in0=hi.t[:], in1=mid.t[:], op=ALU.subtract), reads=[hi, mid], writes=[d2])
            kb.op("dve", lambda e: e.tensor_tensor(out=d2.t[:], in0=d2.t[:], in1=ge.t[:], op=ALU.mult), reads=[d2, ge], writes=[d2])
            kb.op("dve", lambda e: e.tensor_tensor(out=lo.t[:], in0=lo.t[:], in1=d1.t[:], op=ALU.add), reads=[lo, d1], writes=[lo])
            kb.op("dve", lambda e: e.tensor_tensor(out=hi.t[:], in0=mid.t[:], in1=d2.t[:], op=ALU.add), reads=[mid, d2], writes=[hi])
        M = _alloc(es, nc, "H_M", [128, 256, NE], F32)
        wgt = _alloc(es, nc, "H_wgt", [128, 256, NE], F32)
        Pin = _alloc(es, nc, "H_Pin", [128, NE, 256], F32)
        idx = _alloc(es, nc, "H_idx", [128, NE, 256], U32)
        zer = _alloc(es, nc, "H_zer", [128, 256], F32)
        cnt = _alloc(es, nc, "H_cnt", [128, NE], F32)
        offs = _alloc(es, nc, "H_offs", [128, NE], F32)
        sut = _alloc(es, nc, "H_sut", [128, 128], F32)
        kb.dma("sp", sut.t[:], I["sut"][:, :], writes=[sut])
        kb.op("dve", lambda e: e.memset(zer.t[:], 0.0), writes=[zer])
        kb.op("dve", lambda e: e.tensor_tensor(out=M.t[:], in0=A.t[:], in1=lo.t[:, :].unsqueeze(1).broadcast_to([128, 256, NE]), op=ALU.is_gt),
              reads=[A, lo], writes=[M])
        kb.op("dve", lambda e: e.tensor_tensor(out=wgt.t[:], in0=A.t[:], in1=M.t[:], op=ALU.mult), reads=[A, M], writes=[wgt])
        Mt = M.t[:].rearrange("p j e -> p e j")
        for ex in range(NE):
            kb.op("dve", lambda e, ex=ex: e.tensor_tensor_scan(out=Pin.t[:, ex, :], data0=Mt[:, ex, :], data1=zer.t[:], initial=0.0, op0=ALU.add, op1=ALU.add),
                  reads=[M, zer], writes=[Pin])
        kb.op("dve", lambda e: e.tensor_copy(out=cnt.t[:, :].unsqueeze(2), in_=Pin.t[:, :, 255:256]), reads=[Pin], writes=[cnt])
        kb.op("pe", lambda e: e.matmul(pt.t[:], sut.t[:], cnt.t[:], start=True, stop=True), reads=[sut, cnt], writes=[pt])
        kb.op("dve", lambda e: e.tensor_scalar(out=offs.t[:], in0=pt.t[:], scalar1=-BIG, scalar2=None, op0=ALU.add), reads=[pt], writes=[offs])
        kb.op("dve", lambda e: e.tensor_tensor(out=Pin.t[:], in0=Pin.t[:], in1=Mt, op=ALU.subtract), reads=[Pin, M], writes=[Pin])
        kb.op("dve", lambda e: e.tensor_tensor(out=Pin.t[:], in0=Pin.t[:], in1=offs.t[:, :].unsqueeze(2).broadcast_to([128, NE, 256]), op=ALU.add),
              reads=[Pin, offs], writes=[Pin])
        kb.op("dve", lambda e: e.tensor_tensor(out=Pin.t[:], in0=Pin.t[:], in1=Mt, op=ALU.mult), reads=[Pin, M], writes=[Pin])
        kb.op("dve", lambda e: e.tensor_scalar(out=Pin.t[:], in0=Pin.t[:], scalar1=BIG, scalar2=None, op0=ALU.add), reads=[Pin], writes=[Pin])
        kb.op("dve", lambda e: e.tensor_copy(out=idx.t[:], in_=Pin.t[:]), reads=[Pin], writes=[idx])
        for ex in range(NE):
            kb.dma("pool", g.G[ex][:, :], I["zrows"][:, :], writes=[g.Gbuf[ex]])
        xj = [_alloc(es, nc, "H_xj%d" % i, [128, ROW], BF16) for i in range(4)]
        h2v = S["h2r"].rearrange("(p j) d -> p j d", j=256)
        for j in range(256):
            X = xj[j % 4]
            kb.dma("sp", X.t[:, 0:D], h2v[:, j, :], reads=[g.DBall["h2r"]], writes=[X])
            with nc.allow_non_contiguous_dma(reason="token ids"):
                kb.dma("sp", X.t[:, D + 32:D + 34].bitcast(F32), I["tokid"][:, j:j + 1], writes=[X])
            kb.op("dve", lambda e, j=j: e.tensor_copy(out=X.t[:, D:D + 32].bitcast(F32), in_=wgt.t[:, j, :]), reads=[wgt], writes=[X])
            for ex in range(NE):
                kb.idma(reads=[X, idx], writes=[g.Gbuf[ex]], out=g.G[ex][:, :],
                        out_offset=bass.IndirectOffsetOnAxis(ap=idx.t[:, ex, j:j + 1], axis=0), in_=X.t[:, :], in_offset=None,
                        bounds_check=CAPK - 1, oob_is_err=False)


def stage_I2(g, l):
    nc, kb, I, S = g.nc, g.kb, g.I, g.S
    y = g.y
    NB = 256
    NFC = (DFF + 127) // 128
    with ExitStack() as es:
        wgu = _alloc(es, nc, "I_wgu", [128, 8, 2 * DFF], BF16)
        wd = _alloc(es, nc, "I_wd", [128, NFC, D], BF16)
        Gt = [_alloc(es, nc, "I_G%d" % i, [128, ROW], BF16) for i in range(4)]
        h2 = [_alloc(es, nc, "I_h2%d" % i, [128, 8, NB], BF16) for i in range(2)]
        aT = _alloc(es, nc, "I_aT", [128, NFC, NB], BF16)
        sgt = [_alloc(es, nc, "I_sg%d" % i, [128, NB], F32) for i in range(3)]
        xo = [_alloc(es, nc, "I_x%d" % i, [128, D], F32) for i in range(2)]
        ptr = _alloc(es, nc, "I_ptr", [128, 8, 128], BF16, psum=True)
        pg = [_alloc(es, nc, "I_pg%d" % i, [128, NB], F32, psum=True) for i in range(2)]
        pu = [_alloc(es, nc, "I_pu%d" % i, [128, NB], F32, psum=True) for i in range(2)]
        pq = [_alloc(es, nc, "I_pq%d" % i, [128, 512], F32, psum=True) for i in range(2)]
        n = {"h": 0, "f": 0, "x": 0, "q": 0, "g": 0}
        nblk = int(os.environ.get("KBLK", CAPK // NB))
        for ex in range(NE):
            for kc in range(8):
                kb.dma("pool", wgu.t[:, kc, :], I["w_gate_up"][l, ex, kc * 128:(kc + 1) * 128, :], writes=[wgu])
            for fc in range(NFC):
                fw = min(128, DFF - fc * 128)
                kb.dma("pool", wd.t[0:fw, fc, :], I["w_down"][l, ex, fc * 128:fc * 128 + fw, :], writes=[wd])
            for bk in range(nblk):
                s0 = bk * NB
                hh = h2[n["h"] % 2]
                n["h"] += 1
                gts = []
                for tb in range(NB // 128):
                    G_ = Gt[n["g"] % 4]
                    n["g"] += 1
                    kb.dma("sp", G_.t[:], g.G[ex][s0 + tb * 128:s0 + (tb + 1) * 128, :], reads=[g.Gbuf[ex]], writes=[G_])
                    for kc in range(8):
                        kb.op("pe", lambda e, kc=kc, G_=G_: e.transpose(out=ptr.t[:, kc, :], in_=G_.t[:, kc * 128:(kc + 1) * 128], identity=g.ident_bf.t[:]),
                              reads=[G_, g.ident_bf], writes=[ptr])
                    kb.op("act", lambda e, tb=tb: e.copy(out=hh.t[:, :, tb * 128:(tb + 1) * 128], in_=ptr.t[:]), reads=[ptr], writes=[hh])
                    gts.append(G_)
                for fc in range(NFC):
                    fw = min(128, DFF - fc * 128)
                    p_g = pg[n["f"] % 2]
                    p_u = pu[n["f"] % 2]
                    s_ = sgt[n["f"] % 3]
                    n["f"] += 1
                    for kc in range(8):
                        kb.op("pe", lambda e, kc=kc: e.matmul(p_g.t[0:fw, :], wgu.t[:, kc, fc * 128:fc * 128 + fw], hh.t[:, kc, :], start=(kc == 0), stop=(kc == 7)),
                              reads=[wgu, hh], writes=[p_g])
                    for kc in range(8):
                        kb.op("pe", lambda e, kc=kc: e.matmul(p_u.t[0:fw, :], wgu.t[:, kc, DFF + fc * 128:DFF + fc * 128 + fw], hh.t[:, kc, :], start=(kc == 0), stop=(kc == 7)),
                              reads=[wgu, hh], writes=[p_u])
                    kb.op("act", lambda e: e.activation(out=s_.t[0:fw, :], in_=p_g.t[0:fw, :], func=AF.Sigmoid), reads=[p_g], writes=[s_])
                    kb.op("dve", lambda e: e.tensor_tensor(out=s_.t[0:fw, :], in0=p_g.t[0:fw, :], in1=s_.t[0:fw, :], op=ALU.mult), reads=[p_g, s_], writes=[s_])
                    kb.op("dve", lambda e: e.tensor_tensor(out=aT.t[0:fw, fc, :], in0=p_u.t[0:fw, :], in1=s_.t[0:fw, :], op=ALU.mult), reads=[p_u, s_], writes=[aT])
                for tb in range(NB // 128):
                    G_ = gts[tb]
                    x = xo[n["x"] % 2]
                    n["x"] += 1
                    gate = G_.t[:, D:D + 32].bitcast(F32)[:, ex:ex + 1]
                    for hf in range(2):
                        p = pq[n["q"] % 2]
                        n["q"] += 1
                        for fc in range(NFC):
                            fw = min(128, DFF - fc * 128)
                            kb.op("pe", lambda e, fc=fc, fw=fw: e.matmul(p.t[:], aT.t[0:fw, fc, tb * 128:(tb + 1) * 128], wd.t[0:fw, fc, hf * 512:(hf + 1) * 512],
                                                                        start=(fc == 0), stop=(fc == NFC - 1)), reads=[aT, wd], writes=[p])
                        kb.op("dve", lambda e: e.tensor_scalar(out=x.t[:, hf * 512:(hf + 1) * 512], in0=p.t[:], scalar1=gate, scalar2=None, op0=ALU.mult),
                              reads=[p, G_], writes=[x])
                    kb.idma(reads=[x, G_], writes=[g.ybuf], out=y[:, :],
                            out_offset=bass.IndirectOffsetOnAxis(ap=G_.t[:, D + 32:D + 34].bitcast(U32), axis=0), in_=x.t[:, :], in_offset=None,
                            bounds_check=GT - 1, oob_is_err=False, compute_op=ALU.add)


def host_consts():
    c = {}
    c["ident"] = np.eye(128, dtype=np.float32)
    c["ones"] = np.ones((128, 128), np.float32)
    bo = np.zeros((128, 128), np.float32)
    bo[:64, :64] = 1
    bo[64:, 64:] = 1
    c["bones"] = bo
    s = np.arange(128)[:, None]
    t = np.arange(128)[None, :]
    same = (s // 64) == (t // 64)
    c["trif"] = (same & (s <= t)).astype(np.float32)
    c["trib"] = (same & (s >= t)).astype(np.float32)
    return c


def host_smask(typ):
    m = np.zeros((8, 16), np.float32)
    for i, r in enumerate(range(60, 68)):
        r0 = na_window(r, typ)
        for j, rr in enumerate(range(56, 72)):
            m[i, j] = 0.0 if (r0 <= rr < r0 + 8) else NEG
    return np.ascontiguousarray(np.broadcast_to(m.reshape(1, 128), (128, 128))).astype(np.float32)


def host_bt(rel_bias):
    out = np.zeros((DEPTH, 128, 4, 31, 64), np.float32)
    qc = np.arange(64)
    c0 = np.clip(qc - 8, 0, 48)
    kc = np.arange(64)
    inwin = (kc[:, None] >= c0[None, :]) & (kc[:, None] < c0[None, :] + 16)
    dc = np.clip(kc[:, None] - qc[None, :] + 15, 0, 30)
    for c in range(4):
        for hh in range(2):
            h = 2 * c + hh
            for dr in range(15):
                gath = rel_bias[:, h, dr][:, dc]
                out[:, hh * 64:(hh + 1) * 64, c, dr + 8, :] = np.where(inwin[None], gath, np.float32(NEG))
    return np.ascontiguousarray(out.reshape(DEPTH, 128, 4 * 31 * 64))


_NC_CACHE = {}


def _run(inp, cores):
    if "nc" not in _NC_CACHE:
        _NC_CACHE["nc"] = build_program()
    nc = _NC_CACHE["nc"]
    used = list(nc._k_inputs.keys())
    consts = host_consts()
    bt = host_bt(np.asarray(inp["na_rel_bias"], dtype=np.float32))
    in_maps = []
    for c in cores:
        m = {}
        if c == 0:
            m["x"] = np.ascontiguousarray(inp["x_prompt"].reshape(GT, D))
            m["mem"] = np.ascontiguousarray(inp["mem_prompt"].reshape(NCH, 2, 256, D))
            typ = 0
        else:
            m["x"] = np.ascontiguousarray(inp["x_sample"].reshape(GT, D))
            m["mem"] = np.ascontiguousarray(np.stack([inp["mem_sample"], inp["mem_sample"]], axis=1))
            typ = 1
        m["cflag"] = np.full((128, 1), float(typ), np.float32)
        m["smask"] = host_smask(typ)
        m["bt"] = bt
        m["tokid"] = (np.arange(128, dtype=np.uint32)[:, None] * 256 + np.arange(256, dtype=np.uint32)[None, :]).view(np.float32)
        m["zrows"] = np.zeros((CAPK, ROW), np.float32)
        m["sut"] = (np.arange(128)[:, None] < np.arange(128)[None, :]).astype(np.float32)
        m.update(consts)
        mm = {}
        for k in used:
            mm[k] = np.ascontiguousarray(m[k] if k in m else inp[k], dtype=np.float32)
        in_maps.append(mm)
    res = run_bass_kernel_spmd(nc, in_maps, core_ids=list(range(len(cores))))
    return res


def kernel(**inputs):
    inp = {k: np.asarray(v) for k, v in inputs.items()}
    res = _run(inp, [0, 1])
    ys = [np.asarray(r["y"]) for r in res.results]
    y_prompt = ys[0].reshape(8, 4096, D).astype(np.float32)
    y_sample = ys[1].reshape(4, 8192, D).astype(np.float32)
    return (y_prompt, y_sample)
```
